# Optimizing a Trainium2 kernel written in Bass

```python
import math
import jax, jax.numpy as jnp
from jax import lax
import numpy as np

D_MODEL = 1024
BATCH = 16
SEQ = 2048
DEPTH = 1

MOBA_HEADS = 8
MOBA_HEAD_DIM = 64
MOBA_BLOCK = 256
MOBA_TOPK = 3
MOBA_Q_CHUNK = 64
MOBA_WIDTH = MOBA_HEADS * MOBA_HEAD_DIM
DIFF_HEADS = 4
DIFF_HEAD_DIM = 64
DIFF_Q_BLOCK = 128
DIFF_QK_WIDTH = DIFF_HEADS * 2 * DIFF_HEAD_DIM
DIFF_V_WIDTH = DIFF_HEADS * 2 * DIFF_HEAD_DIM
N_BRANCHES = 2
IN_COLS = 3 * MOBA_WIDTH + 2 * DIFF_QK_WIDTH + DIFF_V_WIDTH + N_BRANCHES * D_MODEL
ROPE_THETA = 10000.0
N_GROUPS = 4
EXPERTS_PER_GROUP = 8
N_EXPERTS = N_GROUPS * EXPERTS_PER_GROUP
EXPERT_TOPK = 2
D_EXPERT = 512
MOE_ROW_BLOCK = 128

EPS = 1e-6
NEG = -1e30

kernel_name = "hybrid_moba_diffattn_hmoe_block"


def rmsnorm(x, g):
    xf = x.astype(jnp.float32)
    var = jnp.mean(xf * xf, axis=-1, keepdims=True)
    return (xf * lax.rsqrt(var + EPS)).astype(x.dtype) * g


def rope_tables(seq, dim, dtype):
    inv = 1.0 / (ROPE_THETA ** (jnp.arange(0, dim, 2, dtype=jnp.float32) / dim))
    ang = jnp.arange(seq, dtype=jnp.float32)[:, None] * inv[None, :]
    ang = jnp.concatenate([ang, ang], axis=-1)
    return jnp.cos(ang).astype(dtype), jnp.sin(ang).astype(dtype)


def apply_rope(x, cos, sin):
    half = x.shape[-1] // 2
    rot = jnp.concatenate([-x[..., half:], x[..., :half]], axis=-1)
    return x * cos[:, None, :] + rot * sin[:, None, :]


def moba_attention(q, k, v):
    B, S, H, dh = q.shape
    nb = max(-(-S // MOBA_BLOCK), MOBA_TOPK)
    L = nb * MOBA_BLOCK
    pad = ((0, 0), (0, L - S), (0, 0), (0, 0))
    kb = jnp.pad(k, pad).reshape(B, nb, MOBA_BLOCK, H, dh).transpose(0, 3, 1, 2, 4)
    vb = jnp.pad(v, pad).reshape(B, nb, MOBA_BLOCK, H, dh).transpose(0, 3, 1, 2, 4)
    kmean = jnp.mean(kb.astype(jnp.float32), axis=3)
    qh = q.transpose(0, 2, 1, 3)
    nq = S // MOBA_Q_CHUNK
    scale = dh ** -0.5
    hidx = jnp.arange(H)[:, None, None]
    blk_ids = jnp.arange(nb)

    def one_batch(args):
        qb, kbh, vbh, kmh = args
        qcs = qb.reshape(H, nq, MOBA_Q_CHUNK, dh).transpose(1, 0, 2, 3)
        starts = jnp.arange(nq, dtype=jnp.int32) * MOBA_Q_CHUNK

        def one_chunk(a):
            qc, start = a
            blk = start // MOBA_BLOCK
            gate = jnp.einsum('hqd,hnd->hqn', qc.astype(jnp.float32), kmh)
            gate = jnp.where(blk_ids < blk, gate, -jnp.inf)
            _, sel = lax.top_k(gate, MOBA_TOPK)
            sel_valid = sel < blk
            ksel = kbh[hidx, sel].reshape(H, MOBA_Q_CHUNK, MOBA_TOPK * MOBA_BLOCK, dh)
            vsel = vbh[hidx, sel].reshape(H, MOBA_Q_CHUNK, MOBA_TOPK * MOBA_BLOCK, dh)
            s_sel = jnp.einsum('hqd,hqkd->hqk', qc, ksel).astype(jnp.float32) * scale
            sel_mask = jnp.repeat(sel_valid, MOBA_BLOCK, axis=-1)
            s_sel = jnp.where(sel_mask, s_sel, NEG)
            kown = lax.dynamic_index_in_dim(kbh, blk, axis=1, keepdims=False)
            vown = lax.dynamic_index_in_dim(vbh, blk, axis=1, keepdims=False)
            s_own = jnp.einsum('hqd,hkd->hqk', qc, kown).astype(jnp.float32) * scale
            qpos = start + jnp.arange(MOBA_Q_CHUNK)
            kpos = blk * MOBA_BLOCK + jnp.arange(MOBA_BLOCK)
            s_own = jnp.where(kpos[None, None, :] <= qpos[None, :, None], s_own, NEG)
            p = jax.nn.softmax(jnp.concatenate([s_sel, s_own], axis=-1), axis=-1).astype(v.dtype)
            n_sel = MOBA_TOPK * MOBA_BLOCK
            out = (jnp.einsum('hqk,hqkd->hqd', p[..., :n_sel], vsel)
                   + jnp.einsum('hqk,hkd->hqd', p[..., n_sel:], vown))
            return out

        o = lax.map(one_chunk, (qcs, starts))
        return o.transpose(0, 2, 1, 3).reshape(S, H, dh)

    return lax.map(one_batch, (qh, kb, vb, kmean))


def diff_attention(q1, q2, k1, k2, v, lam):
    B, S, H, dh = q1.shape
    nq = S // DIFF_Q_BLOCK
    scale = dh ** -0.5
    kpos = jnp.arange(S)
    q1b = q1.reshape(B, nq, DIFF_Q_BLOCK, H, dh).transpose(1, 0, 2, 3, 4)
    q2b = q2.reshape(B, nq, DIFF_Q_BLOCK, H, dh).transpose(1, 0, 2, 3, 4)
    starts = jnp.arange(nq, dtype=jnp.int32) * DIFF_Q_BLOCK

    def one_block(a):
        qb1, qb2, start = a
        qpos = start + jnp.arange(DIFF_Q_BLOCK)
        mask = kpos[None, :] <= qpos[:, None]
        s1 = jnp.einsum('bqhd,bkhd->bhqk', qb1, k1).astype(jnp.float32) * scale
        s2 = jnp.einsum('bqhd,bkhd->bhqk', qb2, k2).astype(jnp.float32) * scale
        p1 = jax.nn.softmax(jnp.where(mask, s1, NEG), axis=-1)
        p2 = jax.nn.softmax(jnp.where(mask, s2, NEG), axis=-1)
        pd = (p1 - lam * p2).astype(v.dtype)
        return jnp.einsum('bhqk,bkhe->bqhe', pd, v)

    o = lax.map(one_block, (q1b, q2b, starts))
    return o.transpose(1, 0, 2, 3, 4).reshape(B, S, H, 2 * dh)


def hier_moe(h, w_group, b_group, w_router, b_router, w1, w3, w2):
    B, S, D = h.shape
    T = B * S
    xt = h.reshape(T, D)
    g_logits = jnp.matmul(xt, w_group).astype(jnp.float32) + b_group
    p_group = jax.nn.softmax(g_logits, axis=-1)
    g_sel = jnp.argmax(g_logits, axis=-1)
    p_g = jnp.take_along_axis(p_group, g_sel[:, None], axis=-1)
    e_logits = (jnp.matmul(xt, w_router).astype(jnp.float32) + b_router).reshape(T, N_GROUPS, EXPERTS_PER_GROUP)
    e_logits = jnp.take_along_axis(e_logits, g_sel[:, None, None], axis=1)[:, 0]
    p_in = jax.nn.softmax(e_logits, axis=-1)
    top_p, top_e = lax.top_k(p_in, EXPERT_TOPK)
    top_w = top_p / jnp.sum(top_p, axis=-1, keepdims=True) * p_g
    expert_id = g_sel[:, None].astype(jnp.int32) * EXPERTS_PER_GROUP + top_e.astype(jnp.int32)

    A = T * EXPERT_TOPK
    m = MOE_ROW_BLOCK
    ids = expert_id.reshape(A)
    tok = jnp.repeat(jnp.arange(T, dtype=jnp.int32), EXPERT_TOPK)
    wts = top_w.reshape(A)
    order = jnp.argsort(ids)
    ids_s, tok_s, w_s = ids[order], tok[order], wts[order]
    counts = jax.ops.segment_sum(jnp.ones((A,), jnp.int32), ids, num_segments=N_EXPERTS)
    starts = jnp.cumsum(counts) - counts
    padded = (counts + m - 1) // m * m
    pends = jnp.cumsum(padded)
    pstarts = pends - padded
    dest = pstarts[ids_s] + (jnp.arange(A, dtype=jnp.int32) - starts[ids_s])
    P = (-(-A // m)) * m + N_EXPERTS * m
    nblk = P // m
    x_disp = jnp.zeros((P, D), h.dtype).at[dest].set(xt[tok_s])
    w_disp = jnp.zeros((P,), jnp.float32).at[dest].set(w_s)
    tok_disp = jnp.zeros((P,), jnp.int32).at[dest].set(tok_s)
    blk_expert = jnp.minimum(jnp.searchsorted(pends, jnp.arange(nblk, dtype=jnp.int32) * m, side='right'),
                             N_EXPERTS - 1).astype(jnp.int32)

    def expert_block(a):
        xblk, e = a
        gate = jnp.matmul(xblk, w1[e])
        up = jnp.matmul(xblk, w3[e])
        return jnp.matmul(jax.nn.silu(gate) * up, w2[e])

    yb = lax.map(expert_block, (x_disp.reshape(nblk, m, D), blk_expert))
    y = jax.ops.segment_sum(yb.reshape(P, D) * w_disp[:, None].astype(h.dtype), tok_disp, num_segments=T)
    return y.reshape(B, S, D)


def setup_inputs(seed: int = 0) -> dict:
    key = jax.random.key(seed)
    ks = jax.random.split(key, 24)
    f32 = jnp.float32
    L, D = DEPTH, D_MODEL

    def nrm(k, shape, scale):
        return jax.random.normal(k, shape, f32) * scale

    return {
        "x": nrm(ks[0], (BATCH, SEQ, D), 1.0),
        "g_mix": 1.0 + nrm(ks[1], (L, D), 0.02),
        "w_in": nrm(ks[2], (L, D, IN_COLS), D ** -0.5),
        "w_branch_moba": nrm(ks[3], (L, MOBA_WIDTH, D), MOBA_WIDTH ** -0.5),
        "w_branch_diff": nrm(ks[4], (L, DIFF_V_WIDTH, D), DIFF_V_WIDTH ** -0.5),
        "w_out": nrm(ks[5], (L, D, D), D ** -0.5),
        "diff_lambda_q1": nrm(ks[6], (L, DIFF_HEAD_DIM), 0.1),
        "diff_lambda_k1": nrm(ks[7], (L, DIFF_HEAD_DIM), 0.1),
        "diff_lambda_q2": nrm(ks[8], (L, DIFF_HEAD_DIM), 0.1),
        "diff_lambda_k2": nrm(ks[9], (L, DIFF_HEAD_DIM), 0.1),
        "diff_subln_g": 1.0 + nrm(ks[10], (L, 2 * DIFF_HEAD_DIM), 0.02),
        "g_ffn": 1.0 + nrm(ks[11], (L, D), 0.02),
        "w_group": nrm(ks[12], (L, D, N_GROUPS), D ** -0.5),
        "b_group": nrm(ks[13], (L, N_GROUPS), 0.01),
        "w_router": nrm(ks[14], (L, D, N_EXPERTS), D ** -0.5),
        "b_router": nrm(ks[15], (L, N_EXPERTS), 0.01),
        "w_expert_gate": nrm(ks[16], (L, N_EXPERTS, D, D_EXPERT), D ** -0.5),
        "w_expert_up": nrm(ks[17], (L, N_EXPERTS, D, D_EXPERT), D ** -0.5),
        "w_expert_down": nrm(ks[18], (L, N_EXPERTS, D_EXPERT, D), D_EXPERT ** -0.5),
        "g_final": 1.0 + nrm(ks[19], (D,), 0.02),
    }


def reference(x, g_mix, w_in, w_branch_moba, w_branch_diff, w_out,
              diff_lambda_q1, diff_lambda_k1, diff_lambda_q2, diff_lambda_k2, diff_subln_g,
              g_ffn, w_group, b_group, w_router, b_router,
              w_expert_gate, w_expert_up, w_expert_down, g_final):
    B, S, D = x.shape
    cos, sin = rope_tables(S, MOBA_HEAD_DIM, x.dtype)
    splits = [int(s) for s in np.cumsum([MOBA_WIDTH, MOBA_WIDTH, MOBA_WIDTH,
                                         DIFF_QK_WIDTH, DIFF_QK_WIDTH, DIFF_V_WIDTH])]
    for l in range(DEPTH):
        h = rmsnorm(x, g_mix[l])
        proj = jnp.matmul(h, w_in[l])
        qa, ka, va, qd, kd, vd, gates = jnp.split(proj, splits, axis=-1)
        qa = apply_rope(qa.reshape(B, S, MOBA_HEADS, MOBA_HEAD_DIM), cos, sin)
        ka = apply_rope(ka.reshape(B, S, MOBA_HEADS, MOBA_HEAD_DIM), cos, sin)
        va = va.reshape(B, S, MOBA_HEADS, MOBA_HEAD_DIM)
        o_a = moba_attention(qa, ka, va).reshape(B, S, MOBA_WIDTH)
        qd = apply_rope(qd.reshape(B, S, DIFF_HEADS * 2, DIFF_HEAD_DIM), cos, sin).reshape(B, S, DIFF_HEADS, 2, DIFF_HEAD_DIM)
        kd = apply_rope(kd.reshape(B, S, DIFF_HEADS * 2, DIFF_HEAD_DIM), cos, sin).reshape(B, S, DIFF_HEADS, 2, DIFF_HEAD_DIM)
        vd = vd.reshape(B, S, DIFF_HEADS, 2 * DIFF_HEAD_DIM)
        lambda_init = 0.8 - 0.6 * math.exp(-0.3 * l)
        lam = (jnp.exp(jnp.sum(diff_lambda_q1[l].astype(jnp.float32) * diff_lambda_k1[l].astype(jnp.float32)))
               - jnp.exp(jnp.sum(diff_lambda_q2[l].astype(jnp.float32) * diff_lambda_k2[l].astype(jnp.float32)))
               + lambda_init)
        o_d = diff_attention(qd[..., 0, :], qd[..., 1, :], kd[..., 0, :], kd[..., 1, :], vd, lam)
        o_d = (rmsnorm(o_d, diff_subln_g[l]) * (1.0 - lambda_init)).reshape(B, S, DIFF_V_WIDTH)
        gsig = jax.nn.sigmoid(gates).reshape(B, S, N_BRANCHES, D)
        merged = (gsig[:, :, 0] * jnp.matmul(o_a, w_branch_moba[l])
                  + gsig[:, :, 1] * jnp.matmul(o_d, w_branch_diff[l]))
        x = x + jnp.matmul(merged, w_out[l])
        h2 = rmsnorm(x, g_ffn[l])
        x = x + hier_moe(h2, w_group[l], b_group[l], w_router[l], b_router[l],
                         w_expert_gate[l], w_expert_up[l], w_expert_down[l])
    return rmsnorm(x, g_final)
```

```python
import math
from contextlib import ExitStack
from functools import partial

import numpy as np
import concourse.bass as bass
import concourse.mybir as mybir
from concourse.bass_utils import run_bass_kernel_spmd

F32 = mybir.dt.float32
BF16 = mybir.dt.bfloat16
I32 = mybir.dt.int32
AF = mybir.ActivationFunctionType
ALU = mybir.AluOpType
AX = mybir.AxisListType

NCORES = 8
SEQ = 2048
D = 1024
TOK = 2 * SEQ
CAP = 512
NE = 32
EPS = 1e-6
NB = 2


class Sched:
    def __init__(self, nc, stack, tag):
        self.nc, self.stack, self.tag = nc, stack, tag
        self.ops, self.lastw, self.readers, self.dsem = [], {}, {}, {}
        self.grpmax, self.gctr = {}, 0
        self.esem = {e: stack.enter_context(nc.semaphore(f"{tag}_{e}")) for e in ("pe", "act", "dve", "pool")}

    EXCL = ("ps", "pG", "pU", "pY", "pT")

    def newgrp(self):
        self.gctr += 1
        return self.gctr

    def add(self, eng, fn, rd=(), wr=(), dkey=None, grp=None):
        ex = [r for r in rd if (r[0] if isinstance(r, tuple) else r) in self.EXCL]
        if ex:
            rd = [r for r in rd if r not in ex]
            wr = list(wr) + [r for r in ex if r not in wr]
        deps = set()
        for r in rd:
            w = self.lastw.get(r)
            if w is not None:
                deps.add(w)
        for r in wr:
            w = self.lastw.get(r)
            if w is not None:
                deps.add(w)
            deps.update(self.readers.get(r, ()))
        i = len(self.ops)
        op = dict(eng=eng, fn=fn, deps=deps, dkey=dkey, inc=False, seq=0)
        if dkey is not None:
            if dkey not in self.dsem:
                self.dsem[dkey] = [self.stack.enter_context(self.nc.semaphore(f"{self.tag}_d_{dkey}")), 0]
            self.dsem[dkey][1] += 16
            op["dval"] = self.dsem[dkey][1]
            op["grp"] = grp
            if grp is not None:
                self.grpmax[(dkey, grp)] = op["dval"]
        self.ops.append(op)
        for r in rd:
            self.readers.setdefault(r, []).append(i)
        for r in wr:
            self.lastw[r] = i
            self.readers[r] = []
        return i

    def finalize(self):
        for op in self.ops:
            for d in op["deps"]:
                Dd = self.ops[d]
                if Dd["dkey"] is None:
                    Dd["inc"] = True
        cnt = {e: 0 for e in self.esem}
        for op in self.ops:
            if op["dkey"] is None and op["inc"]:
                cnt[op["eng"]] += 1
                op["seq"] = cnt[op["eng"]]

    def run(self, eng, e):
        waited = {}
        for op in self.ops:
            if op["eng"] != eng:
                continue
            need = {}
            for d in op["deps"]:
                Dd = self.ops[d]
                if Dd["dkey"] is not None:
                    key, sem, val = "d" + Dd["dkey"], self.dsem[Dd["dkey"]][0], Dd["dval"]
                    if Dd["grp"] is not None:
                        val = self.grpmax[(Dd["dkey"], Dd["grp"])]
                else:
                    if Dd["eng"] == "pe" and eng == "pe" and op["dkey"] is None:
                        continue
                    key, sem, val = Dd["eng"], self.esem[Dd["eng"]], Dd["seq"]
                if key not in need or need[key][1] < val:
                    need[key] = (sem, val)
            for key in sorted(need):
                sem, val = need[key]
                if waited.get(key, 0) >= val:
                    continue
                e.wait_ge(sem, val)
                waited[key] = val
            ins = op["fn"](e)
            if op["dkey"] is not None:
                ins.then_inc(self.dsem[op["dkey"]][0], 16)
            elif op["inc"]:
                ins.then_inc(self.esem[op["eng"]], 1)
        if eng == "sp":
            for k, (sem, val) in self.dsem.items():
                e.wait_ge(sem, val)

    def emit(self):
        self.finalize()
        with self.nc.Block() as block:
            @block.tensor
            def _(e):
                self.run("pe", e)

            @block.scalar
            def _(e):
                self.run("act", e)

            @block.vector
            def _(e):
                self.run("dve", e)

            @block.gpsimd
            def _(e):
                self.run("pool", e)

            @block.sync
            def _(e):
                self.run("sp", e)


CB_IDENT, CB_PERM, CB_ONES, CB_USTR, CB_MASK, CB_COS, CB_SIN = 0, 128, 256, 384, 512, 2560, 4608
NCB = 6656
CF_IDENT, CF_EBASE, CF_GMIXT, CF_GFFNT, CF_GSUB, CF_LAM = 0, 128, 160, 168, 176, 177
NCF = 177 + 256


def build_program(stage=99):
    nc = bass.Bass("TRN2", target_bir_lowering=False)
    bndreg = {}

    def bnd(e, tag):
        if tag not in bndreg:
            r = e.alloc_register("bnd" + tag)
            e.reg_mov(r, NE * CAP - 1)
            bndreg[tag] = r
        return bndreg[tag]

    def din(name, shape, dtype=F32):
        return nc.dram_tensor(name, shape, dtype, kind="ExternalInput").ap()

    x = din("x", [TOK, D])
    w_in = din("w_in", [D, 5120])
    w_bm = din("w_bm", [512, D])
    w_bd = din("w_bd", [512, D])
    w_out = din("w_out", [D, D])
    w1 = din("w1", [NE, D, 512])
    w3 = din("w3", [NE, D, 512])
    w2 = din("w2", [NE, 512, D])
    wr_d = din("wr", [D, 36])
    brow_d = din("brow", [1, 36])
    gffnB_d = din("gffnB", [128, D])
    gfinB_d = din("gfinB", [128, D])
    cf_d = din("cf", [128, NCF])
    cb_d = din("cb", [128, NCB])
    sel_d = din("sel", [8, 1024])
    y = nc.dram_tensor("y", [TOK, D], F32, kind="ExternalOutput").ap()
    ybuf = nc.dram_tensor("ybuf", [NE * CAP, D], F32, kind="ExternalOutput").ap()
    xdisp = nc.dram_tensor("xdisp", [NE * CAP, D], BF16, kind="Internal").ap()

    w_in_k = w_in.rearrange("(k p) c -> p k c", p=128)
    w_bm_k = w_bm.rearrange("(k p) c -> p k c", p=128)
    w_bd_k = w_bd.rearrange("(k p) c -> p k c", p=128)
    w_out_k = w_out.rearrange("(k p) c -> p k c", p=128)
    wr_k = wr_d.rearrange("(k p) c -> p k c", p=128)

    with ExitStack() as top:
        def sb(name, shape, dtype, st=top):
            return st.enter_context(nc.sbuf_tensor(name, shape, dtype))

        sl = sb("sl", [128, 64], I32)
        wts = sb("wts", [128, 64], F32)
        gfinB = sb("gfinB_sb", [128, D], F32)

        with ExitStack() as st:
            S = Sched(nc, top, "A")
            A = partial(sb, st=st)
            ps = [st.enter_context(nc.psum_tensor(f"ps{i}", [128, 512], F32)) for i in range(8)]
            cf = A("cf_sb", [128, NCF], F32)
            cb = A("cb_sb", [128, NCB], BF16)
            sel = A("sel_sb", [8, 1024], BF16)
            gffnB = A("gffnB_sb", [128, D], F32)
            wr = A("wr_sb", [128, 8, 36], F32)
            brow = A("brow_sb", [1, 36], F32)
            onesF = A("onesF", [128, 128], F32)
            epsT = A("epsT", [128, 1], F32)
            hT = A("hT", [128, 8 * SEQ], BF16)
            qkv = A("qkv", [128, 8 * SEQ], BF16)
            oa = A("oa", [128, 4 * SEQ], BF16)
            od = A("od", [128, 4 * SEQ], BF16)
            xt = [A(f"xt{i}", [128, D], F32) for i in range(2)]
            junk = A("junk", [128, D], BF16)
            wq = [A(f"wq{i}", [128, 8, 128], BF16) for i in range(3)]
            wb = [A(f"wb{i}", [128, 4, 128], BF16) for i in range(2)]
            TF = [A(f"tf{i}", [128, 512], F32) for i in range(6)]
            TB = [A(f"tb{i}", [128, 512], BF16) for i in range(6)]
            h2 = [A(f"h2_{i}", [128, D], BF16) for i in range(2)]
            h2T = A("h2T", [128, 8, 128], F32)
            biasT = [A(f"biasT{i}", [8, 1024], BF16) for i in range(2)]
            ksf = A("ksf", [128, 8], F32)
            ksum = [A(f"ksum{i}", [128, 8], BF16) for i in range(2)]
            gsb = [A(f"gsb{i}", [128, 8], F32) for i in range(4)]
            sm = A("sm", [128, 640], F32)
            carry = A("carry", [128, 32], F32)
            neglam = A("neglam", [128, 1], F32)
            gsub8 = A("gsub8", [128, 1], F32)

            identF = cf[:, CF_IDENT:CF_IDENT + 128]
            ebase = cf[:, CF_EBASE:CF_EBASE + 32]
            identB = cb[:, CB_IDENT:CB_IDENT + 128]
            perm = cb[:, CB_PERM:CB_PERM + 128]
            onesB = cb[:, CB_ONES:CB_ONES + 128]
            ustr = cb[:, CB_USTR:CB_USTR + 128]

            def maskj(j, n):
                return cb[:, CB_MASK + j * 512:CB_MASK + j * 512 + n]

            S.add("sp", lambda e: e.dma_start(out=cf[:], in_=cf_d), wr=["cf"], dkey="cf")
            gcb = S.newgrp()
            for i in range(0, NCB, 1664):
                S.add("pool", lambda e, i=i: e.dma_start(out=cb[:, i:i + 1664], in_=cb_d[:, i:i + 1664]),
                      wr=[("cbp", i)], dkey="cb", grp=gcb)
            S.add("dve", lambda e: e.memset(sm[:, 510:511], 0.0), rd=[("cbp", i) for i in range(0, NCB, 1664)], wr=["cb"])
            S.add("pool", lambda e: e.dma_start(out=sel[:], in_=sel_d), wr=["sel"], dkey="sel")
            S.add("sp", lambda e: e.dma_start(out=gffnB[:], in_=gffnB_d), wr=["gffnB"], dkey="gffnB")
            S.add("sp", lambda e: e.dma_start(out=gfinB[:], in_=gfinB_d), wr=["gfinB"], dkey="gfinB")
            S.add("sp", lambda e: e.dma_start(out=wr[:], in_=wr_k), wr=["wr"], dkey="wr")
            S.add("sp", lambda e: e.dma_start(out=brow[:], in_=brow_d), wr=["brow"], dkey="brow")
            zt = A("zt", [128, D], BF16)
            S.add("dve", lambda e: e.memset(zt[:], 0.0), wr=["zt"])
            S.add("dve", lambda e: e.memset(onesF[:], 1.0), wr=["onesF"])
            S.add("dve", lambda e: e.memset(epsT[:], EPS), wr=["epsT"])
            S.add("dve", lambda e: e.memset(carry[:], 0.0), wr=["carry"])
            for i in range(4):
                S.add("dve", lambda e, i=i: e.memset(gsb[i][:], -1e30), wr=[f"gsb{i}"])
            lam = cf[:, CF_LAM:CF_LAM + 256]
            S.add("dve", lambda e: e.tensor_tensor(out=sm[:, 512:576], in0=lam[:, 0:64], in1=lam[:, 64:128], op=ALU.mult),
                  rd=["cf"], wr=["lamsc"])
            S.add("dve", lambda e: e.reduce_sum(out=sm[:, 500:501], in_=sm[:, 512:576], axis=AX.X), rd=["lamsc"], wr=["lamsc"])
            S.add("dve", lambda e: e.tensor_tensor(out=sm[:, 576:640], in0=lam[:, 128:192], in1=lam[:, 192:256], op=ALU.mult),
                  rd=["cf", "lamsc"], wr=["lamsc"])
            S.add("dve", lambda e: e.reduce_sum(out=sm[:, 501:502], in_=sm[:, 576:640], axis=AX.X), rd=["lamsc"], wr=["lamsc"])
            S.add("act", lambda e: e.activation(out=sm[:, 502:504], in_=sm[:, 500:502], func=AF.Exp), rd=["lamsc"], wr=["lamsc"])
            S.add("dve", lambda e: e.tensor_tensor(out=sm[:, 504:505], in0=sm[:, 503:504], in1=sm[:, 502:503], op=ALU.subtract),
                  rd=["lamsc"], wr=["lamsc"])
            S.add("dve", lambda e: e.tensor_scalar(out=neglam[:], in0=sm[:, 504:505], scalar1=-0.2, scalar2=None, op0=ALU.add),
                  rd=["lamsc"], wr=["neglam"])
            S.add("dve", lambda e: e.tensor_scalar(out=gsub8[:], in0=cf[:, CF_GSUB:CF_GSUB + 1], scalar1=0.8, scalar2=None,
                                                   op0=ALU.mult), rd=["cf"], wr=["gsub8"])

            cnt = {"wq": 0, "tf": 0, "tb": 0, "psA": 0}

            def rr(name, n):
                v = cnt[name] % n
                cnt[name] += 1
                return v

            def rmsnorm_rs(src, srcres, col):
                S.add("act", lambda e: e.activation(out=junk[:], in_=src[:], func=AF.Square, accum_out=sm[:, col:col + 1]),
                      rd=[srcres], wr=["junk", ("sm", col)])
                S.add("act", lambda e: e.activation(out=sm[:, col:col + 1], in_=sm[:, col:col + 1], func=AF.Sqrt,
                                                    bias=epsT[:, 0:1], scale=1.0 / D), rd=[("sm", col), "epsT"], wr=[("sm", col)])
                S.add("dve", lambda e: e.reciprocal(out=sm[:, col:col + 1], in_=sm[:, col:col + 1]),
                      rd=[("sm", col)], wr=[("sm", col)])

            def transposes_f32(src, srcres, dst_fn, dstres, gT_off):
                for half in range(2):
                    pb = 2 + rr("psA", 2)
                    for k4 in range(4):
                        k = half * 4 + k4
                        S.add("pe", lambda e, pb=pb, k=k, k4=k4: e.transpose(out=ps[pb][:, k4 * 128:(k4 + 1) * 128],
                                                                             in_=src[:, k * 128:(k + 1) * 128], identity=identF),
                              rd=[srcres, "cf"], wr=[("ps", pb)])
                    for k4 in range(4):
                        k = half * 4 + k4
                        eng = "act" if k4 % 2 == 0 else "dve"
                        if eng == "act":
                            S.add("act", lambda e, pb=pb, k=k, k4=k4: e.activation(
                                out=dst_fn(k), in_=ps[pb][:, k4 * 128:(k4 + 1) * 128], func=AF.Copy,
                                scale=cf[:, gT_off + k:gT_off + k + 1]), rd=[("ps", pb), "cf"], wr=[dstres(k)])
                        else:
                            S.add("dve", lambda e, pb=pb, k=k, k4=k4: e.tensor_scalar(
                                out=dst_fn(k), in0=ps[pb][:, k4 * 128:(k4 + 1) * 128],
                                scalar1=cf[:, gT_off + k:gT_off + k + 1], scalar2=None, op0=ALU.mult),
                                rd=[("ps", pb), "cf"], wr=[dstres(k)])

            def load_wq(c0):
                s = rr("wq", 3)
                g_ = S.newgrp()
                for k in range(8):
                    S.add("pool", lambda e, k=k: e.dma_start(out=wq[s][:, k, :], in_=w_in[k * 128:(k + 1) * 128, c0:c0 + 128]),
                          wr=[("wq", s, k)], dkey=f"wq{s}", grp=g_)
                return s

            def proj_fm(wslot, tc, pb, nk=8, wt=None, src=None, srcres=None):
                for k in range(nk):
                    if wt is None:
                        S.add("pe", lambda e, k=k: e.matmul(ps[pb][:], wq[wslot][:, k, :], hT[:, k * SEQ + tc * 512:k * SEQ + tc * 512 + 512],
                                                            start=(k == 0), stop=(k == nk - 1)),
                              rd=[("wq", wslot, k), ("hT", tc)], wr=[("ps", pb)])
                    else:
                        S.add("pe", lambda e, k=k: e.matmul(ps[pb][:], wt[:, k, :], src[:, k * SEQ + tc * 512:k * SEQ + tc * 512 + 512],
                                                            start=(k == 0), stop=(k == nk - 1)),
                              rd=[srcres[0], (srcres[1], tc)], wr=[("ps", pb)])

            def rope_to(pb, tc, dst, dstres):
                import os
                ROPE = int(os.environ.get("ROPE", "9"))
                if ROPE == 0:
                    S.add("act", lambda e: e.copy(out=dst, in_=ps[pb][:]), rd=[("ps", pb)], wr=[dstres])
                    return
                tbi = rr("tb", 6)
                t1, t2 = rr("tf", 6), rr("tf", 6)
                pr = rr("psA", 2)
                if ROPE == 3:
                    pr += 4
                S.add("act", lambda e: e.copy(out=TB[tbi][:], in_=ps[pb][:]), rd=[("ps", pb)], wr=[("tb", tbi)])
                if ROPE == 4:
                    pr = pb
                else:
                    S.add("pe", lambda e: e.matmul(ps[pr][:], perm, TB[tbi][:], start=True, stop=True),
                          rd=[("tb", tbi), "cb"], wr=[("ps", pr)])
                if ROPE in (2, 3, 4):
                    S.add("dve", lambda e: e.tensor_copy(out=TF[t1][:], in_=ps[pb][:]), rd=[("ps", pb), "cb"], wr=[("tf", t1)])
                    S.add("dve", lambda e: e.tensor_copy(out=TF[t2][:], in_=ps[pr][:]), rd=[("ps", pr), "cb"], wr=[("tf", t2)])
                else:
                    S.add("dve", lambda e: e.tensor_tensor(out=TF[t1][:], in0=ps[pb][:], in1=cb[:, CB_COS + tc * 512:CB_COS + tc * 512 + 512],
                                                           op=ALU.mult), rd=[("ps", pb), "cb"], wr=[("tf", t1)])
                    S.add("dve", lambda e: e.tensor_tensor(out=TF[t2][:], in0=ps[pr][:], in1=cb[:, CB_SIN + tc * 512:CB_SIN + tc * 512 + 512],
                                                           op=ALU.mult), rd=[("ps", pr), "cb"], wr=[("tf", t2)])
                if ROPE in (1, 2, 3, 4):
                    S.add("dve", lambda e: e.tensor_tensor(out=dst, in0=TF[t1][:], in1=TF[t2][:], op=ALU.add),
                          rd=[("tf", t1), ("tf", t2)], wr=[dstres])
                    return
                S.add("pool", lambda e: e.tensor_tensor(out=dst, in0=TF[t1][:], in1=TF[t2][:], op=ALU.add),
                      rd=[("tf", t1), ("tf", t2)], wr=[dstres])

            ALLQ = [("Q", s_, t_) for s_ in range(2) for t_ in range(4)] + [("K", s_, t_) for s_ in range(2) for t_ in range(4)] \
                + [("V", t_) for t_ in range(16)]
            OAALL = [("oa", t_) for t_ in range(4)]

            def stage1(b):
                for tt in range(16):
                    xs = tt % 2
                    r0 = b * SEQ + tt * 128
                    S.add("sp", lambda e, xs=xs, r0=r0: e.dma_start(out=xt[xs][:], in_=x[r0:r0 + 128, :]),
                          wr=[("xt", xs)], dkey=f"xt{xs}")
                    rmsnorm_rs(xt[xs], ("xt", xs), xs)
                    S.add("dve", lambda e, xs=xs: e.tensor_scalar(out=xt[xs][:], in0=xt[xs][:], scalar1=sm[:, xs:xs + 1], scalar2=None,
                                                                  op0=ALU.mult), rd=[("xt", xs), ("sm", xs)], wr=[("xt", xs)])
                    transposes_f32(xt[xs], ("xt", xs),
                                   lambda k, tt=tt: hT[:, k * SEQ + tt * 128:k * SEQ + tt * 128 + 128],
                                   lambda k, tt=tt: ("hT", tt // 4), CF_GMIXT)

            def qk_proj(cq, ck, slot):
                for (c0, base, nm) in ((cq, slot * SEQ, "Q"), (ck, 2 * SEQ + slot * SEQ, "K")):
                    ws = load_wq(c0)
                    for tc in range(4):
                        pb = 2 + rr("psA", 2)
                        proj_fm(ws, tc, pb)
                        rope_to(pb, tc, qkv[:, base + tc * 512:base + tc * 512 + 512], (nm, slot, tc))

            def v_proj(c0):
                g_ = S.newgrp()
                for k in range(8):
                    S.add("pool", lambda e, k=k: e.dma_start(out=od[:, k * 512:(k + 1) * 512], in_=w_in[k * 128:(k + 1) * 128, c0:c0 + 512]),
                          wr=[("od", k)], dkey="wv", grp=g_)
                for tt in range(16):
                    pb = 2 + rr("psA", 2)
                    for k in range(8):
                        S.add("pe", lambda e, k=k, tt=tt, pb=pb: e.matmul(ps[pb][:], hT[:, k * SEQ + tt * 128:k * SEQ + tt * 128 + 128],
                                                                          od[:, k * 512:(k + 1) * 512], start=(k == 0), stop=(k == 7)),
                              rd=[("od", k), ("hT", tt // 4)], wr=[("ps", pb)])
                    if tt % 2 == 0:
                        S.add("act", lambda e, tt=tt, pb=pb: e.copy(out=qkv[:, 4 * SEQ + tt * 512:4 * SEQ + tt * 512 + 512], in_=ps[pb][:]),
                              rd=[("ps", pb)], wr=[("V", tt)])
                    else:
                        S.add("dve", lambda e, tt=tt, pb=pb: e.tensor_copy(out=qkv[:, 4 * SEQ + tt * 512:4 * SEQ + tt * 512 + 512], in_=ps[pb][:]),
                              rd=[("ps", pb)], wr=[("V", tt)])

            def moba_ksum(slot):
                KT0 = 2 * SEQ + slot * SEQ
                S.add("dve", lambda e: e.reduce_sum(out=ksf[:], in_=qkv[:, KT0:KT0 + SEQ].rearrange("p (j t) -> p j t", t=256),
                                                    axis=AX.X), rd=[("K", slot, t) for t in range(4)], wr=["ksf"])
                S.add("dve", lambda e: e.tensor_copy(out=ksum[slot][:], in_=ksf[:]), rd=["ksf"], wr=[("ksum", slot)])

            def moba_head(p, hh, slot, mode="attn", inter=None):
                h = 2 * p + hh
                bp = hh * 64
                bs = h % 2
                QT0, KT0 = slot * SEQ, 2 * SEQ + slot * SEQ
                def gate_step(qt):
                    nb = qt // 2
                    gi = nb - 4
                    pg = 2 + rr("psA", 2)
                    S.add("pe", lambda e, qt=qt, pg=pg: e.matmul(ps[pg][:, 0:8], qkv[bp:bp + 64, QT0 + qt * 128:QT0 + qt * 128 + 128],
                                                                 ksum[slot][bp:bp + 64, 0:8], start=True, stop=True),
                          rd=[("Q", slot, qt // 4), ("ksum", slot)], wr=[("ps", pg)])
                    S.add("dve", lambda e, pg=pg, gi=gi, nb=nb: e.tensor_copy(out=gsb[gi][:, 0:nb], in_=ps[pg][:, 0:nb]),
                          rd=[("ps", pg)], wr=[f"gsb{gi}"])
                    S.add("dve", lambda e, gi=gi: e.max(out=sm[:, 16:24], in_=gsb[gi][:, 0:8]), rd=[f"gsb{gi}"], wr=["m8"])
                    S.add("dve", lambda e, gi=gi: e.tensor_scalar(out=sm[:, 24:32], in0=gsb[gi][:, 0:8], scalar1=sm[:, 18:19],
                                                                  scalar2=30000.0, op0=ALU.is_ge, op1=ALU.mult),
                          rd=[f"gsb{gi}", "m8"], wr=["bq"])
                    pt = 2 + rr("psA", 2)
                    S.add("pe", lambda e, pt=pt: e.transpose(out=ps[pt][0:8, 0:128], in_=sm[:, 24:32], identity=identF),
                          rd=["bq", "cf"], wr=[("ps", pt)])
                    S.add("dve", lambda e, pt=pt, qt=qt: e.tensor_scalar(
                        out=biasT[bs][0:8, (qt - 8) * 128:(qt - 8) * 128 + 128], in0=ps[pt][0:8, 0:128],
                        scalar1=-30000.0, scalar2=None, op0=ALU.add), rd=[("ps", pt)], wr=[("biasT", bs, (qt - 8) // 2)])
                if mode == "gate":
                    return [partial(gate_step, qt) for qt in range(8, 16)]
                for qb in range(8):
                    if inter:
                        inter.pop(0)()
                    po, pl = 4 + (qb % 2) * 2, 5 + (qb % 2) * 2
                    nkt = 2 * qb + 2
                    def pv_ops(kt, tbi, po=po, pl=pl, nkt=nkt):
                        S.add("pe", lambda e: e.matmul(
                            ps[po][0:64, 0:256], qkv[:, 4 * SEQ + kt * 512 + h * 64:4 * SEQ + kt * 512 + h * 64 + 64], TB[tbi][:, 0:256],
                            start=(kt == 0), stop=(kt == nkt - 1)), rd=[("V", kt), ("tb", tbi)], wr=[("ps", po)])
                        S.add("pe", lambda e: e.matmul(
                            ps[pl][0:64, 0:256], onesB[:, 0:64], TB[tbi][:, 0:256],
                            start=(kt == 0), stop=(kt == nkt - 1)), rd=["cb", ("tb", tbi)], wr=[("ps", pl)])
                    pend = None
                    for kt in range(nkt):
                        pS = kt % 2
                        own = (kt // 2 == qb)
                        need_bias = (not own) and qb >= 4
                        S.add("pe", lambda e, kt=kt, pS=pS, qb=qb, nbias=need_bias: e.matmul(
                            ps[pS][:, 0:256], qkv[bp:bp + 64, KT0 + kt * 128:KT0 + kt * 128 + 128],
                            qkv[bp:bp + 64, QT0 + qb * 256:QT0 + qb * 256 + 256], start=True, stop=(not nbias)),
                            rd=[("K", slot, kt // 4), ("Q", slot, qb // 2)], wr=[("ps", pS)])
                        if need_bias:
                            j = kt // 2
                            S.add("pe", lambda e, pS=pS, j=j, qb=qb: e.matmul(
                                ps[pS][:, 0:256], sel[0:8, j * 128:(j + 1) * 128], biasT[bs][0:8, (qb - 4) * 256:(qb - 4) * 256 + 256],
                                start=False, stop=True), rd=["sel", ("biasT", bs, qb - 4)], wr=[("ps", pS)])
                        tbi = rr("tb", 6)
                        S.add("act", lambda e, pS=pS, tbi=tbi: e.activation(out=TB[tbi][:, 0:256], in_=ps[pS][:, 0:256], func=AF.Exp,
                                                                            scale=0.125), rd=[("ps", pS)], wr=[("tb", tbi)])
                        if own:
                            kto = kt - 2 * qb
                            S.add("pool", lambda e, tbi=tbi, kto=kto: e.tensor_tensor(out=TB[tbi][:, 0:256], in0=TB[tbi][:, 0:256],
                                                                                     in1=maskj(kto, 256), op=ALU.mult),
                                  rd=[("tb", tbi), "cb"], wr=[("tb", tbi)])
                        if pend is not None:
                            pv_ops(*pend)
                        pend = (kt, tbi)
                    pv_ops(*pend)
                    t1 = rr("tf", 6)
                    S.add("dve", lambda e, t1=t1, pl=pl: e.reciprocal(out=TF[t1][0:64, 0:256], in_=ps[pl][0:64, 0:256]),
                          rd=[("ps", pl)], wr=[("tf", t1)])
                    S.add("dve", lambda e, t1=t1, po=po, qb=qb: e.tensor_tensor(
                        out=oa[bp:bp + 64, p * SEQ + qb * 256:p * SEQ + qb * 256 + 256], in0=ps[po][0:64, 0:256], in1=TF[t1][0:64, 0:256],
                        op=ALU.mult), rd=[("ps", po), ("tf", t1)], wr=[("oa", qb // 2), "wout_all"] + [("wout", k_) for k_ in range(8)])

            def diff_head(h, slot):
                QT0, KT0 = slot * SEQ, 2 * SEQ + slot * SEQ
                for qc in range(4):
                    nkt = 4 * qc + 4
                    def pv_ops(kt, tbis, nkt=nkt):
                        for m in range(2):
                            tbi = tbis[m]
                            S.add("pe", lambda e, tbi=tbi, m=m: e.matmul(
                                ps[4 + m][:], qkv[:, 4 * SEQ + kt * 512 + h * 128:4 * SEQ + kt * 512 + h * 128 + 128], TB[tbi][:],
                                start=(kt == 0), stop=(kt == nkt - 1)), rd=[("V", kt), ("tb", tbi)], wr=[("ps", 4 + m)])
                            S.add("pe", lambda e, tbi=tbi, m=m: e.matmul(
                                ps[6 + m][:], onesB, TB[tbi][:], start=(kt == 0), stop=(kt == nkt - 1)),
                                rd=["cb", ("tb", tbi)], wr=[("ps", 6 + m)])
                    pend = None
                    for kt in range(nkt):
                        tbis = []
                        for m in range(2):
                            bp = m * 64
                            pS = m
                            S.add("pe", lambda e, kt=kt, pS=pS, bp=bp, qc=qc: e.matmul(
                                ps[pS][:], qkv[bp:bp + 64, KT0 + kt * 128:KT0 + kt * 128 + 128],
                                qkv[bp:bp + 64, QT0 + qc * 512:QT0 + qc * 512 + 512], start=True, stop=True),
                                rd=[("K", slot, kt // 4), ("Q", slot, qc)], wr=[("ps", pS)])
                            tbi = rr("tb", 6)
                            tbis.append(tbi)
                            S.add("act", lambda e, pS=pS, tbi=tbi: e.activation(out=TB[tbi][:], in_=ps[pS][:], func=AF.Exp, scale=0.125),
                                  rd=[("ps", pS)], wr=[("tb", tbi)])
                            if kt >= 4 * qc:
                                j = kt - 4 * qc
                                eng = "pool" if m == 0 else "dve"
                                S.add(eng, lambda e, tbi=tbi, j=j: e.tensor_tensor(out=TB[tbi][:], in0=TB[tbi][:], in1=maskj(j, 512),
                                                                                  op=ALU.mult), rd=[("tb", tbi), "cb"], wr=[("tb", tbi)])
                        if pend is not None:
                            pv_ops(*pend)
                        pend = (kt, tuple(tbis))
                    pv_ops(*pend)
                    r1, r2, u1, u2 = rr("tf", 6), rr("tf", 6), rr("tf", 6), rr("tf", 6)
                    S.add("dve", lambda e, r1=r1: e.reciprocal(out=TF[r1][:], in_=ps[6][:]), rd=[("ps", 6)], wr=[("tf", r1)])
                    S.add("dve", lambda e, r2=r2: e.reciprocal(out=TF[r2][:], in_=ps[7][:]), rd=[("ps", 7)], wr=[("tf", r2)])
                    S.add("dve", lambda e, r1=r1, u1=u1: e.tensor_tensor(out=TF[u1][:], in0=ps[4][:], in1=TF[r1][:], op=ALU.mult),
                          rd=[("ps", 4), ("tf", r1)], wr=[("tf", u1)])
                    S.add("dve", lambda e, r2=r2, u2=u2: e.tensor_tensor(out=TF[u2][:], in0=ps[5][:], in1=TF[r2][:], op=ALU.mult),
                          rd=[("ps", 5), ("tf", r2)], wr=[("tf", u2)])
                    S.add("dve", lambda e, r1=r1, u1=u1, u2=u2: e.scalar_tensor_tensor(
                        out=TF[r1][:], in0=TF[u2][:], scalar=neglam[:, 0:1], in1=TF[u1][:], op0=ALU.mult, op1=ALU.add),
                        rd=[("tf", u1), ("tf", u2), "neglam"], wr=[("tf", r1)])
                    S.add("pool", lambda e, r1=r1, r2=r2: e.tensor_tensor(out=TF[r2][:], in0=TF[r1][:], in1=TF[r1][:], op=ALU.mult),
                          rd=[("tf", r1)], wr=[("tf", r2)])
                    pn = 2 + rr("psA", 2)
                    S.add("pe", lambda e, r2=r2, pn=pn: e.matmul(ps[pn][:], onesF[:], TF[r2][:], start=True, stop=True),
                          rd=[("tf", r2), "onesF"], wr=[("ps", pn)])
                    S.add("act", lambda e, u1=u1, pn=pn: e.activation(out=TF[u1][:], in_=ps[pn][:], func=AF.Sqrt, bias=epsT[:, 0:1],
                                                                      scale=1.0 / 128), rd=[("ps", pn), "epsT"], wr=[("tf", u1)])
                    S.add("dve", lambda e, u1=u1: e.reciprocal(out=TF[u1][:], in_=TF[u1][:]), rd=[("tf", u1)], wr=[("tf", u1)])
                    S.add("dve", lambda e, u1=u1, r1=r1: e.tensor_tensor(out=TF[r1][:], in0=TF[r1][:], in1=TF[u1][:], op=ALU.mult),
                          rd=[("tf", u1), ("tf", r1)], wr=[("tf", r1)])
                    S.add("act", lambda e, r1=r1, qc=qc: e.activation(out=od[:, h * SEQ + qc * 512:h * SEQ + qc * 512 + 512], in_=TF[r1][:],
                                                                      func=AF.Copy, scale=gsub8[:, 0:1]),
                          rd=[("tf", r1), "gsub8"], wr=[("od", h * 4 + qc)])

            def merge_oc(oc):
                g0s = load_wq(3072 + oc * 128)
                g1s = load_wq(4096 + oc * 128)
                g_ = S.newgrp()
                for k in range(4):
                    S.add("pool", lambda e, k=k: e.dma_start(out=wb[0][:, k, :], in_=w_bm[k * 128:(k + 1) * 128, oc * 128:(oc + 1) * 128]),
                          wr=[("wb", 0, k)], dkey="wb0", grp=g_)
                    S.add("pool", lambda e, k=k: e.dma_start(out=wb[1][:, k, :], in_=w_bd[k * 128:(k + 1) * 128, oc * 128:(oc + 1) * 128]),
                          wr=[("wb", 1, k)], dkey="wb1", grp=g_)
                for tc in range(4):
                    sg = []
                    for gi, gs in enumerate((g0s, g1s)):
                        pb = gi
                        proj_fm(gs, tc, pb)
                        tbi = rr("tb", 6)
                        S.add("act", lambda e, pb=pb, tbi=tbi: e.activation(out=TB[tbi][:], in_=ps[pb][:], func=AF.Sigmoid),
                              rd=[("ps", pb)], wr=[("tb", tbi)])
                        sg.append(tbi)
                    ms = []
                    for bi in range(2):
                        src = oa if bi == 0 else od
                        pb = 2 + bi
                        for k in range(4):
                            S.add("pe", lambda e, k=k, bi=bi, pb=pb, src=src, tc=tc: e.matmul(
                                ps[pb][:], wb[bi][:, k, :], src[:, k * SEQ + tc * 512:k * SEQ + tc * 512 + 512], start=(k == 0), stop=(k == 3)),
                                rd=[("wb", bi, k)] + ([("oa", tc)] if bi == 0 else [("od", k * 4 + tc)]), wr=[("ps", pb)])
                        ti = rr("tf", 6)
                        S.add("dve", lambda e, pb=pb, ti=ti, tbi=sg[bi]: e.tensor_tensor(out=TF[ti][:], in0=ps[pb][:], in1=TB[tbi][:], op=ALU.mult),
                              rd=[("ps", pb), ("tb", sg[bi])], wr=[("tf", ti)])
                        ms.append(ti)
                    S.add("pool", lambda e, tc=tc, ms=tuple(ms): e.tensor_tensor(
                        out=qkv[:, oc * SEQ + tc * 512:oc * SEQ + tc * 512 + 512], in0=TF[ms[0]][:], in1=TF[ms[1]][:], op=ALU.add),
                        rd=[("tf", ms[0]), ("tf", ms[1])], wr=ALLQ + [("mg", tc)])

            def load_wout():
                g_ = S.newgrp()
                S.add("pool", lambda e: e.memset(sm[:, 509:510], 0.0), rd=["wout_all"], wr=OAALL + ["oagate"])
                for k in range(8):
                    S.add("pool", lambda e, k=k: e.dma_start(out=oa[:, k * 1024:(k + 1) * 1024], in_=w_out[k * 128:(k + 1) * 128, :]),
                          rd=["oagate"], wr=[("wout", k)], dkey="wo", grp=g_)

            def tail_tile(b, tt):
                xs = tt % 2
                tile = b * 16 + tt
                r0 = b * SEQ + tt * 128
                S.add("sp", lambda e: e.dma_start(out=xt[xs][:], in_=x[r0:r0 + 128, :]), wr=[("xt", xs)], dkey=f"xt{xs}")
                for half in range(2):
                    pb = half
                    for k in range(8):
                        S.add("pe", lambda e, k=k, half=half, pb=pb: e.matmul(
                            ps[pb][:], qkv[:, k * SEQ + tt * 128:k * SEQ + tt * 128 + 128], oa[:, k * 1024 + half * 512:k * 1024 + half * 512 + 512],
                            start=(k == 0), stop=(k == 7)), rd=[("mg", tt // 4), ("wout", k), "wout_all"] + ALLQ + OAALL, wr=[("ps", pb)])
                    S.add("dve", lambda e, half=half, pb=pb: e.tensor_tensor(
                        out=xt[xs][:, half * 512:(half + 1) * 512], in0=ps[pb][:], in1=xt[xs][:, half * 512:(half + 1) * 512], op=ALU.add),
                        rd=[("ps", pb), ("xt", xs)], wr=[("xt", xs)])
                S.add("sp", lambda e: e.dma_start(out=y[r0:r0 + 128, :], in_=xt[xs][:]), rd=[("xt", xs)], wr=[("y", tile)], dkey=f"yst{xs}")
                if stage < 2:
                    return
                rmsnorm_rs(xt[xs], ("xt", xs), 2 + xs)
                S.add("dve", lambda e: e.tensor_scalar(out=xt[xs][:], in0=xt[xs][:], scalar1=sm[:, 2 + xs:3 + xs], scalar2=None,
                                                       op0=ALU.mult), rd=[("xt", xs), ("sm", 2 + xs)], wr=[("xt", xs)])
                S.add("pool", lambda e: e.tensor_tensor(out=h2[xs][:], in0=xt[xs][:], in1=gffnB[:], op=ALU.mult),
                      rd=[("xt", xs), "gffnB"], wr=[("h2", xs)])
                transposes_f32(xt[xs], ("xt", xs), lambda k: h2T[:, k, :], lambda k: "h2T", CF_GFFNT)
                pr = 2 + rr("psA", 2)
                for k in range(8):
                    S.add("pe", lambda e, k=k: e.matmul(ps[pr][:, 0:36], h2T[:, k, :], wr[:, k, :], start=(k == 0), stop=False),
                          rd=["h2T", "wr"], wr=[("ps", pr)])
                S.add("pe", lambda e: e.matmul(ps[pr][:, 0:36], onesF[0:1, :], brow[0:1, :], start=False, stop=True),
                      rd=["onesF", "brow"], wr=[("ps", pr)])
                LG, GM, NGM, GS, PG, GSEL, GB, EM, M8, OH0, MM, OH1, DD, SGD = 32, 68, 69, 70, 71, 72, 76, 80, 112, 120, 152, 184, 216, 217
                RK, OK, SV, TMP = 224, 256, 288, 320
                R = "rt"

                def V(fn, rd=(), wr=(R,)):
                    S.add("dve", fn, rd=[R] + list(rd), wr=list(wr))
                S.add("dve", lambda e: e.tensor_copy(out=sm[:, LG:LG + 36], in_=ps[pr][:, 0:36]), rd=[("ps", pr)], wr=[R])
                V(lambda e: e.reduce_max(out=sm[:, GM:GM + 1], in_=sm[:, LG:LG + 4], axis=AX.X))
                V(lambda e: e.tensor_scalar(out=sm[:, NGM:NGM + 1], in0=sm[:, GM:GM + 1], scalar1=-1.0, scalar2=None, op0=ALU.mult))
                S.add("act", lambda e: e.activation(out=sm[:, GSEL:GSEL + 4], in_=sm[:, LG:LG + 4], func=AF.Exp, bias=sm[:, NGM:NGM + 1],
                                                    accum_out=sm[:, GS:GS + 1]), rd=[R], wr=[R])
                V(lambda e: e.reciprocal(out=sm[:, PG:PG + 1], in_=sm[:, GS:GS + 1]))
                V(lambda e: e.tensor_scalar(out=sm[:, GB:GB + 4], in0=sm[:, LG:LG + 4], scalar1=sm[:, GM:GM + 1], scalar2=1e9,
                                            op0=ALU.is_ge, op1=ALU.mult))
                V(lambda e: e.tensor_scalar(out=sm[:, GB:GB + 4], in0=sm[:, GB:GB + 4], scalar1=-1e9, scalar2=None, op0=ALU.add))
                for g in range(4):
                    V(lambda e, g=g: e.tensor_scalar(out=sm[:, EM + g * 8:EM + g * 8 + 8], in0=sm[:, LG + 4 + g * 8:LG + 12 + g * 8],
                                                     scalar1=sm[:, GB + g:GB + g + 1], scalar2=None, op0=ALU.add))
                V(lambda e: e.max(out=sm[:, M8:M8 + 8], in_=sm[:, EM:EM + 32]))
                V(lambda e: e.tensor_scalar(out=sm[:, OH0:OH0 + 32], in0=sm[:, EM:EM + 32], scalar1=sm[:, M8:M8 + 1], scalar2=None,
                                            op0=ALU.is_ge))
                V(lambda e: e.tensor_scalar(out=sm[:, MM:MM + 32], in0=sm[:, EM:EM + 32], scalar1=sm[:, M8 + 1:M8 + 2], scalar2=None,
                                            op0=ALU.is_ge))
                V(lambda e: e.tensor_tensor(out=sm[:, OH1:OH1 + 32], in0=sm[:, MM:MM + 32], in1=sm[:, OH0:OH0 + 32], op=ALU.subtract))
                V(lambda e: e.tensor_tensor(out=sm[:, DD:DD + 1], in0=sm[:, M8:M8 + 1], in1=sm[:, M8 + 1:M8 + 2], op=ALU.subtract))
                S.add("act", lambda e: e.activation(out=sm[:, SGD:SGD + 1], in_=sm[:, DD:DD + 1], func=AF.Sigmoid), rd=[R], wr=[R])
                V(lambda e: e.tensor_tensor(out=wts[:, 2 * tile:2 * tile + 1], in0=sm[:, SGD:SGD + 1], in1=sm[:, PG:PG + 1],
                                            op=ALU.mult), wr=[R, "wts"])
                V(lambda e: e.tensor_tensor(out=wts[:, 2 * tile + 1:2 * tile + 2], in0=sm[:, PG:PG + 1],
                                            in1=wts[:, 2 * tile:2 * tile + 1], op=ALU.subtract), rd=["wts"], wr=[R, "wts"])
                tbi = rr("tb", 6)
                S.add("dve", lambda e: e.tensor_copy(out=TB[tbi][:, 0:32], in_=sm[:, MM:MM + 32]), rd=[R], wr=[("tb", tbi)])
                pk = 2 + rr("psA", 2)
                S.add("pe", lambda e: e.matmul(ps[pk][:, 0:32], ustr, TB[tbi][:, 0:32], start=True, stop=True),
                      rd=[("tb", tbi), "cb"], wr=[("ps", pk)])
                S.add("pe", lambda e: e.matmul(ps[pk][:, 32:64], onesB, TB[tbi][:, 0:32], start=True, stop=True),
                      rd=[("tb", tbi), "cb"], wr=[("ps", pk)])
                S.add("dve", lambda e: e.tensor_tensor(out=sm[:, RK:RK + 32], in0=ps[pk][:, 0:32], in1=carry[:], op=ALU.add),
                      rd=[R, ("ps", pk), "carry"], wr=[R])
                S.add("dve", lambda e: e.tensor_tensor(out=carry[:], in0=ps[pk][:, 32:64], in1=carry[:], op=ALU.add),
                      rd=[R, ("ps", pk), "carry"], wr=["carry"])
                BIG = float(1 << 22)
                V(lambda e: e.tensor_scalar(out=sm[:, OK:OK + 32], in0=sm[:, RK:RK + 32], scalar1=float(CAP), scalar2=None, op0=ALU.is_lt))
                V(lambda e: e.tensor_tensor(out=sm[:, SV:SV + 32], in0=sm[:, RK:RK + 32], in1=ebase, op=ALU.add), rd=["cf"])
                V(lambda e: e.tensor_scalar(out=sm[:, SV:SV + 32], in0=sm[:, SV:SV + 32], scalar1=-BIG, scalar2=None, op0=ALU.add))
                V(lambda e: e.tensor_tensor(out=sm[:, SV:SV + 32], in0=sm[:, SV:SV + 32], in1=sm[:, OK:OK + 32], op=ALU.mult))
                V(lambda e: e.tensor_scalar(out=sm[:, SV:SV + 32], in0=sm[:, SV:SV + 32], scalar1=BIG, scalar2=None, op0=ALU.add))
                for kk, OH in enumerate((OH0, OH1)):
                    V(lambda e, OH=OH: e.tensor_tensor(out=sm[:, TMP:TMP + 32], in0=sm[:, SV:SV + 32], in1=sm[:, OH:OH + 32], op=ALU.mult))
                    V(lambda e, kk=kk: e.reduce_sum(out=sm[:, TMP + 32 + kk:TMP + 33 + kk], in_=sm[:, TMP:TMP + 32], axis=AX.X))
                    V(lambda e, kk=kk: e.tensor_copy(out=sl[:, 2 * tile + kk:2 * tile + kk + 1],
                                                     in_=sm[:, TMP + 32 + kk:TMP + 33 + kk]), wr=[R, ("sl", tile, kk)])
                    S.add("pool", lambda e, kk=kk: e.indirect_dma_start(
                        out=xdisp, out_offset=bass.IndirectOffsetOnAxis(ap=sl[:, 2 * tile + kk:2 * tile + kk + 1], axis=0),
                        in_=h2[xs][:, :], in_offset=None, bounds_check=bnd(e, "A"), oob_is_err=False),
                        rd=[("h2", xs), ("sl", tile, kk)] + [("xz", i_) for i_ in range(NE * CAP // 128)], wr=["xdisp_w"], dkey=f"sc{xs}{kk}")

            import os
            KSTOP = float(os.environ.get("KSTOP", "99"))
            for b in range(NB):
                stage1(b)
                if b == 0:
                    for i in range(NE * CAP // 128):
                        S.add("sp", lambda e, i=i: e.dma_start(out=xdisp[i * 128:(i + 1) * 128, :], in_=zt[:]), rd=["zt"], wr=[("xz", i)], dkey="xz")
                if KSTOP <= 1:
                    break
                v_proj(1024)
                qk_proj(0, 512, 0)
                moba_ksum(0)
                for st_ in moba_head(0, 0, 0, mode="gate"):
                    st_()
                for p in range(4):
                    moba_head(p, 0, p % 2, inter=moba_head(p, 1, p % 2, mode="gate"))
                    nxt = None
                    if p + 1 < 4:
                        qk_proj((p + 1) * 128, 512 + (p + 1) * 128, (p + 1) % 2)
                        moba_ksum((p + 1) % 2)
                        nxt = moba_head(p + 1, 0, (p + 1) % 2, mode="gate")
                    moba_head(p, 1, p % 2, inter=nxt)
                if KSTOP <= 3:
                    break
                v_proj(2560)
                for h in range(4):
                    qk_proj(1536 + h * 128, 2048 + h * 128, h % 2)
                    diff_head(h, h % 2)
                if KSTOP <= 4:
                    break
                for oc in range(8):
                    merge_oc(oc)
                load_wout()
                if KSTOP <= 5:
                    break
                for tt in range(16):
                    tail_tile(b, tt)
            S.emit()

        if stage < 3:
            return nc
        with ExitStack() as st:
            S = Sched(nc, top, "B")
            A = partial(sb, st=st)
            pTs = [st.enter_context(nc.psum_tensor(f"pT{i}", [128, 1024], BF16)) for i in range(2)]
            pG = [st.enter_context(nc.psum_tensor(f"pG{i}", [128, 512], F32)) for i in range(2)]
            pU = [st.enter_context(nc.psum_tensor(f"pU{i}", [128, 512], F32)) for i in range(2)]
            pY = [st.enter_context(nc.psum_tensor(f"pY{i}", [128, 512], F32)) for i in range(2)]
            identB = A("identB2", [128, 128], BF16)
            w1b = [A(f"w1b{i}", [128, 8, 512], BF16) for i in range(2)]
            w3b = [A(f"w3b{i}", [128, 8, 512], BF16) for i in range(2)]
            w2b = [A(f"w2b{i}", [128, 4, 1024], BF16) for i in range(2)]
            xd = [A(f"xd{i}", [128, D], BF16) for i in range(2)]
            xT = [A(f"xT{i}", [128, 8, CAP], BF16) for i in range(2)]
            sgl = [A(f"sgl{i}", [128, 512], F32) for i in range(2)]
            aT = [A(f"aT{i}", [128, 4, CAP], BF16) for i in range(2)]
            yo = [A(f"yo{i}", [128, D], F32) for i in range(2)]
            S.add("pool", lambda e: e.dma_start(out=identB[:], in_=cb_d[:, CB_IDENT:CB_IDENT + 128]), wr=["identB"], dkey="identB")
            nblk = CAP // 128
            ctr = 0
            w3f = [A(f"w3f{i}", [128, 8, 512], F32) for i in range(2)]
            w2f = [A(f"w2f{i}", [128, 4, 1024], F32) for i in range(2)]

            def load_w(ex):
                s = ex % 2
                g_ = S.newgrp()
                for k in range(8):
                    S.add("pool", lambda e, k=k: e.dma_start(out=w1b[s][:, k, :], in_=w1[ex, k * 128:(k + 1) * 128, :]),
                          wr=[("w1", s, k)], dkey=f"w1_{s}", grp=g_)
                for k in range(8):
                    S.add("act", lambda e, k=k: e.dma_start(out=w3f[s][:, k, :], in_=w3[ex, k * 128:(k + 1) * 128, :]),
                          wr=[("w3f", s, k)], dkey=f"w3f{s}", grp=g_)
                for k in range(4):
                    S.add("act", lambda e, k=k: e.dma_start(out=w2f[s][:, k, :], in_=w2[ex, k * 128:(k + 1) * 128, :]),
                          wr=[("w2f", s, k)], dkey=f"w2f{s}", grp=g_)

            def cast_w(ex):
                s = ex % 2
                for k in range(8):
                    if k % 2 == 0:
                        S.add("act", lambda e, k=k: e.copy(out=w3b[s][:, k, :], in_=w3f[s][:, k, :]), rd=[("w3f", s, k)], wr=[("w3", s, k)])
                    else:
                        S.add("dve", lambda e, k=k: e.tensor_copy(out=w3b[s][:, k, :], in_=w3f[s][:, k, :]), rd=[("w3f", s, k)], wr=[("w3", s, k)])
                for k in range(4):
                    if k % 2 == 0:
                        S.add("dve", lambda e, k=k: e.tensor_copy(out=w2b[s][:, k, :], in_=w2f[s][:, k, :]), rd=[("w2f", s, k)], wr=[("w2", s, k)])
                    else:
                        S.add("act", lambda e, k=k: e.copy(out=w2b[s][:, k, :], in_=w2f[s][:, k, :]), rd=[("w2f", s, k)], wr=[("w2", s, k)])

            load_w(0)
            cast_w(0)
            for ex in range(NE):
                s = ex % 2
                if ex + 1 < NE:
                    load_w(ex + 1)
                for blk in range(nblk):
                    xs = ctr % 2
                    ctr += 1
                    r0 = ex * CAP + blk * 128
                    S.add("sp", lambda e, xs=xs, r0=r0: e.dma_start(out=xd[xs][:], in_=xdisp[r0:r0 + 128, :]), wr=[("xd", xs)], dkey=f"xd{xs}")
                    pi = xs
                    for k in range(8):
                        S.add("pe", lambda e, xs=xs, k=k, pi=pi: e.transpose(out=pTs[pi][:, k * 128:(k + 1) * 128], in_=xd[xs][:, k * 128:(k + 1) * 128],
                                                                             identity=identB[:]), rd=[("xd", xs), "identB"], wr=[("pT", pi)])
                    if xs == 0:
                        S.add("act", lambda e, s=s, blk=blk, pi=pi: e.copy(out=xT[s][:, :, blk * 128:(blk + 1) * 128],
                                                                           in_=pTs[pi][:, :].rearrange("p (k c) -> p k c", c=128)),
                              rd=[("pT", pi)], wr=[("xT", s)])
                    else:
                        S.add("dve", lambda e, s=s, blk=blk, pi=pi: e.tensor_copy(out=xT[s][:, :, blk * 128:(blk + 1) * 128],
                                                                                  in_=pTs[pi][:, :].rearrange("p (k c) -> p k c", c=128)),
                              rd=[("pT", pi)], wr=[("xT", s)])
                for fc in range(4):
                    g = fc % 2
                    for k in range(8):
                        S.add("pe", lambda e, s=s, k=k, fc=fc, g=g: e.matmul(pG[g][:, 0:CAP], w1b[s][:, k, fc * 128:(fc + 1) * 128], xT[s][:, k, :],
                                                                             start=(k == 0), stop=(k == 7)), rd=[("w1", s, k), ("xT", s)], wr=[("pG", g)])
                    for k in range(8):
                        S.add("pe", lambda e, s=s, k=k, fc=fc, g=g: e.matmul(pU[g][:, 0:CAP], w3b[s][:, k, fc * 128:(fc + 1) * 128], xT[s][:, k, :],
                                                                             start=(k == 0), stop=(k == 7)), rd=[("w3", s, k), ("xT", s)], wr=[("pU", g)])
                    S.add("act", lambda e, g=g: e.activation(out=sgl[g][:, 0:CAP], in_=pG[g][:, 0:CAP], func=AF.Silu), rd=[("pG", g)], wr=[("sgl", g)])
                    S.add("dve", lambda e, s=s, fc=fc, g=g: e.tensor_tensor(out=aT[s][:, fc, :], in0=pU[g][:, 0:CAP], in1=sgl[g][:, 0:CAP], op=ALU.mult),
                          rd=[("pU", g), ("sgl", g)], wr=[("aT", s)])
                for blk in range(nblk):
                    ys = (ex * nblk + blk) % 2
                    r0 = ex * CAP + blk * 128
                    for half in range(2):
                        for j in range(4):
                            S.add("pe", lambda e, s=s, j=j, blk=blk, half=half: e.matmul(
                                pY[half][:], aT[s][:, j, blk * 128:(blk + 1) * 128], w2b[s][:, j, half * 512:(half + 1) * 512],
                                start=(j == 0), stop=(j == 3)), rd=[("aT", s), ("w2", s, j)], wr=[("pY", half)])
                        if half == 0:
                            S.add("act", lambda e, ys=ys: e.copy(out=yo[ys][:, 0:512], in_=pY[0][:]), rd=[("pY", 0)], wr=[("yo", ys)])
                        else:
                            S.add("dve", lambda e, ys=ys: e.tensor_copy(out=yo[ys][:, 512:1024], in_=pY[1][:]), rd=[("pY", 1)], wr=[("yo", ys)])
                    S.add("sp", lambda e, ys=ys, r0=r0: e.dma_start(out=ybuf[r0:r0 + 128, :], in_=yo[ys][:]), rd=[("yo", ys)], wr=[("ybuf", ex, blk)],
                          dkey=f"yo{ys}")
                if ex + 1 < NE:
                    cast_w(ex + 1)
            S.emit()

        with ExitStack() as st:
            S = Sched(nc, top, "C")
            A = partial(sb, st=st)
            x1 = [A(f"x1_{i}", [128, D], F32) for i in range(4)]
            g0 = [A(f"g0_{i}", [128, D], F32) for i in range(4)]
            g1 = [A(f"g1_{i}", [128, D], F32) for i in range(4)]
            junk = A("junkC", [128, D], BF16)
            smc = A("smc", [128, 8], F32)
            epsT = A("epsTC", [128, 1], F32)
            S.add("dve", lambda e: e.memset(epsT[:], EPS), wr=["epsT"])
            for tile in range(32):
                s = tile % 4
                r0 = tile * 128
                S.add("sp", lambda e, s=s, r0=r0: e.dma_start(out=x1[s][:], in_=y[r0:r0 + 128, :]), wr=[("x1", s)], dkey=f"x1{s}")
                S.add("pool", lambda e, s=s: e.memset(g0[s][:], 0.0), wr=[("g0", s)])
                S.add("pool", lambda e, s=s: e.memset(g1[s][:], 0.0), wr=[("g1", s)])
                S.add("pool", lambda e, s=s, tile=tile: e.indirect_dma_start(
                    out=g0[s][:, :], out_offset=None, in_=ybuf,
                    in_offset=bass.IndirectOffsetOnAxis(ap=sl[:, 2 * tile:2 * tile + 1], axis=0), bounds_check=bnd(e, "C"), oob_is_err=False),
                    wr=[("g0", s)], dkey=f"g0{s}")
                S.add("pool", lambda e, s=s, tile=tile: e.indirect_dma_start(
                    out=g1[s][:, :], out_offset=None, in_=ybuf,
                    in_offset=bass.IndirectOffsetOnAxis(ap=sl[:, 2 * tile + 1:2 * tile + 2], axis=0), bounds_check=bnd(e, "C"), oob_is_err=False),
                    wr=[("g1", s)], dkey=f"g1{s}")
                S.add("dve", lambda e, s=s, tile=tile: e.scalar_tensor_tensor(out=x1[s][:], in0=g0[s][:], scalar=wts[:, 2 * tile:2 * tile + 1],
                                                                              in1=x1[s][:], op0=ALU.mult, op1=ALU.add),
                      rd=[("g0", s), ("x1", s)], wr=[("x1", s)])
                S.add("dve", lambda e, s=s, tile=tile: e.scalar_tensor_tensor(out=x1[s][:], in0=g1[s][:], scalar=wts[:, 2 * tile + 1:2 * tile + 2],
                                                                              in1=x1[s][:], op0=ALU.mult, op1=ALU.add),
                      rd=[("g1", s), ("x1", s)], wr=[("x1", s)])
                S.add("act", lambda e, s=s: e.activation(out=junk[:], in_=x1[s][:], func=AF.Square, accum_out=smc[:, s:s + 1]),
                      rd=[("x1", s)], wr=["junk", ("smc", s)])
                S.add("act", lambda e, s=s: e.activation(out=smc[:, s:s + 1], in_=smc[:, s:s + 1], func=AF.Sqrt, bias=epsT[:, 0:1], scale=1.0 / D),
                      rd=[("smc", s), "epsT"], wr=[("smc", s)])
                S.add("dve", lambda e, s=s: e.reciprocal(out=smc[:, s:s + 1], in_=smc[:, s:s + 1]), rd=[("smc", s)], wr=[("smc", s)])
                S.add("dve", lambda e, s=s: e.scalar_tensor_tensor(out=x1[s][:], in0=x1[s][:], scalar=smc[:, s:s + 1], in1=gfinB[:],
                                                                   op0=ALU.mult, op1=ALU.mult), rd=[("x1", s), ("smc", s)], wr=[("x1", s)])
                S.add("sp", lambda e, s=s, r0=r0: e.dma_start(out=y[r0:r0 + 128, :], in_=x1[s][:]), rd=[("x1", s)], wr=[("y", tile)], dkey=f"yo{s}")
            S.emit()
    return nc


def _consts():
    cb = np.zeros((128, NCB), np.float32)
    cb[:, CB_IDENT:CB_IDENT + 128] = np.eye(128)
    r = np.arange(128)
    partner = np.where(r % 64 < 32, r + 32, r - 32)
    cb[partner, CB_PERM + r] = 1.0
    cb[:, CB_ONES:CB_ONES + 128] = 1.0
    cb[:, CB_USTR:CB_USTR + 128] = (r[:, None] < r[None, :])
    q = np.arange(512)
    for j in range(4):
        cb[:, CB_MASK + j * 512:CB_MASK + (j + 1) * 512] = (q[None, :] >= j * 128 + r[:, None])
    inv = 1.0 / (10000.0 ** (np.arange(0, 64, 2, dtype=np.float32) / 64.0))
    ang = np.arange(SEQ, dtype=np.float32)[:, None] * inv[None, :].astype(np.float32)
    ang = np.concatenate([ang, ang], axis=-1).astype(np.float32)
    cosT = np.cos(ang).T.astype(np.float32)
    sinT = np.sin(ang).T.astype(np.float32)
    sinS = sinT.copy()
    sinS[:32] *= -1.0
    cb[:, CB_COS:CB_COS + SEQ] = np.concatenate([cosT, cosT], 0)
    cb[:, CB_SIN:CB_SIN + SEQ] = np.concatenate([sinS, sinS], 0)
    sel = np.zeros((8, 1024), np.float32)
    for j in range(8):
        sel[j, j * 128:(j + 1) * 128] = 1.0
    return cb, sel


_STAGE = 99


def _prep(x, g_mix, w_in, w_branch_moba, w_branch_diff, w_out,
           diff_lambda_q1, diff_lambda_k1, diff_lambda_q2, diff_lambda_k2, diff_subln_g,
           g_ffn, w_group, b_group, w_router, b_router,
           w_expert_gate, w_expert_up, w_expert_down, g_final):
    f = lambda a: np.ascontiguousarray(np.asarray(a, dtype=np.float32))
    x = f(x)
    cb, sel = _consts()
    cf = np.zeros((128, NCF), np.float32)
    cf[:, CF_IDENT:CF_IDENT + 128] = np.eye(128)
    cf[:, CF_EBASE:CF_EBASE + 32] = (np.arange(32) * CAP)[None, :]
    cf[:, CF_GMIXT:CF_GMIXT + 8] = f(g_mix)[0].reshape(8, 128).T
    cf[:, CF_GFFNT:CF_GFFNT + 8] = f(g_ffn)[0].reshape(8, 128).T
    cf[:, CF_GSUB] = f(diff_subln_g)[0]
    cf[:, CF_LAM:CF_LAM + 256] = np.concatenate([f(diff_lambda_q1)[0], f(diff_lambda_k1)[0], f(diff_lambda_q2)[0],
                                                 f(diff_lambda_k2)[0]])[None, :]
    shared = {
        "w_in": f(w_in)[0], "w_bm": f(w_branch_moba)[0], "w_bd": f(w_branch_diff)[0], "w_out": f(w_out)[0],
        "w1": f(w_expert_gate)[0], "w3": f(w_expert_up)[0], "w2": f(w_expert_down)[0],
        "wr": np.ascontiguousarray(np.concatenate([f(w_group)[0], f(w_router)[0]], axis=1)),
        "brow": np.ascontiguousarray(np.concatenate([f(b_group)[0], f(b_router)[0]])[None, :]),
        "gffnB": np.ascontiguousarray(np.broadcast_to(f(g_ffn)[0][None, :], (128, D))),
        "gfinB": np.ascontiguousarray(np.broadcast_to(f(g_final)[None, :], (128, D))),
        "cf": cf, "cb": cb, "sel": sel,
    }
    xs = x.reshape(NCORES, TOK, D)
    return shared, xs


def kernel(**inputs):
    shared, xs = _prep(**inputs)
    nc = build_program(_STAGE)
    in_maps = [dict(shared, x=np.ascontiguousarray(xs[c])) for c in range(NCORES)]
    res = run_bass_kernel_spmd(nc, in_maps, core_ids=list(range(NCORES)))
    out = np.stack([np.asarray(r["y"]) for r in res.results], axis=0)
    return out.reshape(16, SEQ, D).astype(np.float32)
```

```python
import math
from contextlib import ExitStack
from functools import partial

import numpy as np
import concourse.bass as bass
import concourse.mybir as mybir
from concourse.bass_utils import run_bass_kernel_spmd

F32 = mybir.dt.float32
BF16 = mybir.dt.bfloat16
I32 = mybir.dt.int32
AF = mybir.ActivationFunctionType
ALU = mybir.AluOpType
AX = mybir.AxisListType

NCORES = 8
SEQ = 2048
D = 1024
TOK = 2 * SEQ
CAP = 512
NE = 32
EPS = 1e-6
NB = 2


class Sched:
    def __init__(self, nc, stack, tag):
        self.nc, self.stack, self.tag = nc, stack, tag
        self.ops, self.lastw, self.readers, self.dsem = [], {}, {}, {}
        self.grpmax, self.gctr = {}, 0
        self.esem = {e: stack.enter_context(nc.semaphore(f"{tag}_{e}")) for e in ("pe", "act", "dve", "pool")}

    EXCL = ("ps", "pG", "pU", "pY", "pT")

    def newgrp(self):
        self.gctr += 1
        return self.gctr

    def add(self, eng, fn, rd=(), wr=(), dkey=None, grp=None):
        ex = [r for r in rd if (r[0] if isinstance(r, tuple) else r) in self.EXCL]
        if ex:
            rd = [r for r in rd if r not in ex]
            wr = list(wr) + [r for r in ex if r not in wr]
        deps = set()
        for r in rd:
            w = self.lastw.get(r)
            if w is not None:
                deps.add(w)
        for r in wr:
            w = self.lastw.get(r)
            if w is not None:
                deps.add(w)
            deps.update(self.readers.get(r, ()))
        i = len(self.ops)
        op = dict(eng=eng, fn=fn, deps=deps, dkey=dkey, inc=False, seq=0)
        if dkey is not None:
            if dkey not in self.dsem:
                self.dsem[dkey] = [self.stack.enter_context(self.nc.semaphore(f"{self.tag}_d_{dkey}")), 0]
            self.dsem[dkey][1] += 16
            op["dval"] = self.dsem[dkey][1]
            op["grp"] = grp
            if grp is not None:
                self.grpmax[(dkey, grp)] = op["dval"]
        self.ops.append(op)
        for r in rd:
            self.readers.setdefault(r, []).append(i)
        for r in wr:
            self.lastw[r] = i
            self.readers[r] = []
        return i

    def finalize(self):
        for op in self.ops:
            for d in op["deps"]:
                Dd = self.ops[d]
                if Dd["dkey"] is None:
                    Dd["inc"] = True
        cnt = {e: 0 for e in self.esem}
        for op in self.ops:
            if op["dkey"] is None and op["inc"]:
                cnt[op["eng"]] += 1
                op["seq"] = cnt[op["eng"]]

    def run(self, eng, e):
        waited = {}
        for op in self.ops:
            if op["eng"] != eng:
                continue
            need = {}
            for d in op["deps"]:
                Dd = self.ops[d]
                if Dd["dkey"] is not None:
                    key, sem, val = "d" + Dd["dkey"], self.dsem[Dd["dkey"]][0], Dd["dval"]
                    if Dd["grp"] is not None:
                        val = self.grpmax[(Dd["dkey"], Dd["grp"])]
                else:
                    if Dd["eng"] == "pe" and eng == "pe" and op["dkey"] is None:
                        continue
                    key, sem, val = Dd["eng"], self.esem[Dd["eng"]], Dd["seq"]
                if key not in need or need[key][1] < val:
                    need[key] = (sem, val)
            for key in sorted(need):
                sem, val = need[key]
                if waited.get(key, 0) >= val:
                    continue
                e.wait_ge(sem, val)
                waited[key] = val
            ins = op["fn"](e)
            if op["dkey"] is not None:
                ins.then_inc(self.dsem[op["dkey"]][0], 16)
            elif op["inc"]:
                ins.then_inc(self.esem[op["eng"]], 1)
        if eng == "sp":
            for k, (sem, val) in self.dsem.items():
                e.wait_ge(sem, val)

    def emit(self):
        self.finalize()
        with self.nc.Block() as block:
            @block.tensor
            def _(e):
                self.run("pe", e)

            @block.scalar
            def _(e):
                self.run("act", e)

            @block.vector
            def _(e):
                self.run("dve", e)

            @block.gpsimd
            def _(e):
                self.run("pool", e)

            @block.sync
            def _(e):
                self.run("sp", e)


CB_IDENT, CB_PERM, CB_ONES, CB_USTR, CB_MASK, CB_COS, CB_SIN = 0, 128, 256, 384, 512, 2560, 4608
NCB = 6656
CF_IDENT, CF_EBASE, CF_GMIXT, CF_GFFNT, CF_GSUB, CF_LAM = 0, 128, 160, 168, 176, 177
NCF = 177 + 256


def build_program(stage=99):
    nc = bass.Bass("TRN2", target_bir_lowering=False)
    bndreg = {}

    def bnd(e, tag):
        if tag not in bndreg:
            r = e.alloc_register("bnd" + tag)
            e.reg_mov(r, NE * CAP - 1)
            bndreg[tag] = r
        return bndreg[tag]

    def din(name, shape, dtype=F32):
        return nc.dram_tensor(name, shape, dtype, kind="ExternalInput").ap()

    x = din("x", [TOK, D])
    w_in = din("w_in", [D, 5120])
    w_bm = din("w_bm", [512, D])
    w_bd = din("w_bd", [512, D])
    w_out = din("w_out", [D, D])
    w1 = din("w1", [NE, D, 512])
    w3 = din("w3", [NE, D, 512])
    w2 = din("w2", [NE, 512, D])
    wr_d = din("wr", [D, 36])
    brow_d = din("brow", [1, 36])
    gffnB_d = din("gffnB", [128, D])
    gfinB_d = din("gfinB", [128, D])
    cf_d = din("cf", [128, NCF])
    cb_d = din("cb", [128, NCB])
    sel_d = din("sel", [8, 1024])
    y = nc.dram_tensor("y", [TOK, D], F32, kind="ExternalOutput").ap()
    ybuf = nc.dram_tensor("ybuf", [NE * CAP, D], F32, kind="ExternalOutput").ap()
    xdisp = nc.dram_tensor("xdisp", [NE * CAP, D], BF16, kind="Internal").ap()

    w_in_k = w_in.rearrange("(k p) c -> p k c", p=128)
    w_bm_k = w_bm.rearrange("(k p) c -> p k c", p=128)
    w_bd_k = w_bd.rearrange("(k p) c -> p k c", p=128)
    w_out_k = w_out.rearrange("(k p) c -> p k c", p=128)
    wr_k = wr_d.rearrange("(k p) c -> p k c", p=128)

    with ExitStack() as top:
        def sb(name, shape, dtype, st=top):
            return st.enter_context(nc.sbuf_tensor(name, shape, dtype))

        sl = sb("sl", [128, 64], I32)
        wts = sb("wts", [128, 64], F32)
        gfinB = sb("gfinB_sb", [128, D], F32)

        with ExitStack() as st:
            S = Sched(nc, top, "A")
            A = partial(sb, st=st)
            ps = [st.enter_context(nc.psum_tensor(f"ps{i}", [128, 512], F32)) for i in range(8)]
            cf = A("cf_sb", [128, NCF], F32)
            cb = A("cb_sb", [128, NCB], BF16)
            sel = A("sel_sb", [8, 1024], BF16)
            gffnB = A("gffnB_sb", [128, D], F32)
            wr = A("wr_sb", [128, 8, 36], F32)
            brow = A("brow_sb", [1, 36], F32)
            onesF = A("onesF", [128, 128], F32)
            epsT = A("epsT", [128, 1], F32)
            hT = A("hT", [128, 8 * SEQ], BF16)
            qkv = A("qkv", [128, 8 * SEQ], BF16)
            oa = A("oa", [128, 4 * SEQ], BF16)
            od = A("od", [128, 4 * SEQ], BF16)
            xt = [A(f"xt{i}", [128, D], F32) for i in range(2)]
            junk = A("junk", [128, D], BF16)
            wq = [A(f"wq{i}", [128, 8, 128], BF16) for i in range(3)]
            wb = [A(f"wb{i}", [128, 4, 128], BF16) for i in range(2)]
            TF = [A(f"tf{i}", [128, 512], F32) for i in range(6)]
            TB = [A(f"tb{i}", [128, 512], BF16) for i in range(6)]
            h2 = [A(f"h2_{i}", [128, D], BF16) for i in range(2)]
            h2T = A("h2T", [128, 8, 128], F32)
            biasT = [A(f"biasT{i}", [8, 1024], BF16) for i in range(2)]
            ksf = A("ksf", [128, 8], F32)
            ksum = [A(f"ksum{i}", [128, 8], BF16) for i in range(2)]
            gsb = [A(f"gsb{i}", [128, 8], F32) for i in range(4)]
            sm = A("sm", [128, 640], F32)
            carry = A("carry", [128, 32], F32)
            neglam = A("neglam", [128, 1], F32)
            gsub8 = A("gsub8", [128, 1], F32)

            identF = cf[:, CF_IDENT:CF_IDENT + 128]
            ebase = cf[:, CF_EBASE:CF_EBASE + 32]
            identB = cb[:, CB_IDENT:CB_IDENT + 128]
            perm = cb[:, CB_PERM:CB_PERM + 128]
            onesB = cb[:, CB_ONES:CB_ONES + 128]
            ustr = cb[:, CB_USTR:CB_USTR + 128]

            def maskj(j, n):
                return cb[:, CB_MASK + j * 512:CB_MASK + j * 512 + n]

            S.add("sp", lambda e: e.dma_start(out=cf[:], in_=cf_d), wr=["cf"], dkey="cf")
            gcb = S.newgrp()
            for i in range(0, NCB, 1664):
                S.add("pool", lambda e, i=i: e.dma_start(out=cb[:, i:i + 1664], in_=cb_d[:, i:i + 1664]),
                      wr=[("cbp", i)], dkey="cb", grp=gcb)
            S.add("dve", lambda e: e.memset(sm[:, 510:511], 0.0), rd=[("cbp", i) for i in range(0, NCB, 1664)], wr=["cb"])
            S.add("pool", lambda e: e.dma_start(out=sel[:], in_=sel_d), wr=["sel"], dkey="sel")
            S.add("sp", lambda e: e.dma_start(out=gffnB[:], in_=gffnB_d), wr=["gffnB"], dkey="gffnB")
            S.add("sp", lambda e: e.dma_start(out=gfinB[:], in_=gfinB_d), wr=["gfinB"], dkey="gfinB")
            S.add("sp", lambda e: e.dma_start(out=wr[:], in_=wr_k), wr=["wr"], dkey="wr")
            S.add("sp", lambda e: e.dma_start(out=brow[:], in_=brow_d), wr=["brow"], dkey="brow")
            zt = A("zt", [128, D], BF16)
            S.add("dve", lambda e: e.memset(zt[:], 0.0), wr=["zt"])
            S.add("dve", lambda e: e.memset(onesF[:], 1.0), wr=["onesF"])
            S.add("dve", lambda e: e.memset(epsT[:], EPS), wr=["epsT"])
            S.add("dve", lambda e: e.memset(carry[:], 0.0), wr=["carry"])
            for i in range(4):
                S.add("dve", lambda e, i=i: e.memset(gsb[i][:], -1e30), wr=[f"gsb{i}"])
            lam = cf[:, CF_LAM:CF_LAM + 256]
            S.add("dve", lambda e: e.tensor_tensor(out=sm[:, 512:576], in0=lam[:, 0:64], in1=lam[:, 64:128], op=ALU.mult),
                  rd=["cf"], wr=["lamsc"])
            S.add("dve", lambda e: e.reduce_sum(out=sm[:, 500:501], in_=sm[:, 512:576], axis=AX.X), rd=["lamsc"], wr=["lamsc"])
            S.add("dve", lambda e: e.tensor_tensor(out=sm[:, 576:640], in0=lam[:, 128:192], in1=lam[:, 192:256], op=ALU.mult),
                  rd=["cf", "lamsc"], wr=["lamsc"])
            S.add("dve", lambda e: e.reduce_sum(out=sm[:, 501:502], in_=sm[:, 576:640], axis=AX.X), rd=["lamsc"], wr=["lamsc"])
            S.add("act", lambda e: e.activation(out=sm[:, 502:504], in_=sm[:, 500:502], func=AF.Exp), rd=["lamsc"], wr=["lamsc"])
            S.add("dve", lambda e: e.tensor_tensor(out=sm[:, 504:505], in0=sm[:, 503:504], in1=sm[:, 502:503], op=ALU.subtract),
                  rd=["lamsc"], wr=["lamsc"])
            S.add("dve", lambda e: e.tensor_scalar(out=neglam[:], in0=sm[:, 504:505], scalar1=-0.2, scalar2=None, op0=ALU.add),
                  rd=["lamsc"], wr=["neglam"])
            S.add("dve", lambda e: e.tensor_scalar(out=gsub8[:], in0=cf[:, CF_GSUB:CF_GSUB + 1], scalar1=0.8, scalar2=None,
                                                   op0=ALU.mult), rd=["cf"], wr=["gsub8"])

            cnt = {"wq": 0, "tf": 0, "tb": 0, "psA": 0}

            def rr(name, n):
                v = cnt[name] % n
                cnt[name] += 1
                return v

            def rmsnorm_rs(src, srcres, col):
                S.add("act", lambda e: e.activation(out=junk[:], in_=src[:], func=AF.Square, accum_out=sm[:, col:col + 1]),
                      rd=[srcres], wr=["junk", ("sm", col)])
                S.add("act", lambda e: e.activation(out=sm[:, col:col + 1], in_=sm[:, col:col + 1], func=AF.Sqrt,
                                                    bias=epsT[:, 0:1], scale=1.0 / D), rd=[("sm", col), "epsT"], wr=[("sm", col)])
                S.add("dve", lambda e: e.reciprocal(out=sm[:, col:col + 1], in_=sm[:, col:col + 1]),
                      rd=[("sm", col)], wr=[("sm", col)])

            def transposes_f32(src, srcres, dst_fn, dstres, gT_off):
                for half in range(2):
                    pb = 2 + rr("psA", 2)
                    for k4 in range(4):
                        k = half * 4 + k4
                        S.add("pe", lambda e, pb=pb, k=k, k4=k4: e.transpose(out=ps[pb][:, k4 * 128:(k4 + 1) * 128],
                                                                             in_=src[:, k * 128:(k + 1) * 128], identity=identF),
                              rd=[srcres, "cf"], wr=[("ps", pb)])
                    for k4 in range(4):
                        k = half * 4 + k4
                        eng = "act" if k4 % 2 == 0 else "dve"
                        if eng == "act":
                            S.add("act", lambda e, pb=pb, k=k, k4=k4: e.activation(
                                out=dst_fn(k), in_=ps[pb][:, k4 * 128:(k4 + 1) * 128], func=AF.Copy,
                                scale=cf[:, gT_off + k:gT_off + k + 1]), rd=[("ps", pb), "cf"], wr=[dstres(k)])
                        else:
                            S.add("dve", lambda e, pb=pb, k=k, k4=k4: e.tensor_scalar(
                                out=dst_fn(k), in0=ps[pb][:, k4 * 128:(k4 + 1) * 128],
                                scalar1=cf[:, gT_off + k:gT_off + k + 1], scalar2=None, op0=ALU.mult),
                                rd=[("ps", pb), "cf"], wr=[dstres(k)])

            def load_wq(c0):
                s = rr("wq", 3)
                g_ = S.newgrp()
                for k in range(8):
                    S.add("pool", lambda e, k=k: e.dma_start(out=wq[s][:, k, :], in_=w_in[k * 128:(k + 1) * 128, c0:c0 + 128]),
                          wr=[("wq", s, k)], dkey=f"wq{s}", grp=g_)
                return s

            def proj_fm(wslot, tc, pb, nk=8, wt=None, src=None, srcres=None):
                for k in range(nk):
                    if wt is None:
                        S.add("pe", lambda e, k=k: e.matmul(ps[pb][:], wq[wslot][:, k, :], hT[:, k * SEQ + tc * 512:k * SEQ + tc * 512 + 512],
                                                            start=(k == 0), stop=(k == nk - 1)),
                              rd=[("wq", wslot, k), ("hT", tc)], wr=[("ps", pb)])
                    else:
                        S.add("pe", lambda e, k=k: e.matmul(ps[pb][:], wt[:, k, :], src[:, k * SEQ + tc * 512:k * SEQ + tc * 512 + 512],
                                                            start=(k == 0), stop=(k == nk - 1)),
                              rd=[srcres[0], (srcres[1], tc)], wr=[("ps", pb)])

            def rope_to(pb, tc, dst, dstres):
                import os
                ROPE = int(os.environ.get("ROPE", "9"))
                if ROPE == 0:
                    S.add("act", lambda e: e.copy(out=dst, in_=ps[pb][:]), rd=[("ps", pb)], wr=[dstres])
                    return
                tbi = rr("tb", 6)
                t1, t2 = rr("tf", 6), rr("tf", 6)
                pr = rr("psA", 2)
                if ROPE == 3:
                    pr += 4
                S.add("act", lambda e: e.copy(out=TB[tbi][:], in_=ps[pb][:]), rd=[("ps", pb)], wr=[("tb", tbi)])
                if ROPE == 4:
                    pr = pb
                else:
                    S.add("pe", lambda e: e.matmul(ps[pr][:], perm, TB[tbi][:], start=True, stop=True),
                          rd=[("tb", tbi), "cb"], wr=[("ps", pr)])
                if ROPE in (2, 3, 4):
                    S.add("dve", lambda e: e.tensor_copy(out=TF[t1][:], in_=ps[pb][:]), rd=[("ps", pb), "cb"], wr=[("tf", t1)])
                    S.add("dve", lambda e: e.tensor_copy(out=TF[t2][:], in_=ps[pr][:]), rd=[("ps", pr), "cb"], wr=[("tf", t2)])
                else:
                    S.add("dve", lambda e: e.tensor_tensor(out=TF[t1][:], in0=ps[pb][:], in1=cb[:, CB_COS + tc * 512:CB_COS + tc * 512 + 512],
                                                           op=ALU.mult), rd=[("ps", pb), "cb"], wr=[("tf", t1)])
                    S.add("dve", lambda e: e.tensor_tensor(out=TF[t2][:], in0=ps[pr][:], in1=cb[:, CB_SIN + tc * 512:CB_SIN + tc * 512 + 512],
                                                           op=ALU.mult), rd=[("ps", pr), "cb"], wr=[("tf", t2)])
                if ROPE in (1, 2, 3, 4):
                    S.add("dve", lambda e: e.tensor_tensor(out=dst, in0=TF[t1][:], in1=TF[t2][:], op=ALU.add),
                          rd=[("tf", t1), ("tf", t2)], wr=[dstres])
                    return
                S.add("pool", lambda e: e.tensor_tensor(out=dst, in0=TF[t1][:], in1=TF[t2][:], op=ALU.add),
                      rd=[("tf", t1), ("tf", t2)], wr=[dstres])

            ALLQ = [("Q", s_, t_) for s_ in range(2) for t_ in range(4)] + [("K", s_, t_) for s_ in range(2) for t_ in range(4)] \
                + [("V", t_) for t_ in range(16)]
            OAALL = [("oa", t_) for t_ in range(4)]

            def stage1(b):
                for tt in range(16):
                    xs = tt % 2
                    r0 = b * SEQ + tt * 128
                    S.add("sp", lambda e, xs=xs, r0=r0: e.dma_start(out=xt[xs][:], in_=x[r0:r0 + 128, :]),
                          wr=[("xt", xs)], dkey=f"xt{xs}")
                    rmsnorm_rs(xt[xs], ("xt", xs), xs)
                    S.add("dve", lambda e, xs=xs: e.tensor_scalar(out=xt[xs][:], in0=xt[xs][:], scalar1=sm[:, xs:xs + 1], scalar2=None,
                                                                  op0=ALU.mult), rd=[("xt", xs), ("sm", xs)], wr=[("xt", xs)])
                    transposes_f32(xt[xs], ("xt", xs),
                                   lambda k, tt=tt: hT[:, k * SEQ + tt * 128:k * SEQ + tt * 128 + 128],
                                   lambda k, tt=tt: ("hT", tt // 4), CF_GMIXT)

            def qk_proj(cq, ck, slot):
                for (c0, base, nm) in ((cq, slot * SEQ, "Q"), (ck, 2 * SEQ + slot * SEQ, "K")):
                    ws = load_wq(c0)
                    for tc in range(4):
                        pb = 2 + rr("psA", 2)
                        proj_fm(ws, tc, pb)
                        rope_to(pb, tc, qkv[:, base + tc * 512:base + tc * 512 + 512], (nm, slot, tc))

            def v_proj(c0):
                g_ = S.newgrp()
                for k in range(8):
                    S.add("pool", lambda e, k=k: e.dma_start(out=od[:, k * 512:(k + 1) * 512], in_=w_in[k * 128:(k + 1) * 128, c0:c0 + 512]),
                          wr=[("od", k)], dkey="wv", grp=g_)
                for tt in range(16):
                    pb = 2 + rr("psA", 2)
                    for k in range(8):
                        S.add("pe", lambda e, k=k, tt=tt, pb=pb: e.matmul(ps[pb][:], hT[:, k * SEQ + tt * 128:k * SEQ + tt * 128 + 128],
                                                                          od[:, k * 512:(k + 1) * 512], start=(k == 0), stop=(k == 7)),
                              rd=[("od", k), ("hT", tt // 4)], wr=[("ps", pb)])
                    if tt % 2 == 0:
                        S.add("act", lambda e, tt=tt, pb=pb: e.copy(out=qkv[:, 4 * SEQ + tt * 512:4 * SEQ + tt * 512 + 512], in_=ps[pb][:]),
                              rd=[("ps", pb)], wr=[("V", tt)])
                    else:
                        S.add("dve", lambda e, tt=tt, pb=pb: e.tensor_copy(out=qkv[:, 4 * SEQ + tt * 512:4 * SEQ + tt * 512 + 512], in_=ps[pb][:]),
                              rd=[("ps", pb)], wr=[("V", tt)])

            def moba_ksum(slot):
                KT0 = 2 * SEQ + slot * SEQ
                S.add("dve", lambda e: e.reduce_sum(out=ksf[:], in_=qkv[:, KT0:KT0 + SEQ].rearrange("p (j t) -> p j t", t=256),
                                                    axis=AX.X), rd=[("K", slot, t) for t in range(4)], wr=["ksf"])
                S.add("dve", lambda e: e.tensor_copy(out=ksum[slot][:], in_=ksf[:]), rd=["ksf"], wr=[("ksum", slot)])

            def moba_head(p, hh, slot, mode="attn", inter=None):
                h = 2 * p + hh
                bp = hh * 64
                bs = h % 2
                QT0, KT0 = slot * SEQ, 2 * SEQ + slot * SEQ
                def gate_step(qt):
                    nb = qt // 2
                    gi = nb - 4
                    pg = 2 + rr("psA", 2)
                    S.add("pe", lambda e, qt=qt, pg=pg: e.matmul(ps[pg][:, 0:8], qkv[bp:bp + 64, QT0 + qt * 128:QT0 + qt * 128 + 128],
                                                                 ksum[slot][bp:bp + 64, 0:8], start=True, stop=True),
                          rd=[("Q", slot, qt // 4), ("ksum", slot)], wr=[("ps", pg)])
                    S.add("dve", lambda e, pg=pg, gi=gi, nb=nb: e.tensor_copy(out=gsb[gi][:, 0:nb], in_=ps[pg][:, 0:nb]),
                          rd=[("ps", pg)], wr=[f"gsb{gi}"])
                    S.add("dve", lambda e, gi=gi: e.max(out=sm[:, 16:24], in_=gsb[gi][:, 0:8]), rd=[f"gsb{gi}"], wr=["m8"])
                    S.add("dve", lambda e, gi=gi: e.tensor_scalar(out=sm[:, 24:32], in0=gsb[gi][:, 0:8], scalar1=sm[:, 18:19],
                                                                  scalar2=30000.0, op0=ALU.is_ge, op1=ALU.mult),
                          rd=[f"gsb{gi}", "m8"], wr=["bq"])
                    pt = 2 + rr("psA", 2)
                    S.add("pe", lambda e, pt=pt: e.transpose(out=ps[pt][0:8, 0:128], in_=sm[:, 24:32], identity=identF),
                          rd=["bq", "cf"], wr=[("ps", pt)])
                    S.add("dve", lambda e, pt=pt, qt=qt: e.tensor_scalar(
                        out=biasT[bs][0:8, (qt - 8) * 128:(qt - 8) * 128 + 128], in0=ps[pt][0:8, 0:128],
                        scalar1=-30000.0, scalar2=None, op0=ALU.add), rd=[("ps", pt)], wr=[("biasT", bs, (qt - 8) // 2)])
                if mode == "gate":
                    return [partial(gate_step, qt) for qt in range(8, 16)]
                for qb in range(8):
                    if inter:
                        inter.pop(0)()
                    po, pl = 4 + (qb % 2) * 2, 5 + (qb % 2) * 2
                    nkt = 2 * qb + 2
                    def pv_ops(kt, tbi, po=po, pl=pl, nkt=nkt):
                        S.add("pe", lambda e: e.matmul(
                            ps[po][0:64, 0:256], qkv[:, 4 * SEQ + kt * 512 + h * 64:4 * SEQ + kt * 512 + h * 64 + 64], TB[tbi][:, 0:256],
                            start=(kt == 0), stop=(kt == nkt - 1)), rd=[("V", kt), ("tb", tbi)], wr=[("ps", po)])
                        S.add("pe", lambda e: e.matmul(
                            ps[pl][0:64, 0:256], onesB[:, 0:64], TB[tbi][:, 0:256],
                            start=(kt == 0), stop=(kt == nkt - 1)), rd=["cb", ("tb", tbi)], wr=[("ps", pl)])
                    pend = None
                    for kt in range(nkt):
                        pS = kt % 2
                        own = (kt // 2 == qb)
                        need_bias = (not own) and qb >= 4
                        S.add("pe", lambda e, kt=kt, pS=pS, qb=qb, nbias=need_bias: e.matmul(
                            ps[pS][:, 0:256], qkv[bp:bp + 64, KT0 + kt * 128:KT0 + kt * 128 + 128],
                            qkv[bp:bp + 64, QT0 + qb * 256:QT0 + qb * 256 + 256], start=True, stop=(not nbias)),
                            rd=[("K", slot, kt // 4), ("Q", slot, qb // 2)], wr=[("ps", pS)])
                        if need_bias:
                            j = kt // 2
                            S.add("pe", lambda e, pS=pS, j=j, qb=qb: e.matmul(
                                ps[pS][:, 0:256], sel[0:8, j * 128:(j + 1) * 128], biasT[bs][0:8, (qb - 4) * 256:(qb - 4) * 256 + 256],
                                start=False, stop=True), rd=["sel", ("biasT", bs, qb - 4)], wr=[("ps", pS)])
                        tbi = rr("tb", 6)
                        S.add("act", lambda e, pS=pS, tbi=tbi: e.activation(out=TB[tbi][:, 0:256], in_=ps[pS][:, 0:256], func=AF.Exp,
                                                                            scale=0.125), rd=[("ps", pS)], wr=[("tb", tbi)])
                        if own:
                            kto = kt - 2 * qb
                            S.add("pool", lambda e, tbi=tbi, kto=kto: e.tensor_tensor(out=TB[tbi][:, 0:256], in0=TB[tbi][:, 0:256],
                                                                                     in1=maskj(kto, 256), op=ALU.mult),
                                  rd=[("tb", tbi), "cb"], wr=[("tb", tbi)])
                        if pend is not None:
                            pv_ops(*pend)
                        pend = (kt, tbi)
                    pv_ops(*pend)
                    t1 = rr("tf", 6)
                    S.add("dve", lambda e, t1=t1, pl=pl: e.reciprocal(out=TF[t1][0:64, 0:256], in_=ps[pl][0:64, 0:256]),
                          rd=[("ps", pl)], wr=[("tf", t1)])
                    S.add("dve", lambda e, t1=t1, po=po, qb=qb: e.tensor_tensor(
                        out=oa[bp:bp + 64, p * SEQ + qb * 256:p * SEQ + qb * 256 + 256], in0=ps[po][0:64, 0:256], in1=TF[t1][0:64, 0:256],
                        op=ALU.mult), rd=[("ps", po), ("tf", t1)], wr=[("oa", qb // 2), "wout_all"] + [("wout", k_) for k_ in range(8)])

            def diff_head(h, slot):
                QT0, KT0 = slot * SEQ, 2 * SEQ + slot * SEQ
                for qc in range(4):
                    nkt = 4 * qc + 4
                    def pv_ops(kt, tbis, nkt=nkt):
                        for m in range(2):
                            tbi = tbis[m]
                            S.add("pe", lambda e, tbi=tbi, m=m: e.matmul(
                                ps[4 + m][:], qkv[:, 4 * SEQ + kt * 512 + h * 128:4 * SEQ + kt * 512 + h * 128 + 128], TB[tbi][:],
                                start=(kt == 0), stop=(kt == nkt - 1)), rd=[("V", kt), ("tb", tbi)], wr=[("ps", 4 + m)])
                            S.add("pe", lambda e, tbi=tbi, m=m: e.matmul(
                                ps[6 + m][:], onesB, TB[tbi][:], start=(kt == 0), stop=(kt == nkt - 1)),
                                rd=["cb", ("tb", tbi)], wr=[("ps", 6 + m)])
                    pend = None
                    for kt in range(nkt):
                        tbis = []
                        for m in range(2):
                            bp = m * 64
                            pS = m
                            S.add("pe", lambda e, kt=kt, pS=pS, bp=bp, qc=qc: e.matmul(
                                ps[pS][:], qkv[bp:bp + 64, KT0 + kt * 128:KT0 + kt * 128 + 128],
                                qkv[bp:bp + 64, QT0 + qc * 512:QT0 + qc * 512 + 512], start=True, stop=True),
                                rd=[("K", slot, kt // 4), ("Q", slot, qc)], wr=[("ps", pS)])
                            tbi = rr("tb", 6)
                            tbis.append(tbi)
                            S.add("act", lambda e, pS=pS, tbi=tbi: e.activation(out=TB[tbi][:], in_=ps[pS][:], func=AF.Exp, scale=0.125),
                                  rd=[("ps", pS)], wr=[("tb", tbi)])
                            if kt >= 4 * qc:
                                j = kt - 4 * qc
                                eng = "pool" if m == 0 else "dve"
                                S.add(eng, lambda e, tbi=tbi, j=j: e.tensor_tensor(out=TB[tbi][:], in0=TB[tbi][:], in1=maskj(j, 512),
                                                                                  op=ALU.mult), rd=[("tb", tbi), "cb"], wr=[("tb", tbi)])
                        if pend is not None:
                            pv_ops(*pend)
                        pend = (kt, tuple(tbis))
                    pv_ops(*pend)
                    r1, r2, u1, u2 = rr("tf", 6), rr("tf", 6), rr("tf", 6), rr("tf", 6)
                    S.add("dve", lambda e, r1=r1: e.reciprocal(out=TF[r1][:], in_=ps[6][:]), rd=[("ps", 6)], wr=[("tf", r1)])
                    S.add("dve", lambda e, r2=r2: e.reciprocal(out=TF[r2][:], in_=ps[7][:]), rd=[("ps", 7)], wr=[("tf", r2)])
                    S.add("dve", lambda e, r1=r1, u1=u1: e.tensor_tensor(out=TF[u1][:], in0=ps[4][:], in1=TF[r1][:], op=ALU.mult),
                          rd=[("ps", 4), ("tf", r1)], wr=[("tf", u1)])
                    S.add("dve", lambda e, r2=r2, u2=u2: e.tensor_tensor(out=TF[u2][:], in0=ps[5][:], in1=TF[r2][:], op=ALU.mult),
                          rd=[("ps", 5), ("tf", r2)], wr=[("tf", u2)])
                    S.add("dve", lambda e, r1=r1, u1=u1, u2=u2: e.scalar_tensor_tensor(
                        out=TF[r1][:], in0=TF[u2][:], scalar=neglam[:, 0:1], in1=TF[u1][:], op0=ALU.mult, op1=ALU.add),
                        rd=[("tf", u1), ("tf", u2), "neglam"], wr=[("tf", r1)])
                    S.add("pool", lambda e, r1=r1, r2=r2: e.tensor_tensor(out=TF[r2][:], in0=TF[r1][:], in1=TF[r1][:], op=ALU.mult),
                          rd=[("tf", r1)], wr=[("tf", r2)])
                    pn = 2 + rr("psA", 2)
                    S.add("pe", lambda e, r2=r2, pn=pn: e.matmul(ps[pn][:], onesF[:], TF[r2][:], start=True, stop=True),
                          rd=[("tf", r2), "onesF"], wr=[("ps", pn)])
                    S.add("act", lambda e, u1=u1, pn=pn: e.activation(out=TF[u1][:], in_=ps[pn][:], func=AF.Sqrt, bias=epsT[:, 0:1],
                                                                      scale=1.0 / 128), rd=[("ps", pn), "epsT"], wr=[("tf", u1)])
                    S.add("dve", lambda e, u1=u1: e.reciprocal(out=TF[u1][:], in_=TF[u1][:]), rd=[("tf", u1)], wr=[("tf", u1)])
                    S.add("dve", lambda e, u1=u1, r1=r1: e.tensor_tensor(out=TF[r1][:], in0=TF[r1][:], in1=TF[u1][:], op=ALU.mult),
                          rd=[("tf", u1), ("tf", r1)], wr=[("tf", r1)])
                    S.add("act", lambda e, r1=r1, qc=qc: e.activation(out=od[:, h * SEQ + qc * 512:h * SEQ + qc * 512 + 512], in_=TF[r1][:],
                                                                      func=AF.Copy, scale=gsub8[:, 0:1]),
                          rd=[("tf", r1), "gsub8"], wr=[("od", h * 4 + qc)])

            def merge_oc(oc):
                g0s = load_wq(3072 + oc * 128)
                g1s = load_wq(4096 + oc * 128)
                g_ = S.newgrp()
                for k in range(4):
                    S.add("pool", lambda e, k=k: e.dma_start(out=wb[0][:, k, :], in_=w_bm[k * 128:(k + 1) * 128, oc * 128:(oc + 1) * 128]),
                          wr=[("wb", 0, k)], dkey="wb0", grp=g_)
                    S.add("pool", lambda e, k=k: e.dma_start(out=wb[1][:, k, :], in_=w_bd[k * 128:(k + 1) * 128, oc * 128:(oc + 1) * 128]),
                          wr=[("wb", 1, k)], dkey="wb1", grp=g_)
                for tc in range(4):
                    sg = []
                    for gi, gs in enumerate((g0s, g1s)):
                        pb = gi
                        proj_fm(gs, tc, pb)
                        tbi = rr("tb", 6)
                        S.add("act", lambda e, pb=pb, tbi=tbi: e.activation(out=TB[tbi][:], in_=ps[pb][:], func=AF.Sigmoid),
                              rd=[("ps", pb)], wr=[("tb", tbi)])
                        sg.append(tbi)
                    ms = []
                    for bi in range(2):
                        src = oa if bi == 0 else od
                        pb = 2 + bi
                        for k in range(4):
                            S.add("pe", lambda e, k=k, bi=bi, pb=pb, src=src, tc=tc: e.matmul(
                                ps[pb][:], wb[bi][:, k, :], src[:, k * SEQ + tc * 512:k * SEQ + tc * 512 + 512], start=(k == 0), stop=(k == 3)),
                                rd=[("wb", bi, k)] + ([("oa", tc)] if bi == 0 else [("od", k * 4 + tc)]), wr=[("ps", pb)])
                        ti = rr("tf", 6)
                        S.add("dve", lambda e, pb=pb, ti=ti, tbi=sg[bi]: e.tensor_tensor(out=TF[ti][:], in0=ps[pb][:], in1=TB[tbi][:], op=ALU.mult),
                              rd=[("ps", pb), ("tb", sg[bi])], wr=[("tf", ti)])
                        ms.append(ti)
                    S.add("pool", lambda e, tc=tc, ms=tuple(ms): e.tensor_tensor(
                        out=qkv[:, oc * SEQ + tc * 512:oc * SEQ + tc * 512 + 512], in0=TF[ms[0]][:], in1=TF[ms[1]][:], op=ALU.add),
                        rd=[("tf", ms[0]), ("tf", ms[1])], wr=ALLQ + [("mg", tc)])

            def load_wout():
                g_ = S.newgrp()
                S.add("pool", lambda e: e.memset(sm[:, 509:510], 0.0), rd=["wout_all"], wr=OAALL + ["oagate"])
                for k in range(8):
                    S.add("pool", lambda e, k=k: e.dma_start(out=oa[:, k * 1024:(k + 1) * 1024], in_=w_out[k * 128:(k + 1) * 128, :]),
                          rd=["oagate"], wr=[("wout", k)], dkey="wo", grp=g_)

            def tail_tile(b, tt):
                xs = tt % 2
                tile = b * 16 + tt
                r0 = b * SEQ + tt * 128
                S.add("sp", lambda e: e.dma_start(out=xt[xs][:], in_=x[r0:r0 + 128, :]), wr=[("xt", xs)], dkey=f"xt{xs}")
                for half in range(2):
                    pb = half
                    for k in range(8):
                        S.add("pe", lambda e, k=k, half=half, pb=pb: e.matmul(
                            ps[pb][:], qkv[:, k * SEQ + tt * 128:k * SEQ + tt * 128 + 128], oa[:, k * 1024 + half * 512:k * 1024 + half * 512 + 512],
                            start=(k == 0), stop=(k == 7)), rd=[("mg", tt // 4), ("wout", k), "wout_all"] + ALLQ + OAALL, wr=[("ps", pb)])
                    S.add("dve", lambda e, half=half, pb=pb: e.tensor_tensor(
                        out=xt[xs][:, half * 512:(half + 1) * 512], in0=ps[pb][:], in1=xt[xs][:, half * 512:(half + 1) * 512], op=ALU.add),
                        rd=[("ps", pb), ("xt", xs)], wr=[("xt", xs)])
                S.add("sp", lambda e: e.dma_start(out=y[r0:r0 + 128, :], in_=xt[xs][:]), rd=[("xt", xs)], wr=[("y", tile)], dkey=f"yst{xs}")
                if stage < 2:
                    return
                rmsnorm_rs(xt[xs], ("xt", xs), 2 + xs)
                S.add("dve", lambda e: e.tensor_scalar(out=xt[xs][:], in0=xt[xs][:], scalar1=sm[:, 2 + xs:3 + xs], scalar2=None,
                                                       op0=ALU.mult), rd=[("xt", xs), ("sm", 2 + xs)], wr=[("xt", xs)])
                S.add("pool", lambda e: e.tensor_tensor(out=h2[xs][:], in0=xt[xs][:], in1=gffnB[:], op=ALU.mult),
                      rd=[("xt", xs), "gffnB"], wr=[("h2", xs)])
                transposes_f32(xt[xs], ("xt", xs), lambda k: h2T[:, k, :], lambda k: "h2T", CF_GFFNT)
                pr = 2 + rr("psA", 2)
                for k in range(8):
                    S.add("pe", lambda e, k=k: e.matmul(ps[pr][:, 0:36], h2T[:, k, :], wr[:, k, :], start=(k == 0), stop=False),
                          rd=["h2T", "wr"], wr=[("ps", pr)])
                S.add("pe", lambda e: e.matmul(ps[pr][:, 0:36], onesF[0:1, :], brow[0:1, :], start=False, stop=True),
                      rd=["onesF", "brow"], wr=[("ps", pr)])
                LG, GM, NGM, GS, PG, GSEL, GB, EM, M8, OH0, MM, OH1, DD, SGD = 32, 68, 69, 70, 71, 72, 76, 80, 112, 120, 152, 184, 216, 217
                RK, OK, SV, TMP = 224, 256, 288, 320
                R = "rt"

                def V(fn, rd=(), wr=(R,)):
                    S.add("dve", fn, rd=[R] + list(rd), wr=list(wr))
                S.add("dve", lambda e: e.tensor_copy(out=sm[:, LG:LG + 36], in_=ps[pr][:, 0:36]), rd=[("ps", pr)], wr=[R])
                V(lambda e: e.reduce_max(out=sm[:, GM:GM + 1], in_=sm[:, LG:LG + 4], axis=AX.X))
                V(lambda e: e.tensor_scalar(out=sm[:, NGM:NGM + 1], in0=sm[:, GM:GM + 1], scalar1=-1.0, scalar2=None, op0=ALU.mult))
                S.add("act", lambda e: e.activation(out=sm[:, GSEL:GSEL + 4], in_=sm[:, LG:LG + 4], func=AF.Exp, bias=sm[:, NGM:NGM + 1],
                                                    accum_out=sm[:, GS:GS + 1]), rd=[R], wr=[R])
                V(lambda e: e.reciprocal(out=sm[:, PG:PG + 1], in_=sm[:, GS:GS + 1]))
                V(lambda e: e.tensor_scalar(out=sm[:, GB:GB + 4], in0=sm[:, LG:LG + 4], scalar1=sm[:, GM:GM + 1], scalar2=1e9,
                                            op0=ALU.is_ge, op1=ALU.mult))
                V(lambda e: e.tensor_scalar(out=sm[:, GB:GB + 4], in0=sm[:, GB:GB + 4], scalar1=-1e9, scalar2=None, op0=ALU.add))
                for g in range(4):
                    V(lambda e, g=g: e.tensor_scalar(out=sm[:, EM + g * 8:EM + g * 8 + 8], in0=sm[:, LG + 4 + g * 8:LG + 12 + g * 8],
                                                     scalar1=sm[:, GB + g:GB + g + 1], scalar2=None, op0=ALU.add))
                V(lambda e: e.max(out=sm[:, M8:M8 + 8], in_=sm[:, EM:EM + 32]))
                V(lambda e: e.tensor_scalar(out=sm[:, OH0:OH0 + 32], in0=sm[:, EM:EM + 32], scalar1=sm[:, M8:M8 + 1], scalar2=None,
                                            op0=ALU.is_ge))
                V(lambda e: e.tensor_scalar(out=sm[:, MM:MM + 32], in0=sm[:, EM:EM + 32], scalar1=sm[:, M8 + 1:M8 + 2], scalar2=None,
                                            op0=ALU.is_ge))
                V(lambda e: e.tensor_tensor(out=sm[:, OH1:OH1 + 32], in0=sm[:, MM:MM + 32], in1=sm[:, OH0:OH0 + 32], op=ALU.subtract))
                V(lambda e: e.tensor_tensor(out=sm[:, DD:DD + 1], in0=sm[:, M8:M8 + 1], in1=sm[:, M8 + 1:M8 + 2], op=ALU.subtract))
                S.add("act", lambda e: e.activation(out=sm[:, SGD:SGD + 1], in_=sm[:, DD:DD + 1], func=AF.Sigmoid), rd=[R], wr=[R])
                V(lambda e: e.tensor_tensor(out=wts[:, 2 * tile:2 * tile + 1], in0=sm[:, SGD:SGD + 1], in1=sm[:, PG:PG + 1],
                                            op=ALU.mult), wr=[R, "wts"])
                V(lambda e: e.tensor_tensor(out=wts[:, 2 * tile + 1:2 * tile + 2], in0=sm[:, PG:PG + 1],
                                            in1=wts[:, 2 * tile:2 * tile + 1], op=ALU.subtract), rd=["wts"], wr=[R, "wts"])
                tbi = rr("tb", 6)
                S.add("dve", lambda e: e.tensor_copy(out=TB[tbi][:, 0:32], in_=sm[:, MM:MM + 32]), rd=[R], wr=[("tb", tbi)])
                pk = 2 + rr("psA", 2)
                S.add("pe", lambda e: e.matmul(ps[pk][:, 0:32], ustr, TB[tbi][:, 0:32], start=True, stop=True),
                      rd=[("tb", tbi), "cb"], wr=[("ps", pk)])
                S.add("pe", lambda e: e.matmul(ps[pk][:, 32:64], onesB, TB[tbi][:, 0:32], start=True, stop=True),
                      rd=[("tb", tbi), "cb"], wr=[("ps", pk)])
                S.add("dve", lambda e: e.tensor_tensor(out=sm[:, RK:RK + 32], in0=ps[pk][:, 0:32], in1=carry[:], op=ALU.add),
                      rd=[R, ("ps", pk), "carry"], wr=[R])
                S.add("dve", lambda e: e.tensor_tensor(out=carry[:], in0=ps[pk][:, 32:64], in1=carry[:], op=ALU.add),
                      rd=[R, ("ps", pk), "carry"], wr=["carry"])
                BIG = float(1 << 22)
                V(lambda e: e.tensor_scalar(out=sm[:, OK:OK + 32], in0=sm[:, RK:RK + 32], scalar1=float(CAP), scalar2=None, op0=ALU.is_lt))
                V(lambda e: e.tensor_tensor(out=sm[:, SV:SV + 32], in0=sm[:, RK:RK + 32], in1=ebase, op=ALU.add), rd=["cf"])
                V(lambda e: e.tensor_scalar(out=sm[:, SV:SV + 32], in0=sm[:, SV:SV + 32], scalar1=-BIG, scalar2=None, op0=ALU.add))
                V(lambda e: e.tensor_tensor(out=sm[:, SV:SV + 32], in0=sm[:, SV:SV + 32], in1=sm[:, OK:OK + 32], op=ALU.mult))
                V(lambda e: e.tensor_scalar(out=sm[:, SV:SV + 32], in0=sm[:, SV:SV + 32], scalar1=BIG, scalar2=None, op0=ALU.add))
                for kk, OH in enumerate((OH0, OH1)):
                    V(lambda e, OH=OH: e.tensor_tensor(out=sm[:, TMP:TMP + 32], in0=sm[:, SV:SV + 32], in1=sm[:, OH:OH + 32], op=ALU.mult))
                    V(lambda e, kk=kk: e.reduce_sum(out=sm[:, TMP + 32 + kk:TMP + 33 + kk], in_=sm[:, TMP:TMP + 32], axis=AX.X))
                    V(lambda e, kk=kk: e.tensor_copy(out=sl[:, 2 * tile + kk:2 * tile + kk + 1],
                                                     in_=sm[:, TMP + 32 + kk:TMP + 33 + kk]), wr=[R, ("sl", tile, kk)])
                    S.add("pool", lambda e, kk=kk: e.indirect_dma_start(
                        out=xdisp, out_offset=bass.IndirectOffsetOnAxis(ap=sl[:, 2 * tile + kk:2 * tile + kk + 1], axis=0),
                        in_=h2[xs][:, :], in_offset=None, bounds_check=bnd(e, "A"), oob_is_err=False),
                        rd=[("h2", xs), ("sl", tile, kk)] + [("xz", i_) for i_ in range(NE * CAP // 128)], wr=["xdisp_w"], dkey=f"sc{xs}{kk}")

            import os
            KSTOP = float(os.environ.get("KSTOP", "99"))
            for b in range(NB):
                stage1(b)
                if b == 0:
                    for i in range(NE * CAP // 128):
                        S.add("sp", lambda e, i=i: e.dma_start(out=xdisp[i * 128:(i + 1) * 128, :], in_=zt[:]), rd=["zt"], wr=[("xz", i)], dkey="xz")
                if KSTOP <= 1:
                    break
                v_proj(1024)
                for p in range(4):
                    qk_proj(p * 128, 512 + p * 128, p % 2)
                    moba_ksum(p % 2)
                    for hh in range(2):
                        for st_ in moba_head(p, hh, p % 2, mode="gate"):
                            st_()
                        moba_head(p, hh, p % 2)
                if KSTOP <= 3:
                    break
                v_proj(2560)
                for h in range(4):
                    qk_proj(1536 + h * 128, 2048 + h * 128, h % 2)
                    diff_head(h, h % 2)
                if KSTOP <= 4:
                    break
                for oc in range(8):
                    merge_oc(oc)
                load_wout()
                if KSTOP <= 5:
                    break
                for tt in range(16):
                    tail_tile(b, tt)
            S.emit()

        if stage < 3:
            return nc
        with ExitStack() as st:
            S = Sched(nc, top, "B")
            A = partial(sb, st=st)
            pTs = [st.enter_context(nc.psum_tensor(f"pT{i}", [128, 1024], BF16)) for i in range(2)]
            pG = [st.enter_context(nc.psum_tensor(f"pG{i}", [128, 512], F32)) for i in range(2)]
            pU = [st.enter_context(nc.psum_tensor(f"pU{i}", [128, 512], F32)) for i in range(2)]
            pY = [st.enter_context(nc.psum_tensor(f"pY{i}", [128, 512], F32)) for i in range(2)]
            identB = A("identB2", [128, 128], BF16)
            w1b = [A(f"w1b{i}", [128, 8, 512], BF16) for i in range(2)]
            w3b = [A(f"w3b{i}", [128, 8, 512], BF16) for i in range(2)]
            w2b = [A(f"w2b{i}", [128, 4, 1024], BF16) for i in range(2)]
            xd = [A(f"xd{i}", [128, D], BF16) for i in range(2)]
            xT = [A(f"xT{i}", [128, 8, CAP], BF16) for i in range(2)]
            sgl = [A(f"sgl{i}", [128, 512], F32) for i in range(2)]
            aT = [A(f"aT{i}", [128, 4, CAP], BF16) for i in range(2)]
            yo = [A(f"yo{i}", [128, D], F32) for i in range(2)]
            S.add("pool", lambda e: e.dma_start(out=identB[:], in_=cb_d[:, CB_IDENT:CB_IDENT + 128]), wr=["identB"], dkey="identB")
            nblk = CAP // 128
            ctr = 0
            w3f = [A(f"w3f{i}", [128, 8, 512], F32) for i in range(2)]
            w2f = [A(f"w2f{i}", [128, 4, 1024], F32) for i in range(2)]

            def load_w(ex):
                s = ex % 2
                g_ = S.newgrp()
                for k in range(8):
                    S.add("pool", lambda e, k=k: e.dma_start(out=w1b[s][:, k, :], in_=w1[ex, k * 128:(k + 1) * 128, :]),
                          wr=[("w1", s, k)], dkey=f"w1_{s}", grp=g_)
                for k in range(8):
                    S.add("act", lambda e, k=k: e.dma_start(out=w3f[s][:, k, :], in_=w3[ex, k * 128:(k + 1) * 128, :]),
                          wr=[("w3f", s, k)], dkey=f"w3f{s}", grp=g_)
                for k in range(4):
                    S.add("act", lambda e, k=k: e.dma_start(out=w2f[s][:, k, :], in_=w2[ex, k * 128:(k + 1) * 128, :]),
                          wr=[("w2f", s, k)], dkey=f"w2f{s}", grp=g_)

            def cast_w(ex):
                s = ex % 2
                for k in range(8):
                    if k % 2 == 0:
                        S.add("act", lambda e, k=k: e.copy(out=w3b[s][:, k, :], in_=w3f[s][:, k, :]), rd=[("w3f", s, k)], wr=[("w3", s, k)])
                    else:
                        S.add("dve", lambda e, k=k: e.tensor_copy(out=w3b[s][:, k, :], in_=w3f[s][:, k, :]), rd=[("w3f", s, k)], wr=[("w3", s, k)])
                for k in range(4):
                    if k % 2 == 0:
                        S.add("dve", lambda e, k=k: e.tensor_copy(out=w2b[s][:, k, :], in_=w2f[s][:, k, :]), rd=[("w2f", s, k)], wr=[("w2", s, k)])
                    else:
                        S.add("act", lambda e, k=k: e.copy(out=w2b[s][:, k, :], in_=w2f[s][:, k, :]), rd=[("w2f", s, k)], wr=[("w2", s, k)])

            load_w(0)
            cast_w(0)
            for ex in range(NE):
                s = ex % 2
                if ex + 1 < NE:
                    load_w(ex + 1)
                for blk in range(nblk):
                    xs = ctr % 2
                    ctr += 1
                    r0 = ex * CAP + blk * 128
                    S.add("sp", lambda e, xs=xs, r0=r0: e.dma_start(out=xd[xs][:], in_=xdisp[r0:r0 + 128, :]), wr=[("xd", xs)], dkey=f"xd{xs}")
                    pi = xs
                    for k in range(8):
                        S.add("pe", lambda e, xs=xs, k=k, pi=pi: e.transpose(out=pTs[pi][:, k * 128:(k + 1) * 128], in_=xd[xs][:, k * 128:(k + 1) * 128],
                                                                             identity=identB[:]), rd=[("xd", xs), "identB"], wr=[("pT", pi)])
                    if xs == 0:
                        S.add("act", lambda e, s=s, blk=blk, pi=pi: e.copy(out=xT[s][:, :, blk * 128:(blk + 1) * 128],
                                                                           in_=pTs[pi][:, :].rearrange("p (k c) -> p k c", c=128)),
                              rd=[("pT", pi)], wr=[("xT", s)])
                    else:
                        S.add("dve", lambda e, s=s, blk=blk, pi=pi: e.tensor_copy(out=xT[s][:, :, blk * 128:(blk + 1) * 128],
                                                                                  in_=pTs[pi][:, :].rearrange("p (k c) -> p k c", c=128)),
                              rd=[("pT", pi)], wr=[("xT", s)])
                for fc in range(4):
                    g = fc % 2
                    for k in range(8):
                        S.add("pe", lambda e, s=s, k=k, fc=fc, g=g: e.matmul(pG[g][:, 0:CAP], w1b[s][:, k, fc * 128:(fc + 1) * 128], xT[s][:, k, :],
                                                                             start=(k == 0), stop=(k == 7)), rd=[("w1", s, k), ("xT", s)], wr=[("pG", g)])
                    for k in range(8):
                        S.add("pe", lambda e, s=s, k=k, fc=fc, g=g: e.matmul(pU[g][:, 0:CAP], w3b[s][:, k, fc * 128:(fc + 1) * 128], xT[s][:, k, :],
                                                                             start=(k == 0), stop=(k == 7)), rd=[("w3", s, k), ("xT", s)], wr=[("pU", g)])
                    S.add("act", lambda e, g=g: e.activation(out=sgl[g][:, 0:CAP], in_=pG[g][:, 0:CAP], func=AF.Silu), rd=[("pG", g)], wr=[("sgl", g)])
                    S.add("dve", lambda e, s=s, fc=fc, g=g: e.tensor_tensor(out=aT[s][:, fc, :], in0=pU[g][:, 0:CAP], in1=sgl[g][:, 0:CAP], op=ALU.mult),
                          rd=[("pU", g), ("sgl", g)], wr=[("aT", s)])
                for blk in range(nblk):
                    ys = (ex * nblk + blk) % 2
                    r0 = ex * CAP + blk * 128
                    for half in range(2):
                        for j in range(4):
                            S.add("pe", lambda e, s=s, j=j, blk=blk, half=half: e.matmul(
                                pY[half][:], aT[s][:, j, blk * 128:(blk + 1) * 128], w2b[s][:, j, half * 512:(half + 1) * 512],
                                start=(j == 0), stop=(j == 3)), rd=[("aT", s), ("w2", s, j)], wr=[("pY", half)])
                        if half == 0:
                            S.add("act", lambda e, ys=ys: e.copy(out=yo[ys][:, 0:512], in_=pY[0][:]), rd=[("pY", 0)], wr=[("yo", ys)])
                        else:
                            S.add("dve", lambda e, ys=ys: e.tensor_copy(out=yo[ys][:, 512:1024], in_=pY[1][:]), rd=[("pY", 1)], wr=[("yo", ys)])
                    S.add("sp", lambda e, ys=ys, r0=r0: e.dma_start(out=ybuf[r0:r0 + 128, :], in_=yo[ys][:]), rd=[("yo", ys)], wr=[("ybuf", ex, blk)],
                          dkey=f"yo{ys}")
                if ex + 1 < NE:
                    cast_w(ex + 1)
            S.emit()

        with ExitStack() as st:
            S = Sched(nc, top, "C")
            A = partial(sb, st=st)
            x1 = [A(f"x1_{i}", [128, D], F32) for i in range(4)]
            g0 = [A(f"g0_{i}", [128, D], F32) for i in range(4)]
            g1 = [A(f"g1_{i}", [128, D], F32) for i in range(4)]
            junk = A("junkC", [128, D], BF16)
            smc = A("smc", [128, 8], F32)
            epsT = A("epsTC", [128, 1], F32)
            S.add("dve", lambda e: e.memset(epsT[:], EPS), wr=["epsT"])
            for tile in range(32):
                s = tile % 4
                r0 = tile * 128
                S.add("sp", lambda e, s=s, r0=r0: e.dma_start(out=x1[s][:], in_=y[r0:r0 + 128, :]), wr=[("x1", s)], dkey=f"x1{s}")
                S.add("pool", lambda e, s=s: e.memset(g0[s][:], 0.0), wr=[("g0", s)])
                S.add("pool", lambda e, s=s: e.memset(g1[s][:], 0.0), wr=[("g1", s)])
                S.add("pool", lambda e, s=s, tile=tile: e.indirect_dma_start(
                    out=g0[s][:, :], out_offset=None, in_=ybuf,
                    in_offset=bass.IndirectOffsetOnAxis(ap=sl[:, 2 * tile:2 * tile + 1], axis=0), bounds_check=bnd(e, "C"), oob_is_err=False),
                    wr=[("g0", s)], dkey=f"g0{s}")
                S.add("pool", lambda e, s=s, tile=tile: e.indirect_dma_start(
                    out=g1[s][:, :], out_offset=None, in_=ybuf,
                    in_offset=bass.IndirectOffsetOnAxis(ap=sl[:, 2 * tile + 1:2 * tile + 2], axis=0), bounds_check=bnd(e, "C"), oob_is_err=False),
                    wr=[("g1", s)], dkey=f"g1{s}")
                S.add("dve", lambda e, s=s, tile=tile: e.scalar_tensor_tensor(out=x1[s][:], in0=g0[s][:], scalar=wts[:, 2 * tile:2 * tile + 1],
                                                                              in1=x1[s][:], op0=ALU.mult, op1=ALU.add),
                      rd=[("g0", s), ("x1", s)], wr=[("x1", s)])
                S.add("dve", lambda e, s=s, tile=tile: e.scalar_tensor_tensor(out=x1[s][:], in0=g1[s][:], scalar=wts[:, 2 * tile + 1:2 * tile + 2],
                                                                              in1=x1[s][:], op0=ALU.mult, op1=ALU.add),
                      rd=[("g1", s), ("x1", s)], wr=[("x1", s)])
                S.add("act", lambda e, s=s: e.activation(out=junk[:], in_=x1[s][:], func=AF.Square, accum_out=smc[:, s:s + 1]),
                      rd=[("x1", s)], wr=["junk", ("smc", s)])
                S.add("act", lambda e, s=s: e.activation(out=smc[:, s:s + 1], in_=smc[:, s:s + 1], func=AF.Sqrt, bias=epsT[:, 0:1], scale=1.0 / D),
                      rd=[("smc", s), "epsT"], wr=[("smc", s)])
                S.add("dve", lambda e, s=s: e.reciprocal(out=smc[:, s:s + 1], in_=smc[:, s:s + 1]), rd=[("smc", s)], wr=[("smc", s)])
                S.add("dve", lambda e, s=s: e.scalar_tensor_tensor(out=x1[s][:], in0=x1[s][:], scalar=smc[:, s:s + 1], in1=gfinB[:],
                                                                   op0=ALU.mult, op1=ALU.mult), rd=[("x1", s), ("smc", s)], wr=[("x1", s)])
                S.add("sp", lambda e, s=s, r0=r0: e.dma_start(out=y[r0:r0 + 128, :], in_=x1[s][:]), rd=[("x1", s)], wr=[("y", tile)], dkey=f"yo{s}")
            S.emit()
    return nc


def _consts():
    cb = np.zeros((128, NCB), np.float32)
    cb[:, CB_IDENT:CB_IDENT + 128] = np.eye(128)
    r = np.arange(128)
    partner = np.where(r % 64 < 32, r + 32, r - 32)
    cb[partner, CB_PERM + r] = 1.0
    cb[:, CB_ONES:CB_ONES + 128] = 1.0
    cb[:, CB_USTR:CB_USTR + 128] = (r[:, None] < r[None, :])
    q = np.arange(512)
    for j in range(4):
        cb[:, CB_MASK + j * 512:CB_MASK + (j + 1) * 512] = (q[None, :] >= j * 128 + r[:, None])
    inv = 1.0 / (10000.0 ** (np.arange(0, 64, 2, dtype=np.float32) / 64.0))
    ang = np.arange(SEQ, dtype=np.float32)[:, None] * inv[None, :].astype(np.float32)
    ang = np.concatenate([ang, ang], axis=-1).astype(np.float32)
    cosT = np.cos(ang).T.astype(np.float32)
    sinT = np.sin(ang).T.astype(np.float32)
    sinS = sinT.copy()
    sinS[:32] *= -1.0
    cb[:, CB_COS:CB_COS + SEQ] = np.concatenate([cosT, cosT], 0)
    cb[:, CB_SIN:CB_SIN + SEQ] = np.concatenate([sinS, sinS], 0)
    sel = np.zeros((8, 1024), np.float32)
    for j in range(8):
        sel[j, j * 128:(j + 1) * 128] = 1.0
    return cb, sel


_STAGE = 99


def _prep(x, g_mix, w_in, w_branch_moba, w_branch_diff, w_out,
           diff_lambda_q1, diff_lambda_k1, diff_lambda_q2, diff_lambda_k2, diff_subln_g,
           g_ffn, w_group, b_group, w_router, b_router,
           w_expert_gate, w_expert_up, w_expert_down, g_final):
    f = lambda a: np.ascontiguousarray(np.asarray(a, dtype=np.float32))
    x = f(x)
    cb, sel = _consts()
    cf = np.zeros((128, NCF), np.float32)
    cf[:, CF_IDENT:CF_IDENT + 128] = np.eye(128)
    cf[:, CF_EBASE:CF_EBASE + 32] = (np.arange(32) * CAP)[None, :]
    cf[:, CF_GMIXT:CF_GMIXT + 8] = f(g_mix)[0].reshape(8, 128).T
    cf[:, CF_GFFNT:CF_GFFNT + 8] = f(g_ffn)[0].reshape(8, 128).T
    cf[:, CF_GSUB] = f(diff_subln_g)[0]
    cf[:, CF_LAM:CF_LAM + 256] = np.concatenate([f(diff_lambda_q1)[0], f(diff_lambda_k1)[0], f(diff_lambda_q2)[0],
                                                 f(diff_lambda_k2)[0]])[None, :]
    shared = {
        "w_in": f(w_in)[0], "w_bm": f(w_branch_moba)[0], "w_bd": f(w_branch_diff)[0], "w_out": f(w_out)[0],
        "w1": f(w_expert_gate)[0], "w3": f(w_expert_up)[0], "w2": f(w_expert_down)[0],
        "wr": np.ascontiguousarray(np.concatenate([f(w_group)[0], f(w_router)[0]], axis=1)),
        "brow": np.ascontiguousarray(np.concatenate([f(b_group)[0], f(b_router)[0]])[None, :]),
        "gffnB": np.ascontiguousarray(np.broadcast_to(f(g_ffn)[0][None, :], (128, D))),
        "gfinB": np.ascontiguousarray(np.broadcast_to(f(g_final)[None, :], (128, D))),
        "cf": cf, "cb": cb, "sel": sel,
    }
    xs = x.reshape(NCORES, TOK, D)
    return shared, xs


def kernel(**inputs):
    shared, xs = _prep(**inputs)
    nc = build_program(_STAGE)
    in_maps = [dict(shared, x=np.ascontiguousarray(xs[c])) for c in range(NCORES)]
    res = run_bass_kernel_spmd(nc, in_maps, core_ids=list(range(NCORES)))
    out = np.stack([np.asarray(r["y"]) for r in res.results], axis=0)
    return out.reshape(16, SEQ, D).astype(np.float32)
```

```python
import math
from contextlib import ExitStack
from functools import partial

import numpy as np
import concourse.bass as bass
import concourse.mybir as mybir
from concourse.bass_utils import run_bass_kernel_spmd

F32 = mybir.dt.float32
BF16 = mybir.dt.bfloat16
I32 = mybir.dt.int32
AF = mybir.ActivationFunctionType
ALU = mybir.AluOpType
AX = mybir.AxisListType

NCORES = 8
SEQ = 2048
D = 1024
TOK = 2 * SEQ
CAP = 512
NE = 32
EPS = 1e-6
NB = 2


class Sched:
    def __init__(self, nc, stack, tag):
        self.nc, self.stack, self.tag = nc, stack, tag
        self.ops, self.lastw, self.readers, self.dsem = [], {}, {}, {}
        self.grpmax, self.gctr = {}, 0
        self.esem = {e: stack.enter_context(nc.semaphore(f"{tag}_{e}")) for e in ("pe", "act", "dve", "pool")}

    EXCL = ("ps", "pG", "pU", "pY", "pT")

    def newgrp(self):
        self.gctr += 1
        return self.gctr

    def add(self, eng, fn, rd=(), wr=(), dkey=None, grp=None):
        ex = [r for r in rd if (r[0] if isinstance(r, tuple) else r) in self.EXCL]
        if ex:
            rd = [r for r in rd if r not in ex]
            wr = list(wr) + [r for r in ex if r not in wr]
        deps = set()
        for r in rd:
            w = self.lastw.get(r)
            if w is not None:
                deps.add(w)
        for r in wr:
            w = self.lastw.get(r)
            if w is not None:
                deps.add(w)
            deps.update(self.readers.get(r, ()))
        i = len(self.ops)
        op = dict(eng=eng, fn=fn, deps=deps, dkey=dkey, inc=False, seq=0)
        if dkey is not None:
            if dkey not in self.dsem:
                self.dsem[dkey] = [self.stack.enter_context(self.nc.semaphore(f"{self.tag}_d_{dkey}")), 0]
            self.dsem[dkey][1] += 16
            op["dval"] = self.dsem[dkey][1]
            op["grp"] = grp
            if grp is not None:
                self.grpmax[(dkey, grp)] = op["dval"]
        self.ops.append(op)
        for r in rd:
            self.readers.setdefault(r, []).append(i)
        for r in wr:
            self.lastw[r] = i
            self.readers[r] = []
        return i

    def finalize(self):
        for op in self.ops:
            for d in op["deps"]:
                Dd = self.ops[d]
                if Dd["dkey"] is None:
                    Dd["inc"] = True
        cnt = {e: 0 for e in self.esem}
        for op in self.ops:
            if op["dkey"] is None and op["inc"]:
                cnt[op["eng"]] += 1
                op["seq"] = cnt[op["eng"]]

    def run(self, eng, e):
        waited = {}
        for op in self.ops:
            if op["eng"] != eng:
                continue
            need = {}
            for d in op["deps"]:
                Dd = self.ops[d]
                if Dd["dkey"] is not None:
                    key, sem, val = "d" + Dd["dkey"], self.dsem[Dd["dkey"]][0], Dd["dval"]
                    if Dd["grp"] is not None:
                        val = self.grpmax[(Dd["dkey"], Dd["grp"])]
                else:
                    if Dd["eng"] == "pe" and eng == "pe" and op["dkey"] is None:
                        continue
                    key, sem, val = Dd["eng"], self.esem[Dd["eng"]], Dd["seq"]
                if key not in need or need[key][1] < val:
                    need[key] = (sem, val)
            for key in sorted(need):
                sem, val = need[key]
                if waited.get(key, 0) >= val:
                    continue
                e.wait_ge(sem, val)
                waited[key] = val
            ins = op["fn"](e)
            if op["dkey"] is not None:
                ins.then_inc(self.dsem[op["dkey"]][0], 16)
            elif op["inc"]:
                ins.then_inc(self.esem[op["eng"]], 1)
        if eng == "sp":
            for k, (sem, val) in self.dsem.items():
                e.wait_ge(sem, val)

    def emit(self):
        self.finalize()
        with self.nc.Block() as block:
            @block.tensor
            def _(e):
                self.run("pe", e)

            @block.scalar
            def _(e):
                self.run("act", e)

            @block.vector
            def _(e):
                self.run("dve", e)

            @block.gpsimd
            def _(e):
                self.run("pool", e)

            @block.sync
            def _(e):
                self.run("sp", e)


CB_IDENT, CB_PERM, CB_ONES, CB_USTR, CB_MASK, CB_COS, CB_SIN = 0, 128, 256, 384, 512, 2560, 4608
NCB = 6656
CF_IDENT, CF_EBASE, CF_GMIXT, CF_GFFNT, CF_GSUB, CF_LAM = 0, 128, 160, 168, 176, 177
NCF = 177 + 256


def build_program(stage=99):
    nc = bass.Bass("TRN2", target_bir_lowering=False)
    bndreg = {}

    def bnd(e, tag):
        if tag not in bndreg:
            r = e.alloc_register("bnd" + tag)
            e.reg_mov(r, NE * CAP - 1)
            bndreg[tag] = r
        return bndreg[tag]

    def din(name, shape, dtype=F32):
        return nc.dram_tensor(name, shape, dtype, kind="ExternalInput").ap()

    x = din("x", [TOK, D])
    w_in = din("w_in", [D, 5120])
    w_bm = din("w_bm", [512, D])
    w_bd = din("w_bd", [512, D])
    w_out = din("w_out", [D, D])
    w1 = din("w1", [NE, D, 512])
    w3 = din("w3", [NE, D, 512])
    w2 = din("w2", [NE, 512, D])
    wr_d = din("wr", [D, 36])
    brow_d = din("brow", [1, 36])
    gffnB_d = din("gffnB", [128, D])
    gfinB_d = din("gfinB", [128, D])
    cf_d = din("cf", [128, NCF])
    cb_d = din("cb", [128, NCB])
    sel_d = din("sel", [8, 1024])
    y = nc.dram_tensor("y", [TOK, D], F32, kind="ExternalOutput").ap()
    ybuf = nc.dram_tensor("ybuf", [NE * CAP, D], F32, kind="ExternalOutput").ap()
    xdisp = nc.dram_tensor("xdisp", [NE * CAP, D], BF16, kind="Internal").ap()

    w_in_k = w_in.rearrange("(k p) c -> p k c", p=128)
    w_bm_k = w_bm.rearrange("(k p) c -> p k c", p=128)
    w_bd_k = w_bd.rearrange("(k p) c -> p k c", p=128)
    w_out_k = w_out.rearrange("(k p) c -> p k c", p=128)
    wr_k = wr_d.rearrange("(k p) c -> p k c", p=128)

    with ExitStack() as top:
        def sb(name, shape, dtype, st=top):
            return st.enter_context(nc.sbuf_tensor(name, shape, dtype))

        sl = sb("sl", [128, 64], I32)
        wts = sb("wts", [128, 64], F32)
        gfinB = sb("gfinB_sb", [128, D], F32)

        with ExitStack() as st:
            S = Sched(nc, top, "A")
            A = partial(sb, st=st)
            ps = [st.enter_context(nc.psum_tensor(f"ps{i}", [128, 512], F32)) for i in range(8)]
            cf = A("cf_sb", [128, NCF], F32)
            cb = A("cb_sb", [128, NCB], BF16)
            sel = A("sel_sb", [8, 1024], BF16)
            gffnB = A("gffnB_sb", [128, D], F32)
            wr = A("wr_sb", [128, 8, 36], F32)
            brow = A("brow_sb", [1, 36], F32)
            onesF = A("onesF", [128, 128], F32)
            epsT = A("epsT", [128, 1], F32)
            hT = A("hT", [128, 8 * SEQ], BF16)
            qkv = A("qkv", [128, 8 * SEQ], BF16)
            oa = A("oa", [128, 4 * SEQ], BF16)
            od = A("od", [128, 4 * SEQ], BF16)
            xt = [A(f"xt{i}", [128, D], F32) for i in range(2)]
            junk = A("junk", [128, D], BF16)
            wq = [A(f"wq{i}", [128, 8, 128], BF16) for i in range(3)]
            wb = [A(f"wb{i}", [128, 4, 128], BF16) for i in range(2)]
            TF = [A(f"tf{i}", [128, 512], F32) for i in range(8)]
            TB = [A(f"tb{i}", [128, 512], BF16) for i in range(8)]
            h2 = [A(f"h2_{i}", [128, D], BF16) for i in range(2)]
            h2T = A("h2T", [128, 8, 128], F32)
            biasT = [A(f"biasT{i}", [8, 1024], BF16) for i in range(2)]
            ksf = A("ksf", [128, 8], F32)
            ksum = [A(f"ksum{i}", [128, 8], BF16) for i in range(2)]
            gsb = [A(f"gsb{i}", [128, 8], F32) for i in range(4)]
            sm = A("sm", [128, 640], F32)
            carry = A("carry", [128, 32], F32)
            neglam = A("neglam", [128, 1], F32)
            gsub8 = A("gsub8", [128, 1], F32)

            identF = cf[:, CF_IDENT:CF_IDENT + 128]
            ebase = cf[:, CF_EBASE:CF_EBASE + 32]
            identB = cb[:, CB_IDENT:CB_IDENT + 128]
            perm = cb[:, CB_PERM:CB_PERM + 128]
            onesB = cb[:, CB_ONES:CB_ONES + 128]
            ustr = cb[:, CB_USTR:CB_USTR + 128]

            def maskj(j, n):
                return cb[:, CB_MASK + j * 512:CB_MASK + j * 512 + n]

            S.add("sp", lambda e: e.dma_start(out=cf[:], in_=cf_d), wr=["cf"], dkey="cf")
            gcb = S.newgrp()
            for i in range(0, NCB, 1664):
                S.add("pool", lambda e, i=i: e.dma_start(out=cb[:, i:i + 1664], in_=cb_d[:, i:i + 1664]),
                      wr=[("cbp", i)], dkey="cb", grp=gcb)
            S.add("dve", lambda e: e.memset(sm[:, 510:511], 0.0), rd=[("cbp", i) for i in range(0, NCB, 1664)], wr=["cb"])
            S.add("pool", lambda e: e.dma_start(out=sel[:], in_=sel_d), wr=["sel"], dkey="sel")
            S.add("sp", lambda e: e.dma_start(out=gffnB[:], in_=gffnB_d), wr=["gffnB"], dkey="gffnB")
            S.add("sp", lambda e: e.dma_start(out=gfinB[:], in_=gfinB_d), wr=["gfinB"], dkey="gfinB")
            S.add("sp", lambda e: e.dma_start(out=wr[:], in_=wr_k), wr=["wr"], dkey="wr")
            S.add("sp", lambda e: e.dma_start(out=brow[:], in_=brow_d), wr=["brow"], dkey="brow")
            zt = A("zt", [128, D], BF16)
            S.add("dve", lambda e: e.memset(zt[:], 0.0), wr=["zt"])
            S.add("dve", lambda e: e.memset(onesF[:], 1.0), wr=["onesF"])
            S.add("dve", lambda e: e.memset(epsT[:], EPS), wr=["epsT"])
            S.add("dve", lambda e: e.memset(carry[:], 0.0), wr=["carry"])
            for i in range(4):
                S.add("dve", lambda e, i=i: e.memset(gsb[i][:], -1e30), wr=[f"gsb{i}"])
            lam = cf[:, CF_LAM:CF_LAM + 256]
            S.add("dve", lambda e: e.tensor_tensor(out=sm[:, 512:576], in0=lam[:, 0:64], in1=lam[:, 64:128], op=ALU.mult),
                  rd=["cf"], wr=["lamsc"])
            S.add("dve", lambda e: e.reduce_sum(out=sm[:, 500:501], in_=sm[:, 512:576], axis=AX.X), rd=["lamsc"], wr=["lamsc"])
            S.add("dve", lambda e: e.tensor_tensor(out=sm[:, 576:640], in0=lam[:, 128:192], in1=lam[:, 192:256], op=ALU.mult),
                  rd=["cf", "lamsc"], wr=["lamsc"])
            S.add("dve", lambda e: e.reduce_sum(out=sm[:, 501:502], in_=sm[:, 576:640], axis=AX.X), rd=["lamsc"], wr=["lamsc"])
            S.add("act", lambda e: e.activation(out=sm[:, 502:504], in_=sm[:, 500:502], func=AF.Exp), rd=["lamsc"], wr=["lamsc"])
            S.add("dve", lambda e: e.tensor_tensor(out=sm[:, 504:505], in0=sm[:, 503:504], in1=sm[:, 502:503], op=ALU.subtract),
                  rd=["lamsc"], wr=["lamsc"])
            S.add("dve", lambda e: e.tensor_scalar(out=neglam[:], in0=sm[:, 504:505], scalar1=-0.2, scalar2=None, op0=ALU.add),
                  rd=["lamsc"], wr=["neglam"])
            S.add("dve", lambda e: e.tensor_scalar(out=gsub8[:], in0=cf[:, CF_GSUB:CF_GSUB + 1], scalar1=0.8, scalar2=None,
                                                   op0=ALU.mult), rd=["cf"], wr=["gsub8"])

            cnt = {"wq": 0, "tf": 0, "tb": 0, "psA": 0}

            def rr(name, n):
                v = cnt[name] % n
                cnt[name] += 1
                return v

            def rmsnorm_rs(src, srcres, col):
                S.add("act", lambda e: e.activation(out=junk[:], in_=src[:], func=AF.Square, accum_out=sm[:, col:col + 1]),
                      rd=[srcres], wr=["junk", ("sm", col)])
                S.add("act", lambda e: e.activation(out=sm[:, col:col + 1], in_=sm[:, col:col + 1], func=AF.Sqrt,
                                                    bias=epsT[:, 0:1], scale=1.0 / D), rd=[("sm", col), "epsT"], wr=[("sm", col)])
                S.add("dve", lambda e: e.reciprocal(out=sm[:, col:col + 1], in_=sm[:, col:col + 1]),
                      rd=[("sm", col)], wr=[("sm", col)])

            def transposes_f32(src, srcres, dst_fn, dstres, gT_off):
                for half in range(2):
                    pb = 2 + rr("psA", 2)
                    for k4 in range(4):
                        k = half * 4 + k4
                        S.add("pe", lambda e, pb=pb, k=k, k4=k4: e.transpose(out=ps[pb][:, k4 * 128:(k4 + 1) * 128],
                                                                             in_=src[:, k * 128:(k + 1) * 128], identity=identF),
                              rd=[srcres, "cf"], wr=[("ps", pb)])
                    for k4 in range(4):
                        k = half * 4 + k4
                        eng = "act" if half == 0 else "dve"
                        if eng == "act":
                            S.add("act", lambda e, pb=pb, k=k, k4=k4: e.activation(
                                out=dst_fn(k), in_=ps[pb][:, k4 * 128:(k4 + 1) * 128], func=AF.Copy,
                                scale=cf[:, gT_off + k:gT_off + k + 1]), rd=[("ps", pb), "cf"], wr=[dstres(k)])
                        else:
                            S.add("dve", lambda e, pb=pb, k=k, k4=k4: e.tensor_scalar(
                                out=dst_fn(k), in0=ps[pb][:, k4 * 128:(k4 + 1) * 128],
                                scalar1=cf[:, gT_off + k:gT_off + k + 1], scalar2=None, op0=ALU.mult),
                                rd=[("ps", pb), "cf"], wr=[dstres(k)])

            def load_wq(c0):
                s = rr("wq", 3)
                g_ = S.newgrp()
                for k in range(8):
                    S.add("pool", lambda e, k=k: e.dma_start(out=wq[s][:, k, :], in_=w_in[k * 128:(k + 1) * 128, c0:c0 + 128]),
                          wr=[("wq", s, k)], dkey=f"wq{s}", grp=g_)
                return s

            def proj_fm(wslot, tc, pb, nk=8, wt=None, src=None, srcres=None):
                for k in range(nk):
                    if wt is None:
                        S.add("pe", lambda e, k=k: e.matmul(ps[pb][:], wq[wslot][:, k, :], hT[:, k * SEQ + tc * 512:k * SEQ + tc * 512 + 512],
                                                            start=(k == 0), stop=(k == nk - 1)),
                              rd=[("wq", wslot, k), ("hT", tc)], wr=[("ps", pb)])
                    else:
                        S.add("pe", lambda e, k=k: e.matmul(ps[pb][:], wt[:, k, :], src[:, k * SEQ + tc * 512:k * SEQ + tc * 512 + 512],
                                                            start=(k == 0), stop=(k == nk - 1)),
                              rd=[srcres[0], (srcres[1], tc)], wr=[("ps", pb)])

            def rope_to(pb, tc, dst, dstres):
                import os
                ROPE = int(os.environ.get("ROPE", "9"))
                if ROPE == 0:
                    S.add("act", lambda e: e.copy(out=dst, in_=ps[pb][:]), rd=[("ps", pb)], wr=[dstres])
                    return
                tbi = rr("tb", 8)
                t1, t2 = rr("tf", 8), rr("tf", 8)
                pr = rr("psA", 2)
                if ROPE == 3:
                    pr += 4
                S.add("act", lambda e: e.copy(out=TB[tbi][:], in_=ps[pb][:]), rd=[("ps", pb)], wr=[("tb", tbi)])
                if ROPE == 4:
                    pr = pb
                else:
                    S.add("pe", lambda e: e.matmul(ps[pr][:], perm, TB[tbi][:], start=True, stop=True),
                          rd=[("tb", tbi), "cb"], wr=[("ps", pr)])
                if ROPE in (2, 3, 4):
                    S.add("dve", lambda e: e.tensor_copy(out=TF[t1][:], in_=ps[pb][:]), rd=[("ps", pb), "cb"], wr=[("tf", t1)])
                    S.add("dve", lambda e: e.tensor_copy(out=TF[t2][:], in_=ps[pr][:]), rd=[("ps", pr), "cb"], wr=[("tf", t2)])
                else:
                    S.add("dve", lambda e: e.tensor_tensor(out=TF[t1][:], in0=ps[pb][:], in1=cb[:, CB_COS + tc * 512:CB_COS + tc * 512 + 512],
                                                           op=ALU.mult), rd=[("ps", pb), "cb"], wr=[("tf", t1)])
                    S.add("dve", lambda e: e.tensor_tensor(out=TF[t2][:], in0=ps[pr][:], in1=cb[:, CB_SIN + tc * 512:CB_SIN + tc * 512 + 512],
                                                           op=ALU.mult), rd=[("ps", pr), "cb"], wr=[("tf", t2)])
                if ROPE in (1, 2, 3, 4):
                    S.add("dve", lambda e: e.tensor_tensor(out=dst, in0=TF[t1][:], in1=TF[t2][:], op=ALU.add),
                          rd=[("tf", t1), ("tf", t2)], wr=[dstres])
                    return
                S.add("pool", lambda e: e.tensor_tensor(out=dst, in0=TF[t1][:], in1=TF[t2][:], op=ALU.add),
                      rd=[("tf", t1), ("tf", t2)], wr=[dstres])

            ALLQ = [("Q", s_, t_) for s_ in range(2) for t_ in range(4)] + [("K", s_, t_) for s_ in range(2) for t_ in range(4)] \
                + [("V", t_) for t_ in range(16)]
            OAALL = [("oa", t_) for t_ in range(4)]

            def stage1(b):
                for tt in range(16):
                    xs = tt % 2
                    r0 = b * SEQ + tt * 128
                    S.add("sp", lambda e, xs=xs, r0=r0: e.dma_start(out=xt[xs][:], in_=x[r0:r0 + 128, :]),
                          wr=[("xt", xs)], dkey=f"xt{xs}")
                    rmsnorm_rs(xt[xs], ("xt", xs), xs)
                    S.add("dve", lambda e, xs=xs: e.tensor_scalar(out=xt[xs][:], in0=xt[xs][:], scalar1=sm[:, xs:xs + 1], scalar2=None,
                                                                  op0=ALU.mult), rd=[("xt", xs), ("sm", xs)], wr=[("xt", xs)])
                    transposes_f32(xt[xs], ("xt", xs),
                                   lambda k, tt=tt: hT[:, k * SEQ + tt * 128:k * SEQ + tt * 128 + 128],
                                   lambda k, tt=tt: ("hT", tt // 4), CF_GMIXT)

            def qk_proj(cq, ck, slot):
                for (c0, base, nm) in ((cq, slot * SEQ, "Q"), (ck, 2 * SEQ + slot * SEQ, "K")):
                    ws = load_wq(c0)
                    for tc in range(4):
                        pb = 2 + rr("psA", 2)
                        proj_fm(ws, tc, pb)
                        rope_to(pb, tc, qkv[:, base + tc * 512:base + tc * 512 + 512], (nm, slot, tc))

            def v_proj(c0):
                g_ = S.newgrp()
                for k in range(8):
                    S.add("pool", lambda e, k=k: e.dma_start(out=od[:, k * 512:(k + 1) * 512], in_=w_in[k * 128:(k + 1) * 128, c0:c0 + 512]),
                          wr=[("od", k)], dkey="wv", grp=g_)
                for tt in range(16):
                    pb = 2 + rr("psA", 2)
                    for k in range(8):
                        S.add("pe", lambda e, k=k, tt=tt, pb=pb: e.matmul(ps[pb][:], hT[:, k * SEQ + tt * 128:k * SEQ + tt * 128 + 128],
                                                                          od[:, k * 512:(k + 1) * 512], start=(k == 0), stop=(k == 7)),
                              rd=[("od", k), ("hT", tt // 4)], wr=[("ps", pb)])
                    if tt % 2 == 0:
                        S.add("act", lambda e, tt=tt, pb=pb: e.copy(out=qkv[:, 4 * SEQ + tt * 512:4 * SEQ + tt * 512 + 512], in_=ps[pb][:]),
                              rd=[("ps", pb)], wr=[("V", tt)])
                    else:
                        S.add("dve", lambda e, tt=tt, pb=pb: e.tensor_copy(out=qkv[:, 4 * SEQ + tt * 512:4 * SEQ + tt * 512 + 512], in_=ps[pb][:]),
                              rd=[("ps", pb)], wr=[("V", tt)])

            def moba_ksum(slot):
                KT0 = 2 * SEQ + slot * SEQ
                S.add("dve", lambda e: e.reduce_sum(out=ksf[:], in_=qkv[:, KT0:KT0 + SEQ].rearrange("p (j t) -> p j t", t=256),
                                                    axis=AX.X), rd=[("K", slot, t) for t in range(4)], wr=["ksf"])
                S.add("dve", lambda e: e.tensor_copy(out=ksum[slot][:], in_=ksf[:]), rd=["ksf"], wr=[("ksum", slot)])

            def moba_head(p, hh, slot, mode="attn", inter=None):
                h = 2 * p + hh
                bp = hh * 64
                bs = h % 2
                QT0, KT0 = slot * SEQ, 2 * SEQ + slot * SEQ
                def gate_step(qt):
                    nb = qt // 2
                    gi = nb - 4
                    pg = 2 + rr("psA", 2)
                    S.add("pe", lambda e, qt=qt, pg=pg: e.matmul(ps[pg][:, 0:8], qkv[bp:bp + 64, QT0 + qt * 128:QT0 + qt * 128 + 128],
                                                                 ksum[slot][bp:bp + 64, 0:8], start=True, stop=True),
                          rd=[("Q", slot, qt // 4), ("ksum", slot)], wr=[("ps", pg)])
                    S.add("dve", lambda e, pg=pg, gi=gi, nb=nb: e.tensor_copy(out=gsb[gi][:, 0:nb], in_=ps[pg][:, 0:nb]),
                          rd=[("ps", pg)], wr=[f"gsb{gi}"])
                    S.add("dve", lambda e, gi=gi: e.max(out=sm[:, 16:24], in_=gsb[gi][:, 0:8]), rd=[f"gsb{gi}"], wr=["m8"])
                    S.add("dve", lambda e, gi=gi: e.tensor_scalar(out=sm[:, 24:32], in0=gsb[gi][:, 0:8], scalar1=sm[:, 18:19],
                                                                  scalar2=30000.0, op0=ALU.is_ge, op1=ALU.mult),
                          rd=[f"gsb{gi}", "m8"], wr=["bq"])
                    pt = 2 + rr("psA", 2)
                    S.add("pe", lambda e, pt=pt: e.transpose(out=ps[pt][0:8, 0:128], in_=sm[:, 24:32], identity=identF),
                          rd=["bq", "cf"], wr=[("ps", pt)])
                    S.add("dve", lambda e, pt=pt, qt=qt: e.tensor_scalar(
                        out=biasT[bs][0:8, (qt - 8) * 128:(qt - 8) * 128 + 128], in0=ps[pt][0:8, 0:128],
                        scalar1=-30000.0, scalar2=None, op0=ALU.add), rd=[("ps", pt)], wr=[("biasT", bs, (qt - 8) // 2)])
                if mode == "gate":
                    return [partial(gate_step, qt) for qt in range(8, 16)]
                for qb in range(8):
                    if inter:
                        inter.pop(0)()
                    po, pl = 4 + (qb % 2) * 2, 5 + (qb % 2) * 2
                    nkt = 2 * qb + 2
                    def pv_ops(kt, tbi, po=po, pl=pl, nkt=nkt):
                        S.add("pe", lambda e: e.matmul(
                            ps[po][0:64, 0:256], qkv[:, 4 * SEQ + kt * 512 + h * 64:4 * SEQ + kt * 512 + h * 64 + 64], TB[tbi][:, 0:256],
                            start=(kt == 0), stop=(kt == nkt - 1)), rd=[("V", kt), ("tb", tbi)], wr=[("ps", po)])
                        S.add("pe", lambda e: e.matmul(
                            ps[pl][0:64, 0:256], onesB[:, 0:64], TB[tbi][:, 0:256],
                            start=(kt == 0), stop=(kt == nkt - 1)), rd=["cb", ("tb", tbi)], wr=[("ps", pl)])
                    pend = None
                    for kt in range(nkt):
                        pS = kt % 2
                        own = (kt // 2 == qb)
                        need_bias = (not own) and qb >= 4
                        S.add("pe", lambda e, kt=kt, pS=pS, qb=qb, nbias=need_bias: e.matmul(
                            ps[pS][:, 0:256], qkv[bp:bp + 64, KT0 + kt * 128:KT0 + kt * 128 + 128],
                            qkv[bp:bp + 64, QT0 + qb * 256:QT0 + qb * 256 + 256], start=True, stop=(not nbias)),
                            rd=[("K", slot, kt // 4), ("Q", slot, qb // 2)], wr=[("ps", pS)])
                        if need_bias:
                            j = kt // 2
                            S.add("pe", lambda e, pS=pS, j=j, qb=qb: e.matmul(
                                ps[pS][:, 0:256], sel[0:8, j * 128:(j + 1) * 128], biasT[bs][0:8, (qb - 4) * 256:(qb - 4) * 256 + 256],
                                start=False, stop=True), rd=["sel", ("biasT", bs, qb - 4)], wr=[("ps", pS)])
                        tbi = rr("tb", 8)
                        S.add("act", lambda e, pS=pS, tbi=tbi: e.activation(out=TB[tbi][:, 0:256], in_=ps[pS][:, 0:256], func=AF.Exp,
                                                                            scale=0.125), rd=[("ps", pS)], wr=[("tb", tbi)])
                        if own:
                            kto = kt - 2 * qb
                            S.add("pool", lambda e, tbi=tbi, kto=kto: e.tensor_tensor(out=TB[tbi][:, 0:256], in0=TB[tbi][:, 0:256],
                                                                                     in1=maskj(kto, 256), op=ALU.mult),
                                  rd=[("tb", tbi), "cb"], wr=[("tb", tbi)])
                        if pend is not None:
                            pv_ops(*pend)
                        pend = (kt, tbi)
                    pv_ops(*pend)
                    t1 = rr("tf", 8)
                    S.add("dve", lambda e, t1=t1, pl=pl: e.reciprocal(out=TF[t1][0:64, 0:256], in_=ps[pl][0:64, 0:256]),
                          rd=[("ps", pl)], wr=[("tf", t1)])
                    S.add("dve", lambda e, t1=t1, po=po, qb=qb: e.tensor_tensor(
                        out=oa[bp:bp + 64, p * SEQ + qb * 256:p * SEQ + qb * 256 + 256], in0=ps[po][0:64, 0:256], in1=TF[t1][0:64, 0:256],
                        op=ALU.mult), rd=[("ps", po), ("tf", t1)], wr=[("oa", qb // 2), "wout_all"] + [("wout", k_) for k_ in range(8)])

            def diff_head(h, slot):
                QT0, KT0 = slot * SEQ, 2 * SEQ + slot * SEQ
                for qc in range(4):
                    nkt = 4 * qc + 4
                    def pv_ops(kt, tbis, nkt=nkt):
                        for m in range(2):
                            tbi = tbis[m]
                            S.add("pe", lambda e, tbi=tbi, m=m: e.matmul(
                                ps[4 + m][:], qkv[:, 4 * SEQ + kt * 512 + h * 128:4 * SEQ + kt * 512 + h * 128 + 128], TB[tbi][:],
                                start=(kt == 0), stop=(kt == nkt - 1)), rd=[("V", kt), ("tb", tbi)], wr=[("ps", 4 + m)])
                            S.add("pe", lambda e, tbi=tbi, m=m: e.matmul(
                                ps[6 + m][:], onesB, TB[tbi][:], start=(kt == 0), stop=(kt == nkt - 1)),
                                rd=["cb", ("tb", tbi)], wr=[("ps", 6 + m)])
                    pend = None
                    for kt in range(nkt):
                        tbis = []
                        for m in range(2):
                            bp = m * 64
                            pS = m
                            S.add("pe", lambda e, kt=kt, pS=pS, bp=bp, qc=qc: e.matmul(
                                ps[pS][:], qkv[bp:bp + 64, KT0 + kt * 128:KT0 + kt * 128 + 128],
                                qkv[bp:bp + 64, QT0 + qc * 512:QT0 + qc * 512 + 512], start=True, stop=True),
                                rd=[("K", slot, kt // 4), ("Q", slot, qc)], wr=[("ps", pS)])
                            tbi = rr("tb", 8)
                            tbis.append(tbi)
                            S.add("act", lambda e, pS=pS, tbi=tbi: e.activation(out=TB[tbi][:], in_=ps[pS][:], func=AF.Exp, scale=0.125),
                                  rd=[("ps", pS)], wr=[("tb", tbi)])
                            if kt >= 4 * qc:
                                j = kt - 4 * qc
                                eng = "pool" if m == 0 else "dve"
                                S.add(eng, lambda e, tbi=tbi, j=j: e.tensor_tensor(out=TB[tbi][:], in0=TB[tbi][:], in1=maskj(j, 512),
                                                                                  op=ALU.mult), rd=[("tb", tbi), "cb"], wr=[("tb", tbi)])
                        if pend is not None:
                            pv_ops(*pend)
                        pend = (kt, tuple(tbis))
                    pv_ops(*pend)
                    r1, r2, u1, u2 = rr("tf", 8), rr("tf", 8), rr("tf", 8), rr("tf", 8)
                    S.add("dve", lambda e, r1=r1: e.reciprocal(out=TF[r1][:], in_=ps[6][:]), rd=[("ps", 6)], wr=[("tf", r1)])
                    S.add("dve", lambda e, r2=r2: e.reciprocal(out=TF[r2][:], in_=ps[7][:]), rd=[("ps", 7)], wr=[("tf", r2)])
                    S.add("dve", lambda e, r1=r1, u1=u1: e.tensor_tensor(out=TF[u1][:], in0=ps[4][:], in1=TF[r1][:], op=ALU.mult),
                          rd=[("ps", 4), ("tf", r1)], wr=[("tf", u1)])
                    S.add("dve", lambda e, r2=r2, u2=u2: e.tensor_tensor(out=TF[u2][:], in0=ps[5][:], in1=TF[r2][:], op=ALU.mult),
                          rd=[("ps", 5), ("tf", r2)], wr=[("tf", u2)])
                    S.add("dve", lambda e, r1=r1, u1=u1, u2=u2: e.scalar_tensor_tensor(
                        out=TF[r1][:], in0=TF[u2][:], scalar=neglam[:, 0:1], in1=TF[u1][:], op0=ALU.mult, op1=ALU.add),
                        rd=[("tf", u1), ("tf", u2), "neglam"], wr=[("tf", r1)])
                    S.add("pool", lambda e, r1=r1, r2=r2: e.tensor_tensor(out=TF[r2][:], in0=TF[r1][:], in1=TF[r1][:], op=ALU.mult),
                          rd=[("tf", r1)], wr=[("tf", r2)])
                    pn = 2 + rr("psA", 2)
                    S.add("pe", lambda e, r2=r2, pn=pn: e.matmul(ps[pn][:], onesF[:], TF[r2][:], start=True, stop=True),
                          rd=[("tf", r2), "onesF"], wr=[("ps", pn)])
                    S.add("act", lambda e, u1=u1, pn=pn: e.activation(out=TF[u1][:], in_=ps[pn][:], func=AF.Sqrt, bias=epsT[:, 0:1],
                                                                      scale=1.0 / 128), rd=[("ps", pn), "epsT"], wr=[("tf", u1)])
                    S.add("dve", lambda e, u1=u1: e.reciprocal(out=TF[u1][:], in_=TF[u1][:]), rd=[("tf", u1)], wr=[("tf", u1)])
                    S.add("dve", lambda e, u1=u1, r1=r1: e.tensor_tensor(out=TF[r1][:], in0=TF[r1][:], in1=TF[u1][:], op=ALU.mult),
                          rd=[("tf", u1), ("tf", r1)], wr=[("tf", r1)])
                    S.add("act", lambda e, r1=r1, qc=qc: e.activation(out=od[:, h * SEQ + qc * 512:h * SEQ + qc * 512 + 512], in_=TF[r1][:],
                                                                      func=AF.Copy, scale=gsub8[:, 0:1]),
                          rd=[("tf", r1), "gsub8"], wr=[("od", h * 4 + qc)])

            def merge_oc(oc):
                g0s = load_wq(3072 + oc * 128)
                g1s = load_wq(4096 + oc * 128)
                g_ = S.newgrp()
                for k in range(4):
                    S.add("pool", lambda e, k=k: e.dma_start(out=wb[0][:, k, :], in_=w_bm[k * 128:(k + 1) * 128, oc * 128:(oc + 1) * 128]),
                          wr=[("wb", 0, k)], dkey="wb0", grp=g_)
                    S.add("pool", lambda e, k=k: e.dma_start(out=wb[1][:, k, :], in_=w_bd[k * 128:(k + 1) * 128, oc * 128:(oc + 1) * 128]),
                          wr=[("wb", 1, k)], dkey="wb1", grp=g_)
                for tc in range(4):
                    sg = []
                    for gi, gs in enumerate((g0s, g1s)):
                        pb = gi
                        proj_fm(gs, tc, pb)
                        tbi = rr("tb", 8)
                        S.add("act", lambda e, pb=pb, tbi=tbi: e.activation(out=TB[tbi][:], in_=ps[pb][:], func=AF.Sigmoid),
                              rd=[("ps", pb)], wr=[("tb", tbi)])
                        sg.append(tbi)
                    ms = []
                    for bi in range(2):
                        src = oa if bi == 0 else od
                        pb = 2 + bi
                        for k in range(4):
                            S.add("pe", lambda e, k=k, bi=bi, pb=pb, src=src, tc=tc: e.matmul(
                                ps[pb][:], wb[bi][:, k, :], src[:, k * SEQ + tc * 512:k * SEQ + tc * 512 + 512], start=(k == 0), stop=(k == 3)),
                                rd=[("wb", bi, k)] + ([("oa", tc)] if bi == 0 else [("od", k * 4 + tc)]), wr=[("ps", pb)])
                        ti = rr("tf", 8)
                        S.add("dve", lambda e, pb=pb, ti=ti, tbi=sg[bi]: e.tensor_tensor(out=TF[ti][:], in0=ps[pb][:], in1=TB[tbi][:], op=ALU.mult),
                              rd=[("ps", pb), ("tb", sg[bi])], wr=[("tf", ti)])
                        ms.append(ti)
                    S.add("pool", lambda e, tc=tc, ms=tuple(ms): e.tensor_tensor(
                        out=qkv[:, oc * SEQ + tc * 512:oc * SEQ + tc * 512 + 512], in0=TF[ms[0]][:], in1=TF[ms[1]][:], op=ALU.add),
                        rd=[("tf", ms[0]), ("tf", ms[1])], wr=ALLQ + [("mg", tc)])

            def load_wout():
                g_ = S.newgrp()
                S.add("pool", lambda e: e.memset(sm[:, 509:510], 0.0), rd=["wout_all"], wr=OAALL + ["oagate"])
                for k in range(8):
                    S.add("pool", lambda e, k=k: e.dma_start(out=oa[:, k * 1024:(k + 1) * 1024], in_=w_out[k * 128:(k + 1) * 128, :]),
                          rd=["oagate"], wr=[("wout", k)], dkey="wo", grp=g_)

            def tail_tile(b, tt):
                xs = tt % 2
                tile = b * 16 + tt
                r0 = b * SEQ + tt * 128
                S.add("sp", lambda e: e.dma_start(out=xt[xs][:], in_=x[r0:r0 + 128, :]), wr=[("xt", xs)], dkey=f"xt{xs}")
                for half in range(2):
                    pb = half
                    for k in range(8):
                        S.add("pe", lambda e, k=k, half=half, pb=pb: e.matmul(
                            ps[pb][:], qkv[:, k * SEQ + tt * 128:k * SEQ + tt * 128 + 128], oa[:, k * 1024 + half * 512:k * 1024 + half * 512 + 512],
                            start=(k == 0), stop=(k == 7)), rd=[("mg", tt // 4), ("wout", k), "wout_all"] + ALLQ + OAALL, wr=[("ps", pb)])
                    S.add("dve", lambda e, half=half, pb=pb: e.tensor_tensor(
                        out=xt[xs][:, half * 512:(half + 1) * 512], in0=ps[pb][:], in1=xt[xs][:, half * 512:(half + 1) * 512], op=ALU.add),
                        rd=[("ps", pb), ("xt", xs)], wr=[("xt", xs)])
                S.add("sp", lambda e: e.dma_start(out=y[r0:r0 + 128, :], in_=xt[xs][:]), rd=[("xt", xs)], wr=[("y", tile)], dkey=f"yst{xs}")
                if stage < 2:
                    return
                rmsnorm_rs(xt[xs], ("xt", xs), 2 + xs)
                S.add("dve", lambda e: e.tensor_scalar(out=xt[xs][:], in0=xt[xs][:], scalar1=sm[:, 2 + xs:3 + xs], scalar2=None,
                                                       op0=ALU.mult), rd=[("xt", xs), ("sm", 2 + xs)], wr=[("xt", xs)])
                S.add("pool", lambda e: e.tensor_tensor(out=h2[xs][:], in0=xt[xs][:], in1=gffnB[:], op=ALU.mult),
                      rd=[("xt", xs), "gffnB"], wr=[("h2", xs)])
                transposes_f32(xt[xs], ("xt", xs), lambda k: h2T[:, k, :], lambda k: "h2T", CF_GFFNT)
                pr = 2 + rr("psA", 2)
                for k in range(8):
                    S.add("pe", lambda e, k=k: e.matmul(ps[pr][:, 0:36], h2T[:, k, :], wr[:, k, :], start=(k == 0), stop=False),
                          rd=["h2T", "wr"], wr=[("ps", pr)])
                S.add("pe", lambda e: e.matmul(ps[pr][:, 0:36], onesF[0:1, :], brow[0:1, :], start=False, stop=True),
                      rd=["onesF", "brow"], wr=[("ps", pr)])
                LG, GM, NGM, GS, PG, GSEL, GB, EM, M8, OH0, MM, OH1, DD, SGD = 32, 68, 69, 70, 71, 72, 76, 80, 112, 120, 152, 184, 216, 217
                RK, OK, SV, TMP = 224, 256, 288, 320
                R = "rt"

                def V(fn, rd=(), wr=(R,)):
                    S.add("dve", fn, rd=[R] + list(rd), wr=list(wr))
                S.add("dve", lambda e: e.tensor_copy(out=sm[:, LG:LG + 36], in_=ps[pr][:, 0:36]), rd=[("ps", pr)], wr=[R])
                V(lambda e: e.reduce_max(out=sm[:, GM:GM + 1], in_=sm[:, LG:LG + 4], axis=AX.X))
                V(lambda e: e.tensor_scalar(out=sm[:, NGM:NGM + 1], in0=sm[:, GM:GM + 1], scalar1=-1.0, scalar2=None, op0=ALU.mult))
                S.add("act", lambda e: e.activation(out=sm[:, GSEL:GSEL + 4], in_=sm[:, LG:LG + 4], func=AF.Exp, bias=sm[:, NGM:NGM + 1],
                                                    accum_out=sm[:, GS:GS + 1]), rd=[R], wr=[R])
                V(lambda e: e.reciprocal(out=sm[:, PG:PG + 1], in_=sm[:, GS:GS + 1]))
                V(lambda e: e.tensor_scalar(out=sm[:, GB:GB + 4], in0=sm[:, LG:LG + 4], scalar1=sm[:, GM:GM + 1], scalar2=1e9,
                                            op0=ALU.is_ge, op1=ALU.mult))
                V(lambda e: e.tensor_scalar(out=sm[:, GB:GB + 4], in0=sm[:, GB:GB + 4], scalar1=-1e9, scalar2=None, op0=ALU.add))
                for g in range(4):
                    V(lambda e, g=g: e.tensor_scalar(out=sm[:, EM + g * 8:EM + g * 8 + 8], in0=sm[:, LG + 4 + g * 8:LG + 12 + g * 8],
                                                     scalar1=sm[:, GB + g:GB + g + 1], scalar2=None, op0=ALU.add))
                V(lambda e: e.max(out=sm[:, M8:M8 + 8], in_=sm[:, EM:EM + 32]))
                V(lambda e: e.tensor_scalar(out=sm[:, OH0:OH0 + 32], in0=sm[:, EM:EM + 32], scalar1=sm[:, M8:M8 + 1], scalar2=None,
                                            op0=ALU.is_ge))
                V(lambda e: e.tensor_scalar(out=sm[:, MM:MM + 32], in0=sm[:, EM:EM + 32], scalar1=sm[:, M8 + 1:M8 + 2], scalar2=None,
                                            op0=ALU.is_ge))
                V(lambda e: e.tensor_tensor(out=sm[:, OH1:OH1 + 32], in0=sm[:, MM:MM + 32], in1=sm[:, OH0:OH0 + 32], op=ALU.subtract))
                V(lambda e: e.tensor_tensor(out=sm[:, DD:DD + 1], in0=sm[:, M8:M8 + 1], in1=sm[:, M8 + 1:M8 + 2], op=ALU.subtract))
                S.add("act", lambda e: e.activation(out=sm[:, SGD:SGD + 1], in_=sm[:, DD:DD + 1], func=AF.Sigmoid), rd=[R], wr=[R])
                V(lambda e: e.tensor_tensor(out=wts[:, 2 * tile:2 * tile + 1], in0=sm[:, SGD:SGD + 1], in1=sm[:, PG:PG + 1],
                                            op=ALU.mult), wr=[R, "wts"])
                V(lambda e: e.tensor_tensor(out=wts[:, 2 * tile + 1:2 * tile + 2], in0=sm[:, PG:PG + 1],
                                            in1=wts[:, 2 * tile:2 * tile + 1], op=ALU.subtract), rd=["wts"], wr=[R, "wts"])
                tbi = rr("tb", 8)
                S.add("dve", lambda e: e.tensor_copy(out=TB[tbi][:, 0:32], in_=sm[:, MM:MM + 32]), rd=[R], wr=[("tb", tbi)])
                pk = 2 + rr("psA", 2)
                S.add("pe", lambda e: e.matmul(ps[pk][:, 0:32], ustr, TB[tbi][:, 0:32], start=True, stop=True),
                      rd=[("tb", tbi), "cb"], wr=[("ps", pk)])
                S.add("pe", lambda e: e.matmul(ps[pk][:, 32:64], onesB, TB[tbi][:, 0:32], start=True, stop=True),
                      rd=[("tb", tbi), "cb"], wr=[("ps", pk)])
                S.add("dve", lambda e: e.tensor_tensor(out=sm[:, RK:RK + 32], in0=ps[pk][:, 0:32], in1=carry[:], op=ALU.add),
                      rd=[R, ("ps", pk), "carry"], wr=[R])
                S.add("dve", lambda e: e.tensor_tensor(out=carry[:], in0=ps[pk][:, 32:64], in1=carry[:], op=ALU.add),
                      rd=[R, ("ps", pk), "carry"], wr=["carry"])
                BIG = float(1 << 22)
                V(lambda e: e.tensor_scalar(out=sm[:, OK:OK + 32], in0=sm[:, RK:RK + 32], scalar1=float(CAP), scalar2=None, op0=ALU.is_lt))
                V(lambda e: e.tensor_tensor(out=sm[:, SV:SV + 32], in0=sm[:, RK:RK + 32], in1=ebase, op=ALU.add), rd=["cf"])
                V(lambda e: e.tensor_scalar(out=sm[:, SV:SV + 32], in0=sm[:, SV:SV + 32], scalar1=-BIG, scalar2=None, op0=ALU.add))
                V(lambda e: e.tensor_tensor(out=sm[:, SV:SV + 32], in0=sm[:, SV:SV + 32], in1=sm[:, OK:OK + 32], op=ALU.mult))
                V(lambda e: e.tensor_scalar(out=sm[:, SV:SV + 32], in0=sm[:, SV:SV + 32], scalar1=BIG, scalar2=None, op0=ALU.add))
                for kk, OH in enumerate((OH0, OH1)):
                    V(lambda e, OH=OH: e.tensor_tensor(out=sm[:, TMP:TMP + 32], in0=sm[:, SV:SV + 32], in1=sm[:, OH:OH + 32], op=ALU.mult))
                    V(lambda e, kk=kk: e.reduce_sum(out=sm[:, TMP + 32 + kk:TMP + 33 + kk], in_=sm[:, TMP:TMP + 32], axis=AX.X))
                    V(lambda e, kk=kk: e.tensor_copy(out=sl[:, 2 * tile + kk:2 * tile + kk + 1],
                                                     in_=sm[:, TMP + 32 + kk:TMP + 33 + kk]), wr=[R, ("sl", tile, kk)])
                    S.add("pool", lambda e, kk=kk: e.indirect_dma_start(
                        out=xdisp, out_offset=bass.IndirectOffsetOnAxis(ap=sl[:, 2 * tile + kk:2 * tile + kk + 1], axis=0),
                        in_=h2[xs][:, :], in_offset=None, bounds_check=bnd(e, "A"), oob_is_err=False),
                        rd=[("h2", xs), ("sl", tile, kk)] + [("xz", i_) for i_ in range(NE * CAP // 128)], wr=["xdisp_w"], dkey=f"sc{xs}{kk}")

            import os
            KSTOP = float(os.environ.get("KSTOP", "99"))
            for b in range(NB):
                stage1(b)
                if b == 0:
                    for i in range(NE * CAP // 128):
                        S.add("sp", lambda e, i=i: e.dma_start(out=xdisp[i * 128:(i + 1) * 128, :], in_=zt[:]), rd=["zt"], wr=[("xz", i)], dkey="xz")
                if KSTOP <= 1:
                    break
                v_proj(1024)
                for p in range(4):
                    qk_proj(p * 128, 512 + p * 128, p % 2)
                    moba_ksum(p % 2)
                    for hh in range(2):
                        for st_ in moba_head(p, hh, p % 2, mode="gate"):
                            st_()
                        moba_head(p, hh, p % 2)
                if KSTOP <= 3:
                    break
                v_proj(2560)
                for h in range(4):
                    qk_proj(1536 + h * 128, 2048 + h * 128, h % 2)
                    diff_head(h, h % 2)
                if KSTOP <= 4:
                    break
                for oc in range(8):
                    merge_oc(oc)
                load_wout()
                if KSTOP <= 5:
                    break
                for tt in range(16):
                    tail_tile(b, tt)
            S.emit()

        if stage < 3:
            return nc
        with ExitStack() as st:
            S = Sched(nc, top, "B")
            A = partial(sb, st=st)
            pTs = [st.enter_context(nc.psum_tensor(f"pT{i}", [128, 1024], BF16)) for i in range(2)]
            pG = [st.enter_context(nc.psum_tensor(f"pG{i}", [128, 512], F32)) for i in range(2)]
            pU = [st.enter_context(nc.psum_tensor(f"pU{i}", [128, 512], F32)) for i in range(2)]
            pY = [st.enter_context(nc.psum_tensor(f"pY{i}", [128, 512], F32)) for i in range(2)]
            identB = A("identB2", [128, 128], BF16)
            w1b = [A(f"w1b{i}", [128, 8, 512], BF16) for i in range(2)]
            w3b = [A(f"w3b{i}", [128, 8, 512], BF16) for i in range(2)]
            w2b = [A(f"w2b{i}", [128, 4, 1024], BF16) for i in range(2)]
            xd = [A(f"xd{i}", [128, D], BF16) for i in range(2)]
            xT = [A(f"xT{i}", [128, 8, CAP], BF16) for i in range(2)]
            sgl = [A(f"sgl{i}", [128, 512], F32) for i in range(2)]
            aT = [A(f"aT{i}", [128, 4, CAP], BF16) for i in range(2)]
            yo = [A(f"yo{i}", [128, D], F32) for i in range(2)]
            S.add("pool", lambda e: e.dma_start(out=identB[:], in_=cb_d[:, CB_IDENT:CB_IDENT + 128]), wr=["identB"], dkey="identB")
            nblk = CAP // 128
            ctr = 0
            w3f = [A(f"w3f{i}", [128, 8, 512], F32) for i in range(2)]
            w2f = [A(f"w2f{i}", [128, 4, 1024], F32) for i in range(2)]

            def load_w(ex):
                s = ex % 2
                g_ = S.newgrp()
                for k in range(8):
                    S.add("pool", lambda e, k=k: e.dma_start(out=w1b[s][:, k, :], in_=w1[ex, k * 128:(k + 1) * 128, :]),
                          wr=[("w1", s, k)], dkey=f"w1_{s}", grp=g_)
                for k in range(8):
                    S.add("act", lambda e, k=k: e.dma_start(out=w3f[s][:, k, :], in_=w3[ex, k * 128:(k + 1) * 128, :]),
                          wr=[("w3f", s, k)], dkey=f"w3f{s}", grp=g_)
                for k in range(4):
                    S.add("act", lambda e, k=k: e.dma_start(out=w2f[s][:, k, :], in_=w2[ex, k * 128:(k + 1) * 128, :]),
                          wr=[("w2f", s, k)], dkey=f"w2f{s}", grp=g_)

            def cast_w(ex):
                s = ex % 2
                for k in range(8):
                    if k % 2 == 0:
                        S.add("act", lambda e, k=k: e.copy(out=w3b[s][:, k, :], in_=w3f[s][:, k, :]), rd=[("w3f", s, k)], wr=[("w3", s, k)])
                    else:
                        S.add("dve", lambda e, k=k: e.tensor_copy(out=w3b[s][:, k, :], in_=w3f[s][:, k, :]), rd=[("w3f", s, k)], wr=[("w3", s, k)])
                for k in range(4):
                    if k % 2 == 0:
                        S.add("dve", lambda e, k=k: e.tensor_copy(out=w2b[s][:, k, :], in_=w2f[s][:, k, :]), rd=[("w2f", s, k)], wr=[("w2", s, k)])
                    else:
                        S.add("act", lambda e, k=k: e.copy(out=w2b[s][:, k, :], in_=w2f[s][:, k, :]), rd=[("w2f", s, k)], wr=[("w2", s, k)])

            load_w(0)
            cast_w(0)
            for ex in range(NE):
                s = ex % 2
                if ex + 1 < NE:
                    load_w(ex + 1)
                for blk in range(nblk):
                    xs = ctr % 2
                    ctr += 1
                    r0 = ex * CAP + blk * 128
                    S.add("sp", lambda e, xs=xs, r0=r0: e.dma_start(out=xd[xs][:], in_=xdisp[r0:r0 + 128, :]), wr=[("xd", xs)], dkey=f"xd{xs}")
                    pi = xs
                    for k in range(8):
                        S.add("pe", lambda e, xs=xs, k=k, pi=pi: e.transpose(out=pTs[pi][:, k * 128:(k + 1) * 128], in_=xd[xs][:, k * 128:(k + 1) * 128],
                                                                             identity=identB[:]), rd=[("xd", xs), "identB"], wr=[("pT", pi)])
                    if xs == 0:
                        S.add("act", lambda e, s=s, blk=blk, pi=pi: e.copy(out=xT[s][:, :, blk * 128:(blk + 1) * 128],
                                                                           in_=pTs[pi][:, :].rearrange("p (k c) -> p k c", c=128)),
                              rd=[("pT", pi)], wr=[("xT", s)])
                    else:
                        S.add("dve", lambda e, s=s, blk=blk, pi=pi: e.tensor_copy(out=xT[s][:, :, blk * 128:(blk + 1) * 128],
                                                                                  in_=pTs[pi][:, :].rearrange("p (k c) -> p k c", c=128)),
                              rd=[("pT", pi)], wr=[("xT", s)])
                for fc in range(4):
                    g = fc % 2
                    for k in range(8):
                        S.add("pe", lambda e, s=s, k=k, fc=fc, g=g: e.matmul(pG[g][:, 0:CAP], w1b[s][:, k, fc * 128:(fc + 1) * 128], xT[s][:, k, :],
                                                                             start=(k == 0), stop=(k == 7)), rd=[("w1", s, k), ("xT", s)], wr=[("pG", g)])
                    for k in range(8):
                        S.add("pe", lambda e, s=s, k=k, fc=fc, g=g: e.matmul(pU[g][:, 0:CAP], w3b[s][:, k, fc * 128:(fc + 1) * 128], xT[s][:, k, :],
                                                                             start=(k == 0), stop=(k == 7)), rd=[("w3", s, k), ("xT", s)], wr=[("pU", g)])
                    S.add("act", lambda e, g=g: e.activation(out=sgl[g][:, 0:CAP], in_=pG[g][:, 0:CAP], func=AF.Silu), rd=[("pG", g)], wr=[("sgl", g)])
                    S.add("dve", lambda e, s=s, fc=fc, g=g: e.tensor_tensor(out=aT[s][:, fc, :], in0=pU[g][:, 0:CAP], in1=sgl[g][:, 0:CAP], op=ALU.mult),
                          rd=[("pU", g), ("sgl", g)], wr=[("aT", s)])
                for blk in range(nblk):
                    ys = (ex * nblk + blk) % 2
                    r0 = ex * CAP + blk * 128
                    for half in range(2):
                        for j in range(4):
                            S.add("pe", lambda e, s=s, j=j, blk=blk, half=half: e.matmul(
                                pY[half][:], aT[s][:, j, blk * 128:(blk + 1) * 128], w2b[s][:, j, half * 512:(half + 1) * 512],
                                start=(j == 0), stop=(j == 3)), rd=[("aT", s), ("w2", s, j)], wr=[("pY", half)])
                        if half == 0:
                            S.add("act", lambda e, ys=ys: e.copy(out=yo[ys][:, 0:512], in_=pY[0][:]), rd=[("pY", 0)], wr=[("yo", ys)])
                        else:
                            S.add("dve", lambda e, ys=ys: e.tensor_copy(out=yo[ys][:, 512:1024], in_=pY[1][:]), rd=[("pY", 1)], wr=[("yo", ys)])
                    S.add("sp", lambda e, ys=ys, r0=r0: e.dma_start(out=ybuf[r0:r0 + 128, :], in_=yo[ys][:]), rd=[("yo", ys)], wr=[("ybuf", ex, blk)],
                          dkey=f"yo{ys}")
                if ex + 1 < NE:
                    cast_w(ex + 1)
            S.emit()

        with ExitStack() as st:
            S = Sched(nc, top, "C")
            A = partial(sb, st=st)
            x1 = [A(f"x1_{i}", [128, D], F32) for i in range(4)]
            g0 = [A(f"g0_{i}", [128, D], F32) for i in range(4)]
            g1 = [A(f"g1_{i}", [128, D], F32) for i in range(4)]
            junk = A("junkC", [128, D], BF16)
            smc = A("smc", [128, 8], F32)
            epsT = A("epsTC", [128, 1], F32)
            S.add("dve", lambda e: e.memset(epsT[:], EPS), wr=["epsT"])
            for tile in range(32):
                s = tile % 4
                r0 = tile * 128
                S.add("sp", lambda e, s=s, r0=r0: e.dma_start(out=x1[s][:], in_=y[r0:r0 + 128, :]), wr=[("x1", s)], dkey=f"x1{s}")
                S.add("pool", lambda e, s=s: e.memset(g0[s][:], 0.0), wr=[("g0", s)])
                S.add("pool", lambda e, s=s: e.memset(g1[s][:], 0.0), wr=[("g1", s)])
                S.add("pool", lambda e, s=s, tile=tile: e.indirect_dma_start(
                    out=g0[s][:, :], out_offset=None, in_=ybuf,
                    in_offset=bass.IndirectOffsetOnAxis(ap=sl[:, 2 * tile:2 * tile + 1], axis=0), bounds_check=bnd(e, "C"), oob_is_err=False),
                    wr=[("g0", s)], dkey=f"g0{s}")
                S.add("pool", lambda e, s=s, tile=tile: e.indirect_dma_start(
                    out=g1[s][:, :], out_offset=None, in_=ybuf,
                    in_offset=bass.IndirectOffsetOnAxis(ap=sl[:, 2 * tile + 1:2 * tile + 2], axis=0), bounds_check=bnd(e, "C"), oob_is_err=False),
                    wr=[("g1", s)], dkey=f"g1{s}")
                S.add("dve", lambda e, s=s, tile=tile: e.scalar_tensor_tensor(out=x1[s][:], in0=g0[s][:], scalar=wts[:, 2 * tile:2 * tile + 1],
                                                                              in1=x1[s][:], op0=ALU.mult, op1=ALU.add),
                      rd=[("g0", s), ("x1", s)], wr=[("x1", s)])
                S.add("dve", lambda e, s=s, tile=tile: e.scalar_tensor_tensor(out=x1[s][:], in0=g1[s][:], scalar=wts[:, 2 * tile + 1:2 * tile + 2],
                                                                              in1=x1[s][:], op0=ALU.mult, op1=ALU.add),
                      rd=[("g1", s), ("x1", s)], wr=[("x1", s)])
                S.add("act", lambda e, s=s: e.activation(out=junk[:], in_=x1[s][:], func=AF.Square, accum_out=smc[:, s:s + 1]),
                      rd=[("x1", s)], wr=["junk", ("smc", s)])
                S.add("act", lambda e, s=s: e.activation(out=smc[:, s:s + 1], in_=smc[:, s:s + 1], func=AF.Sqrt, bias=epsT[:, 0:1], scale=1.0 / D),
                      rd=[("smc", s), "epsT"], wr=[("smc", s)])
                S.add("dve", lambda e, s=s: e.reciprocal(out=smc[:, s:s + 1], in_=smc[:, s:s + 1]), rd=[("smc", s)], wr=[("smc", s)])
                S.add("dve", lambda e, s=s: e.scalar_tensor_tensor(out=x1[s][:], in0=x1[s][:], scalar=smc[:, s:s + 1], in1=gfinB[:],
                                                                   op0=ALU.mult, op1=ALU.mult), rd=[("x1", s), ("smc", s)], wr=[("x1", s)])
                S.add("sp", lambda e, s=s, r0=r0: e.dma_start(out=y[r0:r0 + 128, :], in_=x1[s][:]), rd=[("x1", s)], wr=[("y", tile)], dkey=f"yo{s}")
            S.emit()
    return nc


def _consts():
    cb = np.zeros((128, NCB), np.float32)
    cb[:, CB_IDENT:CB_IDENT + 128] = np.eye(128)
    r = np.arange(128)
    partner = np.where(r % 64 < 32, r + 32, r - 32)
    cb[partner, CB_PERM + r] = 1.0
    cb[:, CB_ONES:CB_ONES + 128] = 1.0
    cb[:, CB_USTR:CB_USTR + 128] = (r[:, None] < r[None, :])
    q = np.arange(512)
    for j in range(4):
        cb[:, CB_MASK + j * 512:CB_MASK + (j + 1) * 512] = (q[None, :] >= j * 128 + r[:, None])
    inv = 1.0 / (10000.0 ** (np.arange(0, 64, 2, dtype=np.float32) / 64.0))
    ang = np.arange(SEQ, dtype=np.float32)[:, None] * inv[None, :].astype(np.float32)
    ang = np.concatenate([ang, ang], axis=-1).astype(np.float32)
    cosT = np.cos(ang).T.astype(np.float32)
    sinT = np.sin(ang).T.astype(np.float32)
    sinS = sinT.copy()
    sinS[:32] *= -1.0
    cb[:, CB_COS:CB_COS + SEQ] = np.concatenate([cosT, cosT], 0)
    cb[:, CB_SIN:CB_SIN + SEQ] = np.concatenate([sinS, sinS], 0)
    sel = np.zeros((8, 1024), np.float32)
    for j in range(8):
        sel[j, j * 128:(j + 1) * 128] = 1.0
    return cb, sel


_STAGE = 99


def _prep(x, g_mix, w_in, w_branch_moba, w_branch_diff, w_out,
           diff_lambda_q1, diff_lambda_k1, diff_lambda_q2, diff_lambda_k2, diff_subln_g,
           g_ffn, w_group, b_group, w_router, b_router,
           w_expert_gate, w_expert_up, w_expert_down, g_final):
    f = lambda a: np.ascontiguousarray(np.asarray(a, dtype=np.float32))
    x = f(x)
    cb, sel = _consts()
    cf = np.zeros((128, NCF), np.float32)
    cf[:, CF_IDENT:CF_IDENT + 128] = np.eye(128)
    cf[:, CF_EBASE:CF_EBASE + 32] = (np.arange(32) * CAP)[None, :]
    cf[:, CF_GMIXT:CF_GMIXT + 8] = f(g_mix)[0].reshape(8, 128).T
    cf[:, CF_GFFNT:CF_GFFNT + 8] = f(g_ffn)[0].reshape(8, 128).T
    cf[:, CF_GSUB] = f(diff_subln_g)[0]
    cf[:, CF_LAM:CF_LAM + 256] = np.concatenate([f(diff_lambda_q1)[0], f(diff_lambda_k1)[0], f(diff_lambda_q2)[0],
                                                 f(diff_lambda_k2)[0]])[None, :]
    shared = {
        "w_in": f(w_in)[0], "w_bm": f(w_branch_moba)[0], "w_bd": f(w_branch_diff)[0], "w_out": f(w_out)[0],
        "w1": f(w_expert_gate)[0], "w3": f(w_expert_up)[0], "w2": f(w_expert_down)[0],
        "wr": np.ascontiguousarray(np.concatenate([f(w_group)[0], f(w_router)[0]], axis=1)),
        "brow": np.ascontiguousarray(np.concatenate([f(b_group)[0], f(b_router)[0]])[None, :]),
        "gffnB": np.ascontiguousarray(np.broadcast_to(f(g_ffn)[0][None, :], (128, D))),
        "gfinB": np.ascontiguousarray(np.broadcast_to(f(g_final)[None, :], (128, D))),
        "cf": cf, "cb": cb, "sel": sel,
    }
    xs = x.reshape(NCORES, TOK, D)
    return shared, xs


def kernel(**inputs):
    shared, xs = _prep(**inputs)
    nc = build_program(_STAGE)
    in_maps = [dict(shared, x=np.ascontiguousarray(xs[c])) for c in range(NCORES)]
    res = run_bass_kernel_spmd(nc, in_maps, core_ids=list(range(NCORES)))
    out = np.stack([np.asarray(r["y"]) for r in res.results], axis=0)
    return out.reshape(16, SEQ, D).astype(np.float32)
```

```python
import math
from contextlib import ExitStack
from functools import partial

import numpy as np
import concourse.bass as bass
import concourse.mybir as mybir
from concourse.bass_utils import run_bass_kernel_spmd

F32 = mybir.dt.float32
BF16 = mybir.dt.bfloat16
I32 = mybir.dt.int32
AF = mybir.ActivationFunctionType
ALU = mybir.AluOpType
AX = mybir.AxisListType

NCORES = 8
SEQ = 2048
D = 1024
TOK = 2 * SEQ
CAP = 512
NE = 32
EPS = 1e-6
NB = 2


class Sched:
    def __init__(self, nc, stack, tag):
        self.nc, self.stack, self.tag = nc, stack, tag
        self.ops, self.lastw, self.readers, self.dsem = [], {}, {}, {}
        self.grpmax, self.gctr = {}, 0
        self.esem = {e: stack.enter_context(nc.semaphore(f"{tag}_{e}")) for e in ("pe", "act", "dve", "pool")}

    EXCL = ("ps", "pG", "pU", "pY", "pT")

    def newgrp(self):
        self.gctr += 1
        return self.gctr

    def add(self, eng, fn, rd=(), wr=(), dkey=None, grp=None):
        ex = [r for r in rd if (r[0] if isinstance(r, tuple) else r) in self.EXCL]
        if ex:
            rd = [r for r in rd if r not in ex]
            wr = list(wr) + [r for r in ex if r not in wr]
        deps = set()
        for r in rd:
            w = self.lastw.get(r)
            if w is not None:
                deps.add(w)
        for r in wr:
            w = self.lastw.get(r)
            if w is not None:
                deps.add(w)
            deps.update(self.readers.get(r, ()))
        i = len(self.ops)
        op = dict(eng=eng, fn=fn, deps=deps, dkey=dkey, inc=False, seq=0)
        if dkey is not None:
            if dkey not in self.dsem:
                self.dsem[dkey] = [self.stack.enter_context(self.nc.semaphore(f"{self.tag}_d_{dkey}")), 0]
            self.dsem[dkey][1] += 16
            op["dval"] = self.dsem[dkey][1]
            op["grp"] = grp
            if grp is not None:
                self.grpmax[(dkey, grp)] = op["dval"]
        self.ops.append(op)
        for r in rd:
            self.readers.setdefault(r, []).append(i)
        for r in wr:
            self.lastw[r] = i
            self.readers[r] = []
        return i

    def finalize(self):
        for op in self.ops:
            for d in op["deps"]:
                Dd = self.ops[d]
                if Dd["dkey"] is None:
                    Dd["inc"] = True
        cnt = {e: 0 for e in self.esem}
        for op in self.ops:
            if op["dkey"] is None and op["inc"]:
                cnt[op["eng"]] += 1
                op["seq"] = cnt[op["eng"]]

    def run(self, eng, e):
        waited = {}
        for op in self.ops:
            if op["eng"] != eng:
                continue
            need = {}
            for d in op["deps"]:
                Dd = self.ops[d]
                if Dd["dkey"] is not None:
                    key, sem, val = "d" + Dd["dkey"], self.dsem[Dd["dkey"]][0], Dd["dval"]
                    if Dd["grp"] is not None:
                        val = self.grpmax[(Dd["dkey"], Dd["grp"])]
                else:
                    if Dd["eng"] == "pe" and eng == "pe" and op["dkey"] is None:
                        continue
                    key, sem, val = Dd["eng"], self.esem[Dd["eng"]], Dd["seq"]
                if key not in need or need[key][1] < val:
                    need[key] = (sem, val)
            for key in sorted(need):
                sem, val = need[key]
                if waited.get(key, 0) >= val:
                    continue
                e.wait_ge(sem, val)
                waited[key] = val
            ins = op["fn"](e)
            if op["dkey"] is not None:
                ins.then_inc(self.dsem[op["dkey"]][0], 16)
            elif op["inc"]:
                ins.then_inc(self.esem[op["eng"]], 1)
        if eng == "sp":
            for k, (sem, val) in self.dsem.items():
                e.wait_ge(sem, val)

    def emit(self):
        self.finalize()
        with self.nc.Block() as block:
            @block.tensor
            def _(e):
                self.run("pe", e)

            @block.scalar
            def _(e):
                self.run("act", e)

            @block.vector
            def _(e):
                self.run("dve", e)

            @block.gpsimd
            def _(e):
                self.run("pool", e)

            @block.sync
            def _(e):
                self.run("sp", e)


CB_IDENT, CB_PERM, CB_ONES, CB_USTR, CB_MASK, CB_COS, CB_SIN = 0, 128, 256, 384, 512, 2560, 4608
NCB = 6656
CF_IDENT, CF_EBASE, CF_GMIXT, CF_GFFNT, CF_GSUB, CF_LAM = 0, 128, 160, 168, 176, 177
NCF = 177 + 256


def build_program(stage=99):
    nc = bass.Bass("TRN2", target_bir_lowering=False)
    bndreg = {}

    def bnd(e, tag):
        if tag not in bndreg:
            r = e.alloc_register("bnd" + tag)
            e.reg_mov(r, NE * CAP - 1)
            bndreg[tag] = r
        return bndreg[tag]

    def din(name, shape, dtype=F32):
        return nc.dram_tensor(name, shape, dtype, kind="ExternalInput").ap()

    x = din("x", [TOK, D])
    w_in = din("w_in", [D, 5120])
    w_bm = din("w_bm", [512, D])
    w_bd = din("w_bd", [512, D])
    w_out = din("w_out", [D, D])
    w1 = din("w1", [NE, D, 512])
    w3 = din("w3", [NE, D, 512])
    w2 = din("w2", [NE, 512, D])
    wr_d = din("wr", [D, 36])
    brow_d = din("brow", [1, 36])
    gffnB_d = din("gffnB", [128, D])
    gfinB_d = din("gfinB", [128, D])
    cf_d = din("cf", [128, NCF])
    cb_d = din("cb", [128, NCB])
    sel_d = din("sel", [8, 1024])
    y = nc.dram_tensor("y", [TOK, D], F32, kind="ExternalOutput").ap()
    ybuf = nc.dram_tensor("ybuf", [NE * CAP, D], F32, kind="ExternalOutput").ap()
    xdisp = nc.dram_tensor("xdisp", [NE * CAP, D], BF16, kind="Internal").ap()

    w_in_k = w_in.rearrange("(k p) c -> p k c", p=128)
    w_bm_k = w_bm.rearrange("(k p) c -> p k c", p=128)
    w_bd_k = w_bd.rearrange("(k p) c -> p k c", p=128)
    w_out_k = w_out.rearrange("(k p) c -> p k c", p=128)
    wr_k = wr_d.rearrange("(k p) c -> p k c", p=128)

    with ExitStack() as top:
        def sb(name, shape, dtype, st=top):
            return st.enter_context(nc.sbuf_tensor(name, shape, dtype))

        sl = sb("sl", [128, 64], I32)
        wts = sb("wts", [128, 64], F32)
        gfinB = sb("gfinB_sb", [128, D], F32)

        with ExitStack() as st:
            S = Sched(nc, top, "A")
            A = partial(sb, st=st)
            ps = [st.enter_context(nc.psum_tensor(f"ps{i}", [128, 512], F32)) for i in range(8)]
            cf = A("cf_sb", [128, NCF], F32)
            cb = A("cb_sb", [128, NCB], BF16)
            sel = A("sel_sb", [8, 1024], BF16)
            gffnB = A("gffnB_sb", [128, D], F32)
            wr = A("wr_sb", [128, 8, 36], F32)
            brow = A("brow_sb", [1, 36], F32)
            onesF = A("onesF", [128, 128], F32)
            epsT = A("epsT", [128, 1], F32)
            hT = A("hT", [128, 8 * SEQ], BF16)
            qkv = A("qkv", [128, 8 * SEQ], BF16)
            oa = A("oa", [128, 4 * SEQ], BF16)
            od = A("od", [128, 4 * SEQ], BF16)
            xt = [A(f"xt{i}", [128, D], F32) for i in range(2)]
            junk = A("junk", [128, D], BF16)
            wq = [A(f"wq{i}", [128, 8, 128], BF16) for i in range(3)]
            wb = [A(f"wb{i}", [128, 4, 128], BF16) for i in range(2)]
            TF = [A(f"tf{i}", [128, 512], F32) for i in range(8)]
            TB = [A(f"tb{i}", [128, 512], BF16) for i in range(8)]
            h2 = [A(f"h2_{i}", [128, D], BF16) for i in range(2)]
            h2T = A("h2T", [128, 8, 128], F32)
            biasT = [A(f"biasT{i}", [8, 1024], BF16) for i in range(2)]
            ksf = A("ksf", [128, 8], F32)
            ksum = [A(f"ksum{i}", [128, 8], BF16) for i in range(2)]
            gsb = [A(f"gsb{i}", [128, 8], F32) for i in range(4)]
            sm = A("sm", [128, 640], F32)
            carry = A("carry", [128, 32], F32)
            neglam = A("neglam", [128, 1], F32)
            gsub8 = A("gsub8", [128, 1], F32)

            identF = cf[:, CF_IDENT:CF_IDENT + 128]
            ebase = cf[:, CF_EBASE:CF_EBASE + 32]
            identB = cb[:, CB_IDENT:CB_IDENT + 128]
            perm = cb[:, CB_PERM:CB_PERM + 128]
            onesB = cb[:, CB_ONES:CB_ONES + 128]
            ustr = cb[:, CB_USTR:CB_USTR + 128]

            def maskj(j, n):
                return cb[:, CB_MASK + j * 512:CB_MASK + j * 512 + n]

            S.add("sp", lambda e: e.dma_start(out=cf[:], in_=cf_d), wr=["cf"], dkey="cf")
            gcb = S.newgrp()
            for i in range(0, NCB, 1664):
                S.add("pool", lambda e, i=i: e.dma_start(out=cb[:, i:i + 1664], in_=cb_d[:, i:i + 1664]),
                      wr=[("cbp", i)], dkey="cb", grp=gcb)
            S.add("dve", lambda e: e.memset(sm[:, 510:511], 0.0), rd=[("cbp", i) for i in range(0, NCB, 1664)], wr=["cb"])
            S.add("pool", lambda e: e.dma_start(out=sel[:], in_=sel_d), wr=["sel"], dkey="sel")
            S.add("sp", lambda e: e.dma_start(out=gffnB[:], in_=gffnB_d), wr=["gffnB"], dkey="gffnB")
            S.add("sp", lambda e: e.dma_start(out=gfinB[:], in_=gfinB_d), wr=["gfinB"], dkey="gfinB")
            S.add("sp", lambda e: e.dma_start(out=wr[:], in_=wr_k), wr=["wr"], dkey="wr")
            S.add("sp", lambda e: e.dma_start(out=brow[:], in_=brow_d), wr=["brow"], dkey="brow")
            zt = A("zt", [128, D], BF16)
            S.add("dve", lambda e: e.memset(zt[:], 0.0), wr=["zt"])
            S.add("dve", lambda e: e.memset(onesF[:], 1.0), wr=["onesF"])
            S.add("dve", lambda e: e.memset(epsT[:], EPS), wr=["epsT"])
            S.add("dve", lambda e: e.memset(carry[:], 0.0), wr=["carry"])
            for i in range(4):
                S.add("dve", lambda e, i=i: e.memset(gsb[i][:], -1e30), wr=[f"gsb{i}"])
            lam = cf[:, CF_LAM:CF_LAM + 256]
            S.add("dve", lambda e: e.tensor_tensor(out=sm[:, 512:576], in0=lam[:, 0:64], in1=lam[:, 64:128], op=ALU.mult),
                  rd=["cf"], wr=["lamsc"])
            S.add("dve", lambda e: e.reduce_sum(out=sm[:, 500:501], in_=sm[:, 512:576], axis=AX.X), rd=["lamsc"], wr=["lamsc"])
            S.add("dve", lambda e: e.tensor_tensor(out=sm[:, 576:640], in0=lam[:, 128:192], in1=lam[:, 192:256], op=ALU.mult),
                  rd=["cf", "lamsc"], wr=["lamsc"])
            S.add("dve", lambda e: e.reduce_sum(out=sm[:, 501:502], in_=sm[:, 576:640], axis=AX.X), rd=["lamsc"], wr=["lamsc"])
            S.add("act", lambda e: e.activation(out=sm[:, 502:504], in_=sm[:, 500:502], func=AF.Exp), rd=["lamsc"], wr=["lamsc"])
            S.add("dve", lambda e: e.tensor_tensor(out=sm[:, 504:505], in0=sm[:, 503:504], in1=sm[:, 502:503], op=ALU.subtract),
                  rd=["lamsc"], wr=["lamsc"])
            S.add("dve", lambda e: e.tensor_scalar(out=neglam[:], in0=sm[:, 504:505], scalar1=-0.2, scalar2=None, op0=ALU.add),
                  rd=["lamsc"], wr=["neglam"])
            S.add("dve", lambda e: e.tensor_scalar(out=gsub8[:], in0=cf[:, CF_GSUB:CF_GSUB + 1], scalar1=0.8, scalar2=None,
                                                   op0=ALU.mult), rd=["cf"], wr=["gsub8"])

            cnt = {"wq": 0, "tf": 0, "tb": 0, "psA": 0}

            def rr(name, n):
                v = cnt[name] % n
                cnt[name] += 1
                return v

            def rmsnorm_rs(src, srcres, col):
                S.add("act", lambda e: e.activation(out=junk[:], in_=src[:], func=AF.Square, accum_out=sm[:, col:col + 1]),
                      rd=[srcres], wr=["junk", ("sm", col)])
                S.add("act", lambda e: e.activation(out=sm[:, col:col + 1], in_=sm[:, col:col + 1], func=AF.Sqrt,
                                                    bias=epsT[:, 0:1], scale=1.0 / D), rd=[("sm", col), "epsT"], wr=[("sm", col)])
                S.add("dve", lambda e: e.reciprocal(out=sm[:, col:col + 1], in_=sm[:, col:col + 1]),
                      rd=[("sm", col)], wr=[("sm", col)])

            def transposes_f32(src, srcres, dst_fn, dstres, gT_off):
                for half in range(2):
                    pb = 2 + rr("psA", 2)
                    for k4 in range(4):
                        k = half * 4 + k4
                        S.add("pe", lambda e, pb=pb, k=k, k4=k4: e.transpose(out=ps[pb][:, k4 * 128:(k4 + 1) * 128],
                                                                             in_=src[:, k * 128:(k + 1) * 128], identity=identF),
                              rd=[srcres, "cf"], wr=[("ps", pb)])
                    for k4 in range(4):
                        k = half * 4 + k4
                        eng = "act" if half == 0 else "dve"
                        if eng == "act":
                            S.add("act", lambda e, pb=pb, k=k, k4=k4: e.activation(
                                out=dst_fn(k), in_=ps[pb][:, k4 * 128:(k4 + 1) * 128], func=AF.Copy,
                                scale=cf[:, gT_off + k:gT_off + k + 1]), rd=[("ps", pb), "cf"], wr=[dstres(k)])
                        else:
                            S.add("dve", lambda e, pb=pb, k=k, k4=k4: e.tensor_scalar(
                                out=dst_fn(k), in0=ps[pb][:, k4 * 128:(k4 + 1) * 128],
                                scalar1=cf[:, gT_off + k:gT_off + k + 1], scalar2=None, op0=ALU.mult),
                                rd=[("ps", pb), "cf"], wr=[dstres(k)])

            def load_wq(c0):
                s = rr("wq", 3)
                g_ = S.newgrp()
                for k in range(8):
                    S.add("pool", lambda e, k=k: e.dma_start(out=wq[s][:, k, :], in_=w_in[k * 128:(k + 1) * 128, c0:c0 + 128]),
                          wr=[("wq", s, k)], dkey=f"wq{s}", grp=g_)
                return s

            def proj_fm(wslot, tc, pb, nk=8, wt=None, src=None, srcres=None):
                for k in range(nk):
                    if wt is None:
                        S.add("pe", lambda e, k=k: e.matmul(ps[pb][:], wq[wslot][:, k, :], hT[:, k * SEQ + tc * 512:k * SEQ + tc * 512 + 512],
                                                            start=(k == 0), stop=(k == nk - 1)),
                              rd=[("wq", wslot, k), ("hT", tc)], wr=[("ps", pb)])
                    else:
                        S.add("pe", lambda e, k=k: e.matmul(ps[pb][:], wt[:, k, :], src[:, k * SEQ + tc * 512:k * SEQ + tc * 512 + 512],
                                                            start=(k == 0), stop=(k == nk - 1)),
                              rd=[srcres[0], (srcres[1], tc)], wr=[("ps", pb)])

            def rope_to(pb, tc, dst, dstres):
                import os
                ROPE = int(os.environ.get("ROPE", "9"))
                if ROPE == 0:
                    S.add("act", lambda e: e.copy(out=dst, in_=ps[pb][:]), rd=[("ps", pb)], wr=[dstres])
                    return
                tbi = rr("tb", 8)
                t1, t2 = rr("tf", 8), rr("tf", 8)
                pr = rr("psA", 2)
                if ROPE == 3:
                    pr += 4
                S.add("act", lambda e: e.copy(out=TB[tbi][:], in_=ps[pb][:]), rd=[("ps", pb)], wr=[("tb", tbi)])
                if ROPE == 4:
                    pr = pb
                else:
                    S.add("pe", lambda e: e.matmul(ps[pr][:], perm, TB[tbi][:], start=True, stop=True),
                          rd=[("tb", tbi), "cb"], wr=[("ps", pr)])
                if ROPE in (2, 3, 4):
                    S.add("dve", lambda e: e.tensor_copy(out=TF[t1][:], in_=ps[pb][:]), rd=[("ps", pb), "cb"], wr=[("tf", t1)])
                    S.add("dve", lambda e: e.tensor_copy(out=TF[t2][:], in_=ps[pr][:]), rd=[("ps", pr), "cb"], wr=[("tf", t2)])
                else:
                    S.add("dve", lambda e: e.tensor_tensor(out=TF[t1][:], in0=ps[pb][:], in1=cb[:, CB_COS + tc * 512:CB_COS + tc * 512 + 512],
                                                           op=ALU.mult), rd=[("ps", pb), "cb"], wr=[("tf", t1)])
                    S.add("dve", lambda e: e.tensor_tensor(out=TF[t2][:], in0=ps[pr][:], in1=cb[:, CB_SIN + tc * 512:CB_SIN + tc * 512 + 512],
                                                           op=ALU.mult), rd=[("ps", pr), "cb"], wr=[("tf", t2)])
                if ROPE in (1, 2, 3, 4):
                    S.add("dve", lambda e: e.tensor_tensor(out=dst, in0=TF[t1][:], in1=TF[t2][:], op=ALU.add),
                          rd=[("tf", t1), ("tf", t2)], wr=[dstres])
                    return
                S.add("pool", lambda e: e.tensor_tensor(out=dst, in0=TF[t1][:], in1=TF[t2][:], op=ALU.add),
                      rd=[("tf", t1), ("tf", t2)], wr=[dstres])

            ALLQ = [("Q", s_, t_) for s_ in range(2) for t_ in range(4)] + [("K", s_, t_) for s_ in range(2) for t_ in range(4)] \
                + [("V", t_) for t_ in range(16)]
            OAALL = [("oa", t_) for t_ in range(4)]

            def stage1(b):
                for tt in range(16):
                    xs = tt % 2
                    r0 = b * SEQ + tt * 128
                    S.add("sp", lambda e, xs=xs, r0=r0: e.dma_start(out=xt[xs][:], in_=x[r0:r0 + 128, :]),
                          wr=[("xt", xs)], dkey=f"xt{xs}")
                    rmsnorm_rs(xt[xs], ("xt", xs), xs)
                    S.add("dve", lambda e, xs=xs: e.tensor_scalar(out=xt[xs][:], in0=xt[xs][:], scalar1=sm[:, xs:xs + 1], scalar2=None,
                                                                  op0=ALU.mult), rd=[("xt", xs), ("sm", xs)], wr=[("xt", xs)])
                    transposes_f32(xt[xs], ("xt", xs),
                                   lambda k, tt=tt: hT[:, k * SEQ + tt * 128:k * SEQ + tt * 128 + 128],
                                   lambda k, tt=tt: ("hT", tt // 4), CF_GMIXT)

            def qk_proj(cq, ck, slot):
                for (c0, base, nm) in ((cq, slot * SEQ, "Q"), (ck, 2 * SEQ + slot * SEQ, "K")):
                    ws = load_wq(c0)
                    for tc in range(4):
                        pb = 2 + rr("psA", 2)
                        proj_fm(ws, tc, pb)
                        rope_to(pb, tc, qkv[:, base + tc * 512:base + tc * 512 + 512], (nm, slot, tc))

            def v_proj(c0):
                g_ = S.newgrp()
                for k in range(8):
                    S.add("pool", lambda e, k=k: e.dma_start(out=od[:, k * 512:(k + 1) * 512], in_=w_in[k * 128:(k + 1) * 128, c0:c0 + 512]),
                          wr=[("od", k)], dkey="wv", grp=g_)
                for tt in range(16):
                    pb = 2 + rr("psA", 2)
                    for k in range(8):
                        S.add("pe", lambda e, k=k, tt=tt, pb=pb: e.matmul(ps[pb][:], hT[:, k * SEQ + tt * 128:k * SEQ + tt * 128 + 128],
                                                                          od[:, k * 512:(k + 1) * 512], start=(k == 0), stop=(k == 7)),
                              rd=[("od", k), ("hT", tt // 4)], wr=[("ps", pb)])
                    if tt % 2 == 0:
                        S.add("act", lambda e, tt=tt, pb=pb: e.copy(out=qkv[:, 4 * SEQ + tt * 512:4 * SEQ + tt * 512 + 512], in_=ps[pb][:]),
                              rd=[("ps", pb)], wr=[("V", tt)])
                    else:
                        S.add("dve", lambda e, tt=tt, pb=pb: e.tensor_copy(out=qkv[:, 4 * SEQ + tt * 512:4 * SEQ + tt * 512 + 512], in_=ps[pb][:]),
                              rd=[("ps", pb)], wr=[("V", tt)])

            def moba_ksum(slot):
                KT0 = 2 * SEQ + slot * SEQ
                S.add("dve", lambda e: e.reduce_sum(out=ksf[:], in_=qkv[:, KT0:KT0 + SEQ].rearrange("p (j t) -> p j t", t=256),
                                                    axis=AX.X), rd=[("K", slot, t) for t in range(4)], wr=["ksf"])
                S.add("dve", lambda e: e.tensor_copy(out=ksum[slot][:], in_=ksf[:]), rd=["ksf"], wr=[("ksum", slot)])

            def moba_head(p, hh, slot, mode="attn", inter=None):
                h = 2 * p + hh
                bp = hh * 64
                bs = h % 2
                QT0, KT0 = slot * SEQ, 2 * SEQ + slot * SEQ
                def gate_step(qt):
                    nb = qt // 2
                    gi = nb - 4
                    pg = 2 + rr("psA", 2)
                    S.add("pe", lambda e, qt=qt, pg=pg: e.matmul(ps[pg][:, 0:8], qkv[bp:bp + 64, QT0 + qt * 128:QT0 + qt * 128 + 128],
                                                                 ksum[slot][bp:bp + 64, 0:8], start=True, stop=True),
                          rd=[("Q", slot, qt // 4), ("ksum", slot)], wr=[("ps", pg)])
                    S.add("dve", lambda e, pg=pg, gi=gi, nb=nb: e.tensor_copy(out=gsb[gi][:, 0:nb], in_=ps[pg][:, 0:nb]),
                          rd=[("ps", pg)], wr=[f"gsb{gi}"])
                    S.add("dve", lambda e, gi=gi: e.max(out=sm[:, 16:24], in_=gsb[gi][:, 0:8]), rd=[f"gsb{gi}"], wr=["m8"])
                    S.add("dve", lambda e, gi=gi: e.tensor_scalar(out=sm[:, 24:32], in0=gsb[gi][:, 0:8], scalar1=sm[:, 18:19],
                                                                  scalar2=30000.0, op0=ALU.is_ge, op1=ALU.mult),
                          rd=[f"gsb{gi}", "m8"], wr=["bq"])
                    pt = 2 + rr("psA", 2)
                    S.add("pe", lambda e, pt=pt: e.transpose(out=ps[pt][0:8, 0:128], in_=sm[:, 24:32], identity=identF),
                          rd=["bq", "cf"], wr=[("ps", pt)])
                    S.add("dve", lambda e, pt=pt, qt=qt: e.tensor_scalar(
                        out=biasT[bs][0:8, (qt - 8) * 128:(qt - 8) * 128 + 128], in0=ps[pt][0:8, 0:128],
                        scalar1=-30000.0, scalar2=None, op0=ALU.add), rd=[("ps", pt)], wr=[("biasT", bs, (qt - 8) // 2)])
                if mode == "gate":
                    return [partial(gate_step, qt) for qt in range(8, 16)]
                for qb in range(8):
                    if inter:
                        inter.pop(0)()
                    po, pl = 4 + (qb % 2) * 2, 5 + (qb % 2) * 2
                    nkt = 2 * qb + 2
                    def pv_ops(kt, tbi, po=po, pl=pl, nkt=nkt):
                        S.add("pe", lambda e: e.matmul(
                            ps[po][0:64, 0:256], qkv[:, 4 * SEQ + kt * 512 + h * 64:4 * SEQ + kt * 512 + h * 64 + 64], TB[tbi][:, 0:256],
                            start=(kt == 0), stop=(kt == nkt - 1)), rd=[("V", kt), ("tb", tbi)], wr=[("ps", po)])
                        S.add("pe", lambda e: e.matmul(
                            ps[pl][0:64, 0:256], onesB[:, 0:64], TB[tbi][:, 0:256],
                            start=(kt == 0), stop=(kt == nkt - 1)), rd=["cb", ("tb", tbi)], wr=[("ps", pl)])
                    pend = None
                    for kt in range(nkt):
                        pS = kt % 2
                        own = (kt // 2 == qb)
                        need_bias = (not own) and qb >= 4
                        S.add("pe", lambda e, kt=kt, pS=pS, qb=qb, nbias=need_bias: e.matmul(
                            ps[pS][:, 0:256], qkv[bp:bp + 64, KT0 + kt * 128:KT0 + kt * 128 + 128],
                            qkv[bp:bp + 64, QT0 + qb * 256:QT0 + qb * 256 + 256], start=True, stop=(not nbias)),
                            rd=[("K", slot, kt // 4), ("Q", slot, qb // 2)], wr=[("ps", pS)])
                        if need_bias:
                            j = kt // 2
                            S.add("pe", lambda e, pS=pS, j=j, qb=qb: e.matmul(
                                ps[pS][:, 0:256], sel[0:8, j * 128:(j + 1) * 128], biasT[bs][0:8, (qb - 4) * 256:(qb - 4) * 256 + 256],
                                start=False, stop=True), rd=["sel", ("biasT", bs, qb - 4)], wr=[("ps", pS)])
                        tbi = rr("tb", 8)
                        S.add("act", lambda e, pS=pS, tbi=tbi: e.activation(out=TB[tbi][:, 0:256], in_=ps[pS][:, 0:256], func=AF.Exp,
                                                                            scale=0.125), rd=[("ps", pS)], wr=[("tb", tbi)])
                        if own:
                            kto = kt - 2 * qb
                            S.add("pool", lambda e, tbi=tbi, kto=kto: e.tensor_tensor(out=TB[tbi][:, 0:256], in0=TB[tbi][:, 0:256],
                                                                                     in1=maskj(kto, 256), op=ALU.mult),
                                  rd=[("tb", tbi), "cb"], wr=[("tb", tbi)])
                        if pend is not None:
                            pv_ops(*pend)
                        pend = (kt, tbi)
                    pv_ops(*pend)
                    t1 = rr("tf", 8)
                    S.add("dve", lambda e, t1=t1, pl=pl: e.reciprocal(out=TF[t1][0:64, 0:256], in_=ps[pl][0:64, 0:256]),
                          rd=[("ps", pl)], wr=[("tf", t1)])
                    S.add("dve", lambda e, t1=t1, po=po, qb=qb: e.tensor_tensor(
                        out=oa[bp:bp + 64, p * SEQ + qb * 256:p * SEQ + qb * 256 + 256], in0=ps[po][0:64, 0:256], in1=TF[t1][0:64, 0:256],
                        op=ALU.mult), rd=[("ps", po), ("tf", t1)], wr=[("oa", qb // 2), "wout_all"] + [("wout", k_) for k_ in range(8)])

            def diff_head(h, slot):
                QT0, KT0 = slot * SEQ, 2 * SEQ + slot * SEQ
                for qc in range(4):
                    nkt = 4 * qc + 4
                    def pv_ops(kt, tbis, nkt=nkt, qc=qc):
                        c0 = max(0, kt - 4 * qc) * 128
                        for m in range(2):
                            tbi = tbis[m]
                            S.add("pe", lambda e, tbi=tbi, m=m: e.matmul(
                                ps[4 + m][:, c0:512], qkv[:, 4 * SEQ + kt * 512 + h * 128:4 * SEQ + kt * 512 + h * 128 + 128], TB[tbi][:, c0:512],
                                start=(kt == 0), stop=(kt == nkt - 1)), rd=[("V", kt), ("tb", tbi)], wr=[("ps", 4 + m)])
                            S.add("pe", lambda e, tbi=tbi, m=m: e.matmul(
                                ps[6 + m][:, c0:512], onesB, TB[tbi][:, c0:512], start=(kt == 0), stop=(kt == nkt - 1)),
                                rd=["cb", ("tb", tbi)], wr=[("ps", 6 + m)])
                    pend = None
                    for kt in range(nkt):
                        tbis = []
                        c0 = max(0, kt - 4 * qc) * 128
                        for m in range(2):
                            bp = m * 64
                            pS = m
                            S.add("pe", lambda e, kt=kt, pS=pS, bp=bp, qc=qc, c0=c0: e.matmul(
                                ps[pS][:, c0:512], qkv[bp:bp + 64, KT0 + kt * 128:KT0 + kt * 128 + 128],
                                qkv[bp:bp + 64, QT0 + qc * 512 + c0:QT0 + qc * 512 + 512], start=True, stop=True),
                                rd=[("K", slot, kt // 4), ("Q", slot, qc)], wr=[("ps", pS)])
                            tbi = rr("tb", 8)
                            tbis.append(tbi)
                            S.add("act", lambda e, pS=pS, tbi=tbi, c0=c0: e.activation(out=TB[tbi][:, c0:512], in_=ps[pS][:, c0:512], func=AF.Exp, scale=0.125),
                                  rd=[("ps", pS)], wr=[("tb", tbi)])
                            if kt >= 4 * qc:
                                j = kt - 4 * qc
                                eng = "pool" if m == 0 else "dve"
                                S.add(eng, lambda e, tbi=tbi, j=j, c0=c0: e.tensor_tensor(out=TB[tbi][:, c0:512], in0=TB[tbi][:, c0:512],
                                                                                         in1=cb[:, CB_MASK + j * 512 + c0:CB_MASK + j * 512 + 512],
                                                                                         op=ALU.mult), rd=[("tb", tbi), "cb"], wr=[("tb", tbi)])
                        if pend is not None:
                            pv_ops(*pend)
                        pend = (kt, tuple(tbis))
                    pv_ops(*pend)
                    r1, r2, u1, u2 = rr("tf", 8), rr("tf", 8), rr("tf", 8), rr("tf", 8)
                    S.add("dve", lambda e, r1=r1: e.reciprocal(out=TF[r1][:], in_=ps[6][:]), rd=[("ps", 6)], wr=[("tf", r1)])
                    S.add("dve", lambda e, r2=r2: e.reciprocal(out=TF[r2][:], in_=ps[7][:]), rd=[("ps", 7)], wr=[("tf", r2)])
                    S.add("dve", lambda e, r1=r1, u1=u1: e.tensor_tensor(out=TF[u1][:], in0=ps[4][:], in1=TF[r1][:], op=ALU.mult),
                          rd=[("ps", 4), ("tf", r1)], wr=[("tf", u1)])
                    S.add("dve", lambda e, r2=r2, u2=u2: e.tensor_tensor(out=TF[u2][:], in0=ps[5][:], in1=TF[r2][:], op=ALU.mult),
                          rd=[("ps", 5), ("tf", r2)], wr=[("tf", u2)])
                    S.add("dve", lambda e, r1=r1, u1=u1, u2=u2: e.scalar_tensor_tensor(
                        out=TF[r1][:], in0=TF[u2][:], scalar=neglam[:, 0:1], in1=TF[u1][:], op0=ALU.mult, op1=ALU.add),
                        rd=[("tf", u1), ("tf", u2), "neglam"], wr=[("tf", r1)])
                    S.add("pool", lambda e, r1=r1, r2=r2: e.tensor_tensor(out=TF[r2][:], in0=TF[r1][:], in1=TF[r1][:], op=ALU.mult),
                          rd=[("tf", r1)], wr=[("tf", r2)])
                    pn = 2 + rr("psA", 2)
                    S.add("pe", lambda e, r2=r2, pn=pn: e.matmul(ps[pn][:], onesF[:], TF[r2][:], start=True, stop=True),
                          rd=[("tf", r2), "onesF"], wr=[("ps", pn)])
                    S.add("act", lambda e, u1=u1, pn=pn: e.activation(out=TF[u1][:], in_=ps[pn][:], func=AF.Sqrt, bias=epsT[:, 0:1],
                                                                      scale=1.0 / 128), rd=[("ps", pn), "epsT"], wr=[("tf", u1)])
                    S.add("dve", lambda e, u1=u1: e.reciprocal(out=TF[u1][:], in_=TF[u1][:]), rd=[("tf", u1)], wr=[("tf", u1)])
                    S.add("dve", lambda e, u1=u1, r1=r1: e.tensor_tensor(out=TF[r1][:], in0=TF[r1][:], in1=TF[u1][:], op=ALU.mult),
                          rd=[("tf", u1), ("tf", r1)], wr=[("tf", r1)])
                    S.add("act", lambda e, r1=r1, qc=qc: e.activation(out=od[:, h * SEQ + qc * 512:h * SEQ + qc * 512 + 512], in_=TF[r1][:],
                                                                      func=AF.Copy, scale=gsub8[:, 0:1]),
                          rd=[("tf", r1), "gsub8"], wr=[("od", h * 4 + qc)])

            def merge_oc(oc):
                g0s = load_wq(3072 + oc * 128)
                g1s = load_wq(4096 + oc * 128)
                g_ = S.newgrp()
                for k in range(4):
                    S.add("pool", lambda e, k=k: e.dma_start(out=wb[0][:, k, :], in_=w_bm[k * 128:(k + 1) * 128, oc * 128:(oc + 1) * 128]),
                          wr=[("wb", 0, k)], dkey="wb0", grp=g_)
                    S.add("pool", lambda e, k=k: e.dma_start(out=wb[1][:, k, :], in_=w_bd[k * 128:(k + 1) * 128, oc * 128:(oc + 1) * 128]),
                          wr=[("wb", 1, k)], dkey="wb1", grp=g_)
                for tc in range(4):
                    sg = []
                    for gi, gs in enumerate((g0s, g1s)):
                        pb = gi
                        proj_fm(gs, tc, pb)
                        tbi = rr("tb", 8)
                        S.add("act", lambda e, pb=pb, tbi=tbi: e.activation(out=TB[tbi][:], in_=ps[pb][:], func=AF.Sigmoid),
                              rd=[("ps", pb)], wr=[("tb", tbi)])
                        sg.append(tbi)
                    ms = []
                    for bi in range(2):
                        src = oa if bi == 0 else od
                        pb = 2 + bi
                        for k in range(4):
                            S.add("pe", lambda e, k=k, bi=bi, pb=pb, src=src, tc=tc: e.matmul(
                                ps[pb][:], wb[bi][:, k, :], src[:, k * SEQ + tc * 512:k * SEQ + tc * 512 + 512], start=(k == 0), stop=(k == 3)),
                                rd=[("wb", bi, k)] + ([("oa", tc)] if bi == 0 else [("od", k * 4 + tc)]), wr=[("ps", pb)])
                        ti = rr("tf", 8)
                        S.add("dve", lambda e, pb=pb, ti=ti, tbi=sg[bi]: e.tensor_tensor(out=TF[ti][:], in0=ps[pb][:], in1=TB[tbi][:], op=ALU.mult),
                              rd=[("ps", pb), ("tb", sg[bi])], wr=[("tf", ti)])
                        ms.append(ti)
                    S.add("pool", lambda e, tc=tc, ms=tuple(ms): e.tensor_tensor(
                        out=qkv[:, oc * SEQ + tc * 512:oc * SEQ + tc * 512 + 512], in0=TF[ms[0]][:], in1=TF[ms[1]][:], op=ALU.add),
                        rd=[("tf", ms[0]), ("tf", ms[1])], wr=ALLQ + [("mg", tc)])

            def load_wout():
                g_ = S.newgrp()
                S.add("pool", lambda e: e.memset(sm[:, 509:510], 0.0), rd=["wout_all"], wr=OAALL + ["oagate"])
                for k in range(8):
                    S.add("pool", lambda e, k=k: e.dma_start(out=oa[:, k * 1024:(k + 1) * 1024], in_=w_out[k * 128:(k + 1) * 128, :]),
                          rd=["oagate"], wr=[("wout", k)], dkey="wo", grp=g_)

            def tail_tile(b, tt):
                xs = tt % 2
                tile = b * 16 + tt
                r0 = b * SEQ + tt * 128
                S.add("sp", lambda e: e.dma_start(out=xt[xs][:], in_=x[r0:r0 + 128, :]), wr=[("xt", xs)], dkey=f"xt{xs}")
                for half in range(2):
                    pb = half
                    for k in range(8):
                        S.add("pe", lambda e, k=k, half=half, pb=pb: e.matmul(
                            ps[pb][:], qkv[:, k * SEQ + tt * 128:k * SEQ + tt * 128 + 128], oa[:, k * 1024 + half * 512:k * 1024 + half * 512 + 512],
                            start=(k == 0), stop=(k == 7)), rd=[("mg", tt // 4), ("wout", k), "wout_all"] + ALLQ + OAALL, wr=[("ps", pb)])
                    S.add("dve", lambda e, half=half, pb=pb: e.tensor_tensor(
                        out=xt[xs][:, half * 512:(half + 1) * 512], in0=ps[pb][:], in1=xt[xs][:, half * 512:(half + 1) * 512], op=ALU.add),
                        rd=[("ps", pb), ("xt", xs)], wr=[("xt", xs)])
                S.add("sp", lambda e: e.dma_start(out=y[r0:r0 + 128, :], in_=xt[xs][:]), rd=[("xt", xs)], wr=[("y", tile)], dkey=f"yst{xs}")
                if stage < 2:
                    return
                rmsnorm_rs(xt[xs], ("xt", xs), 2 + xs)
                S.add("dve", lambda e: e.tensor_scalar(out=xt[xs][:], in0=xt[xs][:], scalar1=sm[:, 2 + xs:3 + xs], scalar2=None,
                                                       op0=ALU.mult), rd=[("xt", xs), ("sm", 2 + xs)], wr=[("xt", xs)])
                S.add("pool", lambda e: e.tensor_tensor(out=h2[xs][:], in0=xt[xs][:], in1=gffnB[:], op=ALU.mult),
                      rd=[("xt", xs), "gffnB"], wr=[("h2", xs)])
                transposes_f32(xt[xs], ("xt", xs), lambda k: h2T[:, k, :], lambda k: "h2T", CF_GFFNT)
                pr = 2 + rr("psA", 2)
                for k in range(8):
                    S.add("pe", lambda e, k=k: e.matmul(ps[pr][:, 0:36], h2T[:, k, :], wr[:, k, :], start=(k == 0), stop=False),
                          rd=["h2T", "wr"], wr=[("ps", pr)])
                S.add("pe", lambda e: e.matmul(ps[pr][:, 0:36], onesF[0:1, :], brow[0:1, :], start=False, stop=True),
                      rd=["onesF", "brow"], wr=[("ps", pr)])
                LG, GM, NGM, GS, PG, GSEL, GB, EM, M8, OH0, MM, OH1, DD, SGD = 32, 68, 69, 70, 71, 72, 76, 80, 112, 120, 152, 184, 216, 217
                RK, OK, SV, TMP = 224, 256, 288, 320
                R = "rt"

                def V(fn, rd=(), wr=(R,)):
                    S.add("dve", fn, rd=[R] + list(rd), wr=list(wr))
                S.add("dve", lambda e: e.tensor_copy(out=sm[:, LG:LG + 36], in_=ps[pr][:, 0:36]), rd=[("ps", pr)], wr=[R])
                V(lambda e: e.reduce_max(out=sm[:, GM:GM + 1], in_=sm[:, LG:LG + 4], axis=AX.X))
                V(lambda e: e.tensor_scalar(out=sm[:, NGM:NGM + 1], in0=sm[:, GM:GM + 1], scalar1=-1.0, scalar2=None, op0=ALU.mult))
                S.add("act", lambda e: e.activation(out=sm[:, GSEL:GSEL + 4], in_=sm[:, LG:LG + 4], func=AF.Exp, bias=sm[:, NGM:NGM + 1],
                                                    accum_out=sm[:, GS:GS + 1]), rd=[R], wr=[R])
                V(lambda e: e.reciprocal(out=sm[:, PG:PG + 1], in_=sm[:, GS:GS + 1]))
                V(lambda e: e.tensor_scalar(out=sm[:, GB:GB + 4], in0=sm[:, LG:LG + 4], scalar1=sm[:, GM:GM + 1], scalar2=1e9,
                                            op0=ALU.is_ge, op1=ALU.mult))
                V(lambda e: e.tensor_scalar(out=sm[:, GB:GB + 4], in0=sm[:, GB:GB + 4], scalar1=-1e9, scalar2=None, op0=ALU.add))
                for g in range(4):
                    V(lambda e, g=g: e.tensor_scalar(out=sm[:, EM + g * 8:EM + g * 8 + 8], in0=sm[:, LG + 4 + g * 8:LG + 12 + g * 8],
                                                     scalar1=sm[:, GB + g:GB + g + 1], scalar2=None, op0=ALU.add))
                V(lambda e: e.max(out=sm[:, M8:M8 + 8], in_=sm[:, EM:EM + 32]))
                V(lambda e: e.tensor_scalar(out=sm[:, OH0:OH0 + 32], in0=sm[:, EM:EM + 32], scalar1=sm[:, M8:M8 + 1], scalar2=None,
                                            op0=ALU.is_ge))
                V(lambda e: e.tensor_scalar(out=sm[:, MM:MM + 32], in0=sm[:, EM:EM + 32], scalar1=sm[:, M8 + 1:M8 + 2], scalar2=None,
                                            op0=ALU.is_ge))
                V(lambda e: e.tensor_tensor(out=sm[:, OH1:OH1 + 32], in0=sm[:, MM:MM + 32], in1=sm[:, OH0:OH0 + 32], op=ALU.subtract))
                V(lambda e: e.tensor_tensor(out=sm[:, DD:DD + 1], in0=sm[:, M8:M8 + 1], in1=sm[:, M8 + 1:M8 + 2], op=ALU.subtract))
                S.add("act", lambda e: e.activation(out=sm[:, SGD:SGD + 1], in_=sm[:, DD:DD + 1], func=AF.Sigmoid), rd=[R], wr=[R])
                V(lambda e: e.tensor_tensor(out=wts[:, 2 * tile:2 * tile + 1], in0=sm[:, SGD:SGD + 1], in1=sm[:, PG:PG + 1],
                                            op=ALU.mult), wr=[R, "wts"])
                V(lambda e: e.tensor_tensor(out=wts[:, 2 * tile + 1:2 * tile + 2], in0=sm[:, PG:PG + 1],
                                            in1=wts[:, 2 * tile:2 * tile + 1], op=ALU.subtract), rd=["wts"], wr=[R, "wts"])
                tbi = rr("tb", 8)
                S.add("dve", lambda e: e.tensor_copy(out=TB[tbi][:, 0:32], in_=sm[:, MM:MM + 32]), rd=[R], wr=[("tb", tbi)])
                pk = 2 + rr("psA", 2)
                S.add("pe", lambda e: e.matmul(ps[pk][:, 0:32], ustr, TB[tbi][:, 0:32], start=True, stop=True),
                      rd=[("tb", tbi), "cb"], wr=[("ps", pk)])
                S.add("pe", lambda e: e.matmul(ps[pk][:, 32:64], onesB, TB[tbi][:, 0:32], start=True, stop=True),
                      rd=[("tb", tbi), "cb"], wr=[("ps", pk)])
                S.add("dve", lambda e: e.tensor_tensor(out=sm[:, RK:RK + 32], in0=ps[pk][:, 0:32], in1=carry[:], op=ALU.add),
                      rd=[R, ("ps", pk), "carry"], wr=[R])
                S.add("dve", lambda e: e.tensor_tensor(out=carry[:], in0=ps[pk][:, 32:64], in1=carry[:], op=ALU.add),
                      rd=[R, ("ps", pk), "carry"], wr=["carry"])
                BIG = float(1 << 22)
                V(lambda e: e.tensor_scalar(out=sm[:, OK:OK + 32], in0=sm[:, RK:RK + 32], scalar1=float(CAP), scalar2=None, op0=ALU.is_lt))
                V(lambda e: e.tensor_tensor(out=sm[:, SV:SV + 32], in0=sm[:, RK:RK + 32], in1=ebase, op=ALU.add), rd=["cf"])
                V(lambda e: e.tensor_scalar(out=sm[:, SV:SV + 32], in0=sm[:, SV:SV + 32], scalar1=-BIG, scalar2=None, op0=ALU.add))
                V(lambda e: e.tensor_tensor(out=sm[:, SV:SV + 32], in0=sm[:, SV:SV + 32], in1=sm[:, OK:OK + 32], op=ALU.mult))
                V(lambda e: e.tensor_scalar(out=sm[:, SV:SV + 32], in0=sm[:, SV:SV + 32], scalar1=BIG, scalar2=None, op0=ALU.add))
                for kk, OH in enumerate((OH0, OH1)):
                    V(lambda e, OH=OH: e.tensor_tensor(out=sm[:, TMP:TMP + 32], in0=sm[:, SV:SV + 32], in1=sm[:, OH:OH + 32], op=ALU.mult))
                    V(lambda e, kk=kk: e.reduce_sum(out=sm[:, TMP + 32 + kk:TMP + 33 + kk], in_=sm[:, TMP:TMP + 32], axis=AX.X))
                    V(lambda e, kk=kk: e.tensor_copy(out=sl[:, 2 * tile + kk:2 * tile + kk + 1],
                                                     in_=sm[:, TMP + 32 + kk:TMP + 33 + kk]), wr=[R, ("sl", tile, kk)])
                    S.add("pool", lambda e, kk=kk: e.indirect_dma_start(
                        out=xdisp, out_offset=bass.IndirectOffsetOnAxis(ap=sl[:, 2 * tile + kk:2 * tile + kk + 1], axis=0),
                        in_=h2[xs][:, :], in_offset=None, bounds_check=bnd(e, "A"), oob_is_err=False),
                        rd=[("h2", xs), ("sl", tile, kk)] + [("xz", i_) for i_ in range(NE * CAP // 128)], wr=["xdisp_w"], dkey=f"sc{xs}{kk}")

            import os
            KSTOP = float(os.environ.get("KSTOP", "99"))
            for b in range(NB):
                stage1(b)
                if b == 0:
                    for i in range(NE * CAP // 128):
                        S.add("sp", lambda e, i=i: e.dma_start(out=xdisp[i * 128:(i + 1) * 128, :], in_=zt[:]), rd=["zt"], wr=[("xz", i)], dkey="xz")
                if KSTOP <= 1:
                    break
                v_proj(1024)
                for p in range(4):
                    qk_proj(p * 128, 512 + p * 128, p % 2)
                    moba_ksum(p % 2)
                    for hh in range(2):
                        for st_ in moba_head(p, hh, p % 2, mode="gate"):
                            st_()
                        moba_head(p, hh, p % 2)
                if KSTOP <= 3:
                    break
                v_proj(2560)
                for h in range(4):
                    qk_proj(1536 + h * 128, 2048 + h * 128, h % 2)
                    diff_head(h, h % 2)
                if KSTOP <= 4:
                    break
                for oc in range(8):
                    merge_oc(oc)
                load_wout()
                if KSTOP <= 5:
                    break
                for tt in range(16):
                    tail_tile(b, tt)
            S.emit()

        if stage < 3:
            return nc
        with ExitStack() as st:
            S = Sched(nc, top, "B")
            A = partial(sb, st=st)
            pTs = [st.enter_context(nc.psum_tensor(f"pT{i}", [128, 1024], BF16)) for i in range(2)]
            pG = [st.enter_context(nc.psum_tensor(f"pG{i}", [128, 512], F32)) for i in range(2)]
            pU = [st.enter_context(nc.psum_tensor(f"pU{i}", [128, 512], F32)) for i in range(2)]
            pY = [st.enter_context(nc.psum_tensor(f"pY{i}", [128, 512], F32)) for i in range(2)]
            identB = A("identB2", [128, 128], BF16)
            w1b = [A(f"w1b{i}", [128, 8, 512], BF16) for i in range(2)]
            w3b = [A(f"w3b{i}", [128, 8, 512], BF16) for i in range(2)]
            w2b = [A(f"w2b{i}", [128, 4, 1024], BF16) for i in range(2)]
            xd = [A(f"xd{i}", [128, D], BF16) for i in range(2)]
            xT = [A(f"xT{i}", [128, 8, CAP], BF16) for i in range(2)]
            sgl = [A(f"sgl{i}", [128, 512], F32) for i in range(2)]
            aT = [A(f"aT{i}", [128, 4, CAP], BF16) for i in range(2)]
            yo = [A(f"yo{i}", [128, D], F32) for i in range(2)]
            S.add("pool", lambda e: e.dma_start(out=identB[:], in_=cb_d[:, CB_IDENT:CB_IDENT + 128]), wr=["identB"], dkey="identB")
            nblk = CAP // 128
            ctr = 0
            w3f = [A(f"w3f{i}", [128, 8, 512], F32) for i in range(2)]
            w2f = [A(f"w2f{i}", [128, 4, 1024], F32) for i in range(2)]

            def load_w(ex):
                s = ex % 2
                g_ = S.newgrp()
                for k in range(8):
                    S.add("pool", lambda e, k=k: e.dma_start(out=w1b[s][:, k, :], in_=w1[ex, k * 128:(k + 1) * 128, :]),
                          wr=[("w1", s, k)], dkey=f"w1_{s}", grp=g_)
                for k in range(8):
                    S.add("act", lambda e, k=k: e.dma_start(out=w3f[s][:, k, :], in_=w3[ex, k * 128:(k + 1) * 128, :]),
                          wr=[("w3f", s, k)], dkey=f"w3f{s}", grp=g_)
                for k in range(4):
                    S.add("act", lambda e, k=k: e.dma_start(out=w2f[s][:, k, :], in_=w2[ex, k * 128:(k + 1) * 128, :]),
                          wr=[("w2f", s, k)], dkey=f"w2f{s}", grp=g_)

            def cast_w(ex):
                s = ex % 2
                for k in range(8):
                    if k % 2 == 0:
                        S.add("act", lambda e, k=k: e.copy(out=w3b[s][:, k, :], in_=w3f[s][:, k, :]), rd=[("w3f", s, k)], wr=[("w3", s, k)])
                    else:
                        S.add("dve", lambda e, k=k: e.tensor_copy(out=w3b[s][:, k, :], in_=w3f[s][:, k, :]), rd=[("w3f", s, k)], wr=[("w3", s, k)])
                for k in range(4):
                    if k % 2 == 0:
                        S.add("dve", lambda e, k=k: e.tensor_copy(out=w2b[s][:, k, :], in_=w2f[s][:, k, :]), rd=[("w2f", s, k)], wr=[("w2", s, k)])
                    else:
                        S.add("act", lambda e, k=k: e.copy(out=w2b[s][:, k, :], in_=w2f[s][:, k, :]), rd=[("w2f", s, k)], wr=[("w2", s, k)])

            load_w(0)
            cast_w(0)
            for ex in range(NE):
                s = ex % 2
                if ex + 1 < NE:
                    load_w(ex + 1)
                for blk in range(nblk):
                    xs = ctr % 2
                    ctr += 1
                    r0 = ex * CAP + blk * 128
                    S.add("sp", lambda e, xs=xs, r0=r0: e.dma_start(out=xd[xs][:], in_=xdisp[r0:r0 + 128, :]), wr=[("xd", xs)], dkey=f"xd{xs}")
                    pi = xs
                    for k in range(8):
                        S.add("pe", lambda e, xs=xs, k=k, pi=pi: e.transpose(out=pTs[pi][:, k * 128:(k + 1) * 128], in_=xd[xs][:, k * 128:(k + 1) * 128],
                                                                             identity=identB[:]), rd=[("xd", xs), "identB"], wr=[("pT", pi)])
                    if xs == 0:
                        S.add("act", lambda e, s=s, blk=blk, pi=pi: e.copy(out=xT[s][:, :, blk * 128:(blk + 1) * 128],
                                                                           in_=pTs[pi][:, :].rearrange("p (k c) -> p k c", c=128)),
                              rd=[("pT", pi)], wr=[("xT", s)])
                    else:
                        S.add("dve", lambda e, s=s, blk=blk, pi=pi: e.tensor_copy(out=xT[s][:, :, blk * 128:(blk + 1) * 128],
                                                                                  in_=pTs[pi][:, :].rearrange("p (k c) -> p k c", c=128)),
                              rd=[("pT", pi)], wr=[("xT", s)])
                for fc in range(4):
                    g = fc % 2
                    for k in range(8):
                        S.add("pe", lambda e, s=s, k=k, fc=fc, g=g: e.matmul(pG[g][:, 0:CAP], w1b[s][:, k, fc * 128:(fc + 1) * 128], xT[s][:, k, :],
                                                                             start=(k == 0), stop=(k == 7)), rd=[("w1", s, k), ("xT", s)], wr=[("pG", g)])
                    for k in range(8):
                        S.add("pe", lambda e, s=s, k=k, fc=fc, g=g: e.matmul(pU[g][:, 0:CAP], w3b[s][:, k, fc * 128:(fc + 1) * 128], xT[s][:, k, :],
                                                                             start=(k == 0), stop=(k == 7)), rd=[("w3", s, k), ("xT", s)], wr=[("pU", g)])
                    S.add("act", lambda e, g=g: e.activation(out=sgl[g][:, 0:CAP], in_=pG[g][:, 0:CAP], func=AF.Silu), rd=[("pG", g)], wr=[("sgl", g)])
                    S.add("dve", lambda e, s=s, fc=fc, g=g: e.tensor_tensor(out=aT[s][:, fc, :], in0=pU[g][:, 0:CAP], in1=sgl[g][:, 0:CAP], op=ALU.mult),
                          rd=[("pU", g), ("sgl", g)], wr=[("aT", s)])
                for blk in range(nblk):
                    ys = (ex * nblk + blk) % 2
                    r0 = ex * CAP + blk * 128
                    for half in range(2):
                        for j in range(4):
                            S.add("pe", lambda e, s=s, j=j, blk=blk, half=half: e.matmul(
                                pY[half][:], aT[s][:, j, blk * 128:(blk + 1) * 128], w2b[s][:, j, half * 512:(half + 1) * 512],
                                start=(j == 0), stop=(j == 3)), rd=[("aT", s), ("w2", s, j)], wr=[("pY", half)])
                        if half == 0:
                            S.add("act", lambda e, ys=ys: e.copy(out=yo[ys][:, 0:512], in_=pY[0][:]), rd=[("pY", 0)], wr=[("yo", ys)])
                        else:
                            S.add("dve", lambda e, ys=ys: e.tensor_copy(out=yo[ys][:, 512:1024], in_=pY[1][:]), rd=[("pY", 1)], wr=[("yo", ys)])
                    S.add("sp", lambda e, ys=ys, r0=r0: e.dma_start(out=ybuf[r0:r0 + 128, :], in_=yo[ys][:]), rd=[("yo", ys)], wr=[("ybuf", ex, blk)],
                          dkey=f"yo{ys}")
                if ex + 1 < NE:
                    cast_w(ex + 1)
            S.emit()

        with ExitStack() as st:
            S = Sched(nc, top, "C")
            A = partial(sb, st=st)
            x1 = [A(f"x1_{i}", [128, D], F32) for i in range(4)]
            g0 = [A(f"g0_{i}", [128, D], F32) for i in range(4)]
            g1 = [A(f"g1_{i}", [128, D], F32) for i in range(4)]
            junk = A("junkC", [128, D], BF16)
            smc = A("smc", [128, 8], F32)
            epsT = A("epsTC", [128, 1], F32)
            S.add("dve", lambda e: e.memset(epsT[:], EPS), wr=["epsT"])
            for tile in range(32):
                s = tile % 4
                r0 = tile * 128
                S.add("sp", lambda e, s=s, r0=r0: e.dma_start(out=x1[s][:], in_=y[r0:r0 + 128, :]), wr=[("x1", s)], dkey=f"x1{s}")
                S.add("pool", lambda e, s=s: e.memset(g0[s][:], 0.0), wr=[("g0", s)])
                S.add("pool", lambda e, s=s: e.memset(g1[s][:], 0.0), wr=[("g1", s)])
                S.add("pool", lambda e, s=s, tile=tile: e.indirect_dma_start(
                    out=g0[s][:, :], out_offset=None, in_=ybuf,
                    in_offset=bass.IndirectOffsetOnAxis(ap=sl[:, 2 * tile:2 * tile + 1], axis=0), bounds_check=bnd(e, "C"), oob_is_err=False),
                    wr=[("g0", s)], dkey=f"g0{s}")
                S.add("pool", lambda e, s=s, tile=tile: e.indirect_dma_start(
                    out=g1[s][:, :], out_offset=None, in_=ybuf,
                    in_offset=bass.IndirectOffsetOnAxis(ap=sl[:, 2 * tile + 1:2 * tile + 2], axis=0), bounds_check=bnd(e, "C"), oob_is_err=False),
                    wr=[("g1", s)], dkey=f"g1{s}")
                S.add("dve", lambda e, s=s, tile=tile: e.scalar_tensor_tensor(out=x1[s][:], in0=g0[s][:], scalar=wts[:, 2 * tile:2 * tile + 1],
                                                                              in1=x1[s][:], op0=ALU.mult, op1=ALU.add),
                      rd=[("g0", s), ("x1", s)], wr=[("x1", s)])
                S.add("dve", lambda e, s=s, tile=tile: e.scalar_tensor_tensor(out=x1[s][:], in0=g1[s][:], scalar=wts[:, 2 * tile + 1:2 * tile + 2],
                                                                              in1=x1[s][:], op0=ALU.mult, op1=ALU.add),
                      rd=[("g1", s), ("x1", s)], wr=[("x1", s)])
                S.add("act", lambda e, s=s: e.activation(out=junk[:], in_=x1[s][:], func=AF.Square, accum_out=smc[:, s:s + 1]),
                      rd=[("x1", s)], wr=["junk", ("smc", s)])
                S.add("act", lambda e, s=s: e.activation(out=smc[:, s:s + 1], in_=smc[:, s:s + 1], func=AF.Sqrt, bias=epsT[:, 0:1], scale=1.0 / D),
                      rd=[("smc", s), "epsT"], wr=[("smc", s)])
                S.add("dve", lambda e, s=s: e.reciprocal(out=smc[:, s:s + 1], in_=smc[:, s:s + 1]), rd=[("smc", s)], wr=[("smc", s)])
                S.add("dve", lambda e, s=s: e.scalar_tensor_tensor(out=x1[s][:], in0=x1[s][:], scalar=smc[:, s:s + 1], in1=gfinB[:],
                                                                   op0=ALU.mult, op1=ALU.mult), rd=[("x1", s), ("smc", s)], wr=[("x1", s)])
                S.add("sp", lambda e, s=s, r0=r0: e.dma_start(out=y[r0:r0 + 128, :], in_=x1[s][:]), rd=[("x1", s)], wr=[("y", tile)], dkey=f"yo{s}")
            S.emit()
    return nc


def _consts():
    cb = np.zeros((128, NCB), np.float32)
    cb[:, CB_IDENT:CB_IDENT + 128] = np.eye(128)
    r = np.arange(128)
    partner = np.where(r % 64 < 32, r + 32, r - 32)
    cb[partner, CB_PERM + r] = 1.0
    cb[:, CB_ONES:CB_ONES + 128] = 1.0
    cb[:, CB_USTR:CB_USTR + 128] = (r[:, None] < r[None, :])
    q = np.arange(512)
    for j in range(4):
        cb[:, CB_MASK + j * 512:CB_MASK + (j + 1) * 512] = (q[None, :] >= j * 128 + r[:, None])
    inv = 1.0 / (10000.0 ** (np.arange(0, 64, 2, dtype=np.float32) / 64.0))
    ang = np.arange(SEQ, dtype=np.float32)[:, None] * inv[None, :].astype(np.float32)
    ang = np.concatenate([ang, ang], axis=-1).astype(np.float32)
    cosT = np.cos(ang).T.astype(np.float32)
    sinT = np.sin(ang).T.astype(np.float32)
    sinS = sinT.copy()
    sinS[:32] *= -1.0
    cb[:, CB_COS:CB_COS + SEQ] = np.concatenate([cosT, cosT], 0)
    cb[:, CB_SIN:CB_SIN + SEQ] = np.concatenate([sinS, sinS], 0)
    sel = np.zeros((8, 1024), np.float32)
    for j in range(8):
        sel[j, j * 128:(j + 1) * 128] = 1.0
    return cb, sel


_STAGE = 99


def _prep(x, g_mix, w_in, w_branch_moba, w_branch_diff, w_out,
           diff_lambda_q1, diff_lambda_k1, diff_lambda_q2, diff_lambda_k2, diff_subln_g,
           g_ffn, w_group, b_group, w_router, b_router,
           w_expert_gate, w_expert_up, w_expert_down, g_final):
    f = lambda a: np.ascontiguousarray(np.asarray(a, dtype=np.float32))
    x = f(x)
    cb, sel = _consts()
    cf = np.zeros((128, NCF), np.float32)
    cf[:, CF_IDENT:CF_IDENT + 128] = np.eye(128)
    cf[:, CF_EBASE:CF_EBASE + 32] = (np.arange(32) * CAP)[None, :]
    cf[:, CF_GMIXT:CF_GMIXT + 8] = f(g_mix)[0].reshape(8, 128).T
    cf[:, CF_GFFNT:CF_GFFNT + 8] = f(g_ffn)[0].reshape(8, 128).T
    cf[:, CF_GSUB] = f(diff_subln_g)[0]
    cf[:, CF_LAM:CF_LAM + 256] = np.concatenate([f(diff_lambda_q1)[0], f(diff_lambda_k1)[0], f(diff_lambda_q2)[0],
                                                 f(diff_lambda_k2)[0]])[None, :]
    shared = {
        "w_in": f(w_in)[0], "w_bm": f(w_branch_moba)[0], "w_bd": f(w_branch_diff)[0], "w_out": f(w_out)[0],
        "w1": f(w_expert_gate)[0], "w3": f(w_expert_up)[0], "w2": f(w_expert_down)[0],
        "wr": np.ascontiguousarray(np.concatenate([f(w_group)[0], f(w_router)[0]], axis=1)),
        "brow": np.ascontiguousarray(np.concatenate([f(b_group)[0], f(b_router)[0]])[None, :]),
        "gffnB": np.ascontiguousarray(np.broadcast_to(f(g_ffn)[0][None, :], (128, D))),
        "gfinB": np.ascontiguousarray(np.broadcast_to(f(g_final)[None, :], (128, D))),
        "cf": cf, "cb": cb, "sel": sel,
    }
    xs = x.reshape(NCORES, TOK, D)
    return shared, xs


def kernel(**inputs):
    shared, xs = _prep(**inputs)
    nc = build_program(_STAGE)
    in_maps = [dict(shared, x=np.ascontiguousarray(xs[c])) for c in range(NCORES)]
    res = run_bass_kernel_spmd(nc, in_maps, core_ids=list(range(NCORES)))
    out = np.stack([np.asarray(r["y"]) for r in res.results], axis=0)
    return out.reshape(16, SEQ, D).astype(np.float32)
```
